# Optimizing a Trainium2 kernel written in Bass

```python
import math
import jax, jax.numpy as jnp
from jax import lax
import numpy as np

D_MODEL = 2048
BATCH = 2
SEQ = 8192
DEPTH = 1

CHUNK = 64
EPS = 1e-6

POOL_WIDTH = D_MODEL
POOL_WINDOWS = (2, 4, 8, 16)
POOL_GROUPS = len(POOL_WINDOWS)
POOL_GROUP_DIM = POOL_WIDTH // POOL_GROUPS

SSD_EXPAND = 2
SSD_INNER = SSD_EXPAND * D_MODEL
SSD_HEAD_DIM = 64
SSD_HEADS = SSD_INNER // SSD_HEAD_DIM
SSD_GROUPS = 8
SSD_HEADS_PER_GROUP = SSD_HEADS // SSD_GROUPS
SSD_STATE = 128
SSD_CONV = 4
SSD_BC_DIM = SSD_GROUPS * SSD_STATE
SSD_CONV_DIM = SSD_INNER + 2 * SSD_BC_DIM

N_BRANCHES = 2
_SPLIT_1 = POOL_WIDTH
_SPLIT_2 = _SPLIT_1 + SSD_INNER
_SPLIT_3 = _SPLIT_2 + SSD_CONV_DIM
_SPLIT_4 = _SPLIT_3 + SSD_HEADS
_SPLIT_5 = _SPLIT_4 + D_MODEL
IN_PROJ_DIM = _SPLIT_5 + D_MODEL

N_EXPERTS = 32
TOP_K = 4
D_FF = D_MODEL
SWIGLU_LIMIT = 7.0
SWIGLU_ALPHA = 1.702
EXPERT_BLOCK = 128

N_MOD = 6

kernel_name = "hybrid_pool_ssd_moe_adaln_block"


def rmsnorm(x):
    x32 = x.astype(jnp.float32)
    return x32 * lax.rsqrt(jnp.mean(x32 * x32, axis=-1, keepdims=True) + EPS)


def modulate(h, shift, scale):
    return h * (1.0 + scale[:, None, :]) + shift[:, None, :]


def pool_mixer(p, pool_w, pool_scale):
    bsz, s, _ = p.shape
    p = p.astype(jnp.float32).reshape(bsz, s, POOL_GROUPS, POOL_GROUP_DIM)
    cs = jnp.cumsum(p, axis=1)
    pos = jnp.arange(1, s + 1, dtype=jnp.float32)
    means = []
    for g, w in enumerate(POOL_WINDOWS):
        hi = cs[:, :, g]
        lo = jnp.concatenate([jnp.zeros((bsz, w, POOL_GROUP_DIM), jnp.float32),
                              cs[:, :s - w, g]], axis=1)
        count = jnp.minimum(pos, float(w))
        means.append((hi - lo) / count[None, :, None])
    mean = jnp.stack(means, axis=2)
    mixed = jnp.einsum('bsgc,gcd->bsgd', mean - p, pool_w)
    return mixed.reshape(bsz, s, POOL_WIDTH) * pool_scale


def causal_depthwise_conv(x, w, b):
    y = lax.conv_general_dilated(
        x, w.astype(x.dtype)[:, None, :], window_strides=(1,),
        padding=[(SSD_CONV - 1, 0)], dimension_numbers=('NWC', 'WIO', 'NWC'),
        feature_group_count=x.shape[-1])
    return y + b


def ssd_scan(xh, dt, a, bm, cm):
    bsz, s = xh.shape[:2]
    nc = s // CHUNK
    g, hg, p, n = SSD_GROUPS, SSD_HEADS_PER_GROUP, SSD_HEAD_DIM, SSD_STATE
    x = xh.reshape(bsz, nc, CHUNK, g, hg, p)
    dt = dt.reshape(bsz, nc, CHUNK, g, hg)
    bc = bm.reshape(bsz, nc, CHUNK, g, n)
    cc = cm.reshape(bsz, nc, CHUNK, g, n)
    da_cs = jnp.cumsum(dt * a.reshape(g, hg), axis=2)
    xdt = x * dt[..., None]
    diff = da_cs[:, :, :, None] - da_cs[:, :, None, :]
    causal = jnp.tril(jnp.ones((CHUNK, CHUNK), dtype=bool))
    lmat = jnp.exp(jnp.where(causal[:, :, None, None], diff, -jnp.inf))
    cb = jnp.einsum('bclgn,bcsgn->bclsg', cc, bc)
    y_diag = jnp.einsum('bclsgh,bcsghp->bclghp', cb[..., None] * lmat, xdt)
    decay_to_end = jnp.exp(da_cs[:, :, -1:] - da_cs)
    states = jnp.einsum('bclgn,bclghp->bcghpn', bc, xdt * decay_to_end[..., None])
    chunk_decay = jnp.exp(da_cs[:, :, -1])

    def step(carry, inp):
        st, dec = inp
        return carry * dec[..., None, None] + st, carry

    _, prev = lax.scan(step, jnp.zeros_like(states[:, 0]),
                       (jnp.moveaxis(states, 1, 0), jnp.moveaxis(chunk_decay, 1, 0)))
    prev = jnp.moveaxis(prev, 0, 1)
    y_off = jnp.einsum('bclgn,bcghpn->bclghp', cc, prev) * jnp.exp(da_cs)[..., None]
    return (y_diag + y_off).reshape(bsz, s, SSD_HEADS, p)


def hybrid_mixer(u, w_in, pool_w, pool_scale, conv_w, conv_b, dt_bias, a_log, d_skip,
                 ssd_norm_w, w_branch_pool, w_branch_ssd, w_out):
    bsz, s, _ = u.shape
    proj = jnp.matmul(u, w_in)
    p_in, z, xbc, dt_raw, g_pool, g_ssd = jnp.split(
        proj, [_SPLIT_1, _SPLIT_2, _SPLIT_3, _SPLIT_4, _SPLIT_5], axis=-1)
    y_pool = pool_mixer(p_in, pool_w, pool_scale)
    xbc = jax.nn.silu(causal_depthwise_conv(xbc.astype(jnp.float32), conv_w, conv_b))
    xs, bm, cm = jnp.split(xbc, [SSD_INNER, SSD_INNER + SSD_BC_DIM], axis=-1)
    dt = jax.nn.softplus(dt_raw.astype(jnp.float32) + dt_bias.astype(jnp.float32))
    a = -jnp.exp(a_log.astype(jnp.float32))
    xh = xs.reshape(bsz, s, SSD_HEADS, SSD_HEAD_DIM)
    y = ssd_scan(xh, dt, a,
                 bm.reshape(bsz, s, SSD_GROUPS, SSD_STATE),
                 cm.reshape(bsz, s, SSD_GROUPS, SSD_STATE))
    y = y + d_skip.astype(jnp.float32)[:, None] * xh
    y = y.reshape(bsz, s, SSD_INNER) * jax.nn.silu(z.astype(jnp.float32))
    y = rmsnorm(y.reshape(bsz, s, SSD_GROUPS, SSD_INNER // SSD_GROUPS)).reshape(bsz, s, SSD_INNER)
    y_ssd = y * ssd_norm_w
    merged = (jax.nn.sigmoid(g_pool.astype(jnp.float32)) * jnp.matmul(y_pool, w_branch_pool)
              + jax.nn.sigmoid(g_ssd.astype(jnp.float32)) * jnp.matmul(y_ssd, w_branch_ssd))
    return jnp.matmul(merged, w_out)


def moe_ffn(h, w_router, b_router, w_gate_up, b_gate_up, w_down, b_down):
    bsz, s, d = h.shape
    t = bsz * s
    n_assign = t * TOP_K
    n_blocks = n_assign // EXPERT_BLOCK + N_EXPERTS
    n_slots = n_blocks * EXPERT_BLOCK
    xf = h.reshape(t, d)
    logits = jnp.matmul(xf, w_router).astype(jnp.float32) + b_router.astype(jnp.float32)
    top_vals, top_idx = lax.top_k(logits, TOP_K)
    gates = jax.nn.softmax(top_vals, axis=-1)
    flat_e = top_idx.reshape(-1)
    order = jnp.argsort(flat_e)
    sorted_e = flat_e[order]
    sorted_tok = (order // TOP_K).astype(jnp.int32)
    sorted_gate = gates.reshape(-1)[order]
    counts = jnp.bincount(flat_e, length=N_EXPERTS)
    padded = (counts + EXPERT_BLOCK - 1) // EXPERT_BLOCK * EXPERT_BLOCK
    padded_end = jnp.cumsum(padded)
    padded_start = padded_end - padded
    group_start = jnp.cumsum(counts) - counts
    slot = padded_start[sorted_e] + jnp.arange(n_assign) - group_start[sorted_e]
    slot_tok = jnp.full((n_slots,), t, jnp.int32).at[slot].set(sorted_tok)
    slot_gate = jnp.zeros((n_slots,), jnp.float32).at[slot].set(sorted_gate)
    block_expert = jnp.minimum(
        jnp.searchsorted(padded_end, jnp.arange(n_blocks) * EXPERT_BLOCK, side='right'),
        N_EXPERTS - 1)
    x_pad = jnp.concatenate([xf, jnp.zeros((1, d), xf.dtype)], axis=0).astype(w_gate_up.dtype)
    x_blocks = x_pad[slot_tok].reshape(n_blocks, EXPERT_BLOCK, d)

    def expert_block(args):
        xb, e = args
        gu = jnp.matmul(xb, w_gate_up[e]).astype(jnp.float32) + b_gate_up[e].astype(jnp.float32)
        gate, up = jnp.split(gu, 2, axis=-1)
        gate = jnp.minimum(gate, SWIGLU_LIMIT)
        up = jnp.clip(up, -SWIGLU_LIMIT, SWIGLU_LIMIT)
        act = (up + 1.0) * gate * jax.nn.sigmoid(SWIGLU_ALPHA * gate)
        return (jnp.matmul(act.astype(w_down.dtype), w_down[e]).astype(jnp.float32)
                + b_down[e].astype(jnp.float32))

    y_blocks = lax.map(expert_block, (x_blocks, block_expert))
    y = jax.ops.segment_sum(y_blocks.reshape(n_slots, d) * slot_gate[:, None], slot_tok,
                            num_segments=t + 1)
    return y[:t].reshape(bsz, s, d)


def setup_inputs(seed: int = 0) -> dict:
    key = jax.random.key(seed)
    ks = jax.random.split(key, 24)
    f32 = jnp.float32
    L = DEPTH

    def nrm(k, shape, scale):
        return jax.random.normal(k, shape, f32) * scale

    dt0 = jnp.exp(jax.random.uniform(ks[9], (L, SSD_HEADS), f32, math.log(1e-3), math.log(1e-1)))
    return {
        "x": nrm(ks[0], (BATCH, SEQ, D_MODEL), 1.0),
        "c": nrm(ks[1], (BATCH, D_MODEL), 1.0),
        "w_ada": nrm(ks[2], (L, D_MODEL, N_MOD * D_MODEL), 0.5 * D_MODEL ** -0.5),
        "b_ada": nrm(ks[3], (L, N_MOD * D_MODEL), 0.02),
        "w_in": nrm(ks[4], (L, D_MODEL, IN_PROJ_DIM), D_MODEL ** -0.5),
        "pool_w": nrm(ks[5], (L, POOL_GROUPS, POOL_GROUP_DIM, POOL_GROUP_DIM), POOL_GROUP_DIM ** -0.5),
        "pool_scale": 1.0 + nrm(ks[6], (L, POOL_WIDTH), 0.02),
        "conv_w": nrm(ks[7], (L, SSD_CONV, SSD_CONV_DIM), SSD_CONV ** -0.5),
        "conv_b": nrm(ks[8], (L, SSD_CONV_DIM), 0.02),
        "dt_bias": dt0 + jnp.log(-jnp.expm1(-dt0)),
        "a_log": jnp.log(jax.random.uniform(ks[10], (L, SSD_HEADS), f32, 1.0, 16.0)),
        "d_skip": 1.0 + nrm(ks[11], (L, SSD_HEADS), 0.1),
        "ssd_norm_w": 1.0 + nrm(ks[12], (L, SSD_INNER), 0.02),
        "w_branch_pool": nrm(ks[13], (L, POOL_WIDTH, D_MODEL), POOL_WIDTH ** -0.5),
        "w_branch_ssd": nrm(ks[14], (L, SSD_INNER, D_MODEL), SSD_INNER ** -0.5),
        "w_out": nrm(ks[15], (L, D_MODEL, D_MODEL), D_MODEL ** -0.5),
        "w_router": nrm(ks[16], (L, D_MODEL, N_EXPERTS), D_MODEL ** -0.5),
        "b_router": nrm(ks[17], (L, N_EXPERTS), 0.01),
        "w_gate_up": nrm(ks[18], (L, N_EXPERTS, D_MODEL, 2 * D_FF), D_MODEL ** -0.5),
        "b_gate_up": nrm(ks[19], (L, N_EXPERTS, 2 * D_FF), 0.01),
        "w_down": nrm(ks[20], (L, N_EXPERTS, D_FF, D_MODEL), D_FF ** -0.5),
        "b_down": nrm(ks[21], (L, N_EXPERTS, D_MODEL), 0.01),
        "final_norm_w": 1.0 + nrm(ks[22], (D_MODEL,), 0.02),
    }


def reference(x, c, w_ada, b_ada, w_in, pool_w, pool_scale, conv_w, conv_b, dt_bias, a_log,
              d_skip, ssd_norm_w, w_branch_pool, w_branch_ssd, w_out, w_router, b_router,
              w_gate_up, b_gate_up, w_down, b_down, final_norm_w):
    h = x.astype(jnp.float32)
    c_act = jax.nn.silu(c.astype(jnp.float32))
    for layer in range(DEPTH):
        mod = jnp.matmul(c_act, w_ada[layer]) + b_ada[layer]
        sh1, sc1, g1, sh2, sc2, g2 = jnp.split(mod.astype(jnp.float32), N_MOD, axis=-1)
        u = modulate(rmsnorm(h), sh1, sc1)
        mix = hybrid_mixer(u, w_in[layer], pool_w[layer], pool_scale[layer], conv_w[layer],
                           conv_b[layer], dt_bias[layer], a_log[layer], d_skip[layer],
                           ssd_norm_w[layer], w_branch_pool[layer], w_branch_ssd[layer],
                           w_out[layer])
        h = h + g1[:, None, :] * mix.astype(jnp.float32)
        u = modulate(rmsnorm(h), sh2, sc2)
        ffn = moe_ffn(u, w_router[layer], b_router[layer], w_gate_up[layer], b_gate_up[layer],
                      w_down[layer], b_down[layer])
        h = h + g2[:, None, :] * ffn
    return (rmsnorm(h) * final_norm_w).astype(x.dtype)
```

```python
import numpy as np
import concourse.bass as bass
import concourse.mybir as mybir

F32 = mybir.dt.float32
BF16 = mybir.dt.bfloat16
I32 = mybir.dt.int32
U32 = mybir.dt.uint32
AF = mybir.ActivationFunctionType
ALU = mybir.AluOpType
AX = mybir.AxisListType


class K:
    def __init__(self, nc, n_dma_sems=40, same_engine_sync=True):
        self.nc = nc
        self.eng = {"pe": nc.tensor, "act": nc.scalar, "dve": nc.vector, "pool": nc.gpsimd, "sp": nc.sync}
        self.sem = {e: nc.alloc_semaphore("c_" + e) for e in self.eng}
        self.cnt = {e: 0 for e in self.eng}
        self.seen = {e: {} for e in self.eng}
        self.dsem = [nc.alloc_semaphore("d%d" % i) for i in range(n_dma_sems)]
        self.dcnt = [0] * n_dma_sems
        self.dnext = 0
        self.last_w = {}
        self.readers = {}
        self.pe_pending_reads = []
        self.pe_pending_writes = []
        self.same_engine_sync = same_engine_sync
        self.n_wait = 0
        self.n_ins = 0

    def _wait_tok(self, e, tok):
        if tok is None:
            return
        kind = tok[0]
        if kind == "eng":
            _, e2, c = tok
            if e2 == e and (e == "pe" or not self.same_engine_sync):
                return
            key = ("e", e2)
            if self.seen[e].get(key, 0) >= c:
                return
            self.eng[e].wait_ge(self.sem[e2], c)
            self.seen[e][key] = c
            self.n_wait += 1
        elif kind == "cc":
            _, c = tok
            key = ("cc",)
            if self.seen[e].get(key, 0) >= c:
                return
            self.eng[e].wait_ge(self.cc_sem, c)
            self.seen[e][key] = c
            self.n_wait += 1
        else:
            _, i, c = tok
            key = ("d", i)
            if self.seen[e].get(key, 0) >= c:
                return
            self.eng[e].wait_ge(self.dsem[i], c)
            self.seen[e][key] = c
            self.n_wait += 1

    def _deps(self, e, reads, writes):
        for r in reads:
            self._wait_tok(e, self.last_w.get(r))
        for w in writes:
            self._wait_tok(e, self.last_w.get(w))
            for t in self.readers.get(w, ()):
                self._wait_tok(e, t)

    def _commit(self, tok, reads, writes):
        for r in reads:
            self.readers.setdefault(r, []).append(tok)
        for w in writes:
            self.last_w[w] = tok
            self.readers[w] = []

    def op(self, e, fn, reads=(), writes=()):
        self._deps(e, reads, writes)
        ins = fn(self.eng[e])
        ins.then_inc(self.sem[e], 1)
        self.cnt[e] += 1
        self.n_ins += 1
        tok = ("eng", e, self.cnt[e])
        if e == "pe":
            self._flush_pe(tok)
        self._commit(tok, reads, writes)
        return tok

    def _flush_pe(self, tok):
        if self.pe_pending_reads or self.pe_pending_writes:
            self._commit(tok, self.pe_pending_reads, self.pe_pending_writes)
            self.pe_pending_reads = []
            self.pe_pending_writes = []

    def mm(self, fn, reads=(), writes=(), last=False):
        for w in writes:
            if w in self.pe_pending_writes:
                continue
            self._wait_tok("pe", self.last_w.get(w))
            for t in self.readers.get(w, ()):
                self._wait_tok("pe", t)
        for r in reads:
            self._wait_tok("pe", self.last_w.get(r))
        ins = fn(self.eng["pe"])
        self.n_ins += 1
        for r in reads:
            if r not in self.pe_pending_reads:
                self.pe_pending_reads.append(r)
        for w in writes:
            if w not in self.pe_pending_writes:
                self.pe_pending_writes.append(w)
        if last:
            ins.then_inc(self.sem["pe"], 1)
            self.cnt["pe"] += 1
            tok = ("eng", "pe", self.cnt["pe"])
            self._flush_pe(tok)
            return tok
        return None

    def dma(self, q, out, in_, reads=(), writes=(), **kw):
        self._deps(q, reads, writes)
        i = self.dnext
        self.dnext = (self.dnext + 1) % len(self.dsem)
        self._wait_tok(q, ("dma", i, self.dcnt[i])) if self.dcnt[i] else None
        ins = self.eng[q].dma_start(out=out, in_=in_, **kw)
        ins.then_inc(self.dsem[i], 16)
        self.dcnt[i] += 16
        self.n_ins += 1
        tok = ("dma", i, self.dcnt[i])
        self._commit(tok, reads, writes)
        return tok

    def dma_custom(self, q, fn, reads=(), writes=(), inc=16):
        self._deps(q, reads, writes)
        i = self.dnext
        self.dnext = (self.dnext + 1) % len(self.dsem)
        self._wait_tok(q, ("dma", i, self.dcnt[i])) if self.dcnt[i] else None
        ins = fn(self.eng[q])
        ins.then_inc(self.dsem[i], inc)
        self.dcnt[i] += inc
        self.n_ins += 1
        tok = ("dma", i, self.dcnt[i])
        self._commit(tok, reads, writes)
        return tok

    def finish(self, keys):
        for k_ in keys:
            self._wait_tok("sp", self.last_w.get(k_))
        for i, c in enumerate(self.dcnt):
            if c:
                self._wait_tok("sp", ("dma", i, c))


from contextlib import ExitStack
from concourse.bass_utils import run_bass_kernel_spmd

NCORES = 8
T = 2048
NT = 16
D = 2048
HT = 128
TT = T + HT
EPS = 1e-6
NE = 32
CAP = 768
NB = CAP // 128
NSLOT = NE * CAP
SWA = 1.702
SWL = 7.0


def barrier(k):
    toks = []
    for e in k.eng:
        if k.cnt[e]:
            toks.append(("eng", e, k.cnt[e]))
    for i, c in enumerate(k.dcnt):
        if c:
            toks.append(("dma", i, c))
    for e in k.eng:
        for t in toks:
            if t[0] == "eng" and t[1] == e:
                continue
            k._wait_tok(e, t)


class Scope:
    def __init__(self, nc, k):
        self.nc, self.k, self.es = nc, k, ExitStack()

    def sb(self, shape, dt, name):
        Scope.uid += 1
        return self.es.enter_context(self.nc.sbuf_tensor("%s_%d" % (name, Scope.uid), shape, dt))

    def close(self):
        barrier(self.k)
        self.es.close()


Scope.uid = 0


def build(stage=99, dbg_out=False):
    nc = bass.Bass("TRN2", target_bir_lowering=False)
    din = lambda n, s, dt=F32: nc.dram_tensor(n, s, dt, kind="ExternalInput").ap()
    xh = din("xh", [TT, D])
    cT = din("cT", [128, 16])
    hmask = din("hmask", [128, 1])
    invcnt = din("invcnt", [4, T])
    cm = din("cm", [128, 16])
    cf = din("cf", [128, 4])
    b_ada = din("b_ada", [1, 6 * D])
    pool_scaleT = din("pool_scaleT", [128, 16])
    cwT = din("cwT", [128, 48, 4])
    cbT = din("cbT", [128, 48])
    dt_bias = din("dt_bias", [1, 64])
    a_log = din("a_log", [1, 64])
    d_skip = din("d_skip", [1, 64])
    ssd_norm_w = din("ssd_norm_w", [1, 4096])
    if stage > 7:
        b_guT = din("b_guT", [128, NE, 32])
        b_dn = din("b_dn", [NE, D])
        w_router = din("w_router", [D, NE])
        b_router = din("b_router", [1, NE])
        fnw = din("fnw", [1, D])
        ebase_in = din("ebase", [128, NE])
        trash_in = din("trash", [128, 1])
    out = nc.dram_tensor("out", [T, D], F32, kind="ExternalOutput").ap()

    dk = "Internal"
    MODB = nc.dram_tensor("MODB", [128, 6 * D], F32, kind=dk).ap()
    YP = nc.dram_tensor("YP", [D, T], BF16, kind=dk).ap()
    GP = nc.dram_tensor("GP", [D, T], BF16, kind=dk).ap()
    GS = nc.dram_tensor("GS", [D, T], BF16).ap()
    ZT = nc.dram_tensor("ZT", [T, 4096], BF16, kind=dk).ap()
    XTOK = nc.dram_tensor("XTOK", [T, 5120], BF16, kind=dk).ap()
    BCT = nc.dram_tensor("BCT", [2048, T], BF16, kind=dk).ap()
    YS = nc.dram_tensor("YS", [4096, T], BF16, kind=dk).ap()
    H1 = nc.dram_tensor("H1", [T, D], F32).ap()
    XS = nc.dram_tensor("XS", [NSLOT + 128, D], BF16).ap()
    YSLa = nc.dram_tensor("YSLa", [NSLOT + 128, D // 2], F32).ap()
    YSLb = nc.dram_tensor("YSLb", [NSLOT + 128, D // 2], F32).ap()

    k = K(nc)
    P = Scope(nc, k)
    k.cc_sem = nc.alloc_semaphore("cc_sem")
    cc_n = [0]
    RG = [[0, 1, 2, 3], [4, 5, 6, 7]]

    def piece_rows(R, C):
        rq = 1
        while rq * 2 * C * 2 <= (1 << 20) and (R // 4) % (rq * 2) == 0:
            rq *= 2
        return rq

    CW = 1028
    cf32 = [P.sb([128, CW], F32, "cf32_%d" % i) for i in range(2)]
    cbf = [P.sb([128, CW], BF16, "cbf_%d" % i) for i in range(2)]
    cu = [0]

    def cast_weight(name, R, C, lazy=False):
        rq = piece_rows(R, C)
        NP = (R // 4) // rq
        F = rq * C // 128
        nch = -(-F // CW)
        assert F % nch == 0
        fw_ = F // nch
        qin = nc.dram_tensor(name + "_q", [NP * 128, F], F32, kind="ExternalInput")
        bnc = nc.dram_tensor(name + "_b", [NP * 128, F], BF16)
        full = nc.dram_tensor(name + "_f", [R, C], BF16)
        units = [(i, c) for i in range(NP) for c in range(nch)]
        base = cu[0]
        cu[0] += len(units)

        def load(u):
            i, c = units[u]
            bi = (base + u) % 2
            k.dma("pool", cf32[bi][:, 0:fw_], qin[i * 128:(i + 1) * 128, c * fw_:(c + 1) * fw_], writes=["cf32_%d" % bi])

        def unit(u):
            i, c = units[u]
            bi = (base + u) % 2
            if u == 0:
                for u2 in range(min(2, len(units))):
                    load(u2)
            k.op("pool", lambda e: e.tensor_copy(cbf[bi][:, 0:fw_], cf32[bi][:, 0:fw_]), reads=["cf32_%d" % bi], writes=["cbf_%d" % bi])
            k.dma("pool", bnc[i * 128:(i + 1) * 128, c * fw_:(c + 1) * fw_], cbf[bi][:, 0:fw_], reads=["cbf_%d" % bi], writes=["%s_b%d_%d" % (name, i, c)])
            if u + 2 < len(units):
                load(u + 2)

        for u in range(len(units)):
            if lazy:
                lazyq.append(lambda u=u: unit(u))
            else:
                unit(u)
        return dict(name=name, rq=rq, NP=NP, nch=nch, bnc=bnc, full=full)

    lazyq = []

    def pump(n):
        while n > 0 and lazyq:
            lazyq.pop(0)()
            n -= 1

    def gather_weight(cw_):
        toks = []
        name, rq = cw_["name"], cw_["rq"]
        for i in range(cw_["NP"]):
            k._deps("pool", ["%s_b%d_%d" % (name, i, c) for c in range(cw_["nch"])], [])
            nc.gpsimd.collective_compute("AllGather", ALU.bypass, replica_groups=RG,
                                         ins=[cw_["bnc"][i * 128:(i + 1) * 128, :].opt()],
                                         outs=[cw_["full"][i * 4 * rq:(i + 1) * 4 * rq, :].opt()]).then_inc(k.cc_sem)
            cc_n[0] += 1
            toks.append(cc_n[0])
        return cw_["full"].ap(), toks

    def dist_weight(name, R, C):
        return gather_weight(cast_weight(name, R, C))

    PS = [nc.alloc_psum_tensor("ps%d" % i, [128, 512], F32) for i in range(5)]
    PSB = [nc.alloc_psum_tensor("psb%d" % i, [128, 512], BF16) for i in range(2)]
    PSY = nc.alloc_psum_tensor("psy", [128, 512], F32)
    psi = [0]

    def next_ps():
        i = psi[0] % len(PS)
        psi[0] += 1
        return PS[i], "ps%d" % i

    psbi = [0]

    def next_psb():
        i = psbi[0] % len(PSB)
        psbi[0] += 1
        return PSB[i], "psb%d" % i

    identf = P.sb([128, 128], F32, "identf")
    ident = P.sb([128, 128], BF16, "ident")
    tri = P.sb([128, 128], F32, "tri")
    ustr = P.sb([128, 128], BF16, "ustr")
    onesf = P.sb([128, 128], F32, "onesf")
    onesb = P.sb([128, 128], BF16, "onesb")
    ss = P.sb([128, 1], F32, "ss")
    dt_all = P.sb([128, 16, 64], F32, "dt_all")
    k.op("pool", lambda e: e.memset(identf[:], 1.0), writes=["identf"])
    k.op("pool", lambda e: e.affine_select(out=identf[:], in_=identf[:], pattern=[[-1, 128]],
                                           compare_op=ALU.is_equal, fill=0.0, base=0, channel_multiplier=1),
         reads=["identf"], writes=["identf"])
    k.op("dve", lambda e: e.tensor_copy(ident[:], identf[:]), reads=["identf"], writes=["ident"])
    k.op("pool", lambda e: e.memset(tri[:], 1.0), writes=["tri"])
    k.op("pool", lambda e: e.affine_select(out=tri[:], in_=tri[:], pattern=[[1, 128]],
                                           compare_op=ALU.is_ge, fill=0.0, base=0, channel_multiplier=-1),
         reads=["tri"], writes=["tri"])
    k.op("pool", lambda e: e.memset(onesf[:], 1.0), writes=["onesf"])
    k.op("pool", lambda e: e.affine_select(out=onesf[:], in_=onesf[:], pattern=[[1, 128]],
                                           compare_op=ALU.is_gt, fill=0.0, base=0, channel_multiplier=-1),
         reads=["onesf"], writes=["onesf"])
    k.op("dve", lambda e: e.tensor_copy(ustr[:], onesf[:]), reads=["onesf"], writes=["ustr"])
    k.op("pool", lambda e: e.memset(onesf[:], 1.0), reads=["ustr"], writes=["onesf"])
    k.op("dve", lambda e: e.tensor_copy(onesb[:], onesf[:]), reads=["onesf"], writes=["onesb"])

    wslab = [P.sb([128, 16, 512], BF16, "wslab%d" % i) for i in range(2)]
    wsi = [0]

    def load_slab(W2d, col0, ncols, KC, buf=None, key=None, row0=0, cc=None):
        if buf is None:
            i = wsi[0] % 2
            wsi[0] += 1
            buf = wslab[i]
            key = "wslab%d" % i
        src = W2d[row0:row0 + KC * 128, col0:col0 + ncols].rearrange("(kc p) n -> p kc n", p=128)
        keys = []
        if cc is None:
            cc = WT[id(W2d)]
        k._wait_tok("sp", ("cc", cc))
        for k0 in range(0, KC, 4):
            kk = key if k0 == 0 else key + "x%d" % k0
            keys.append(kk)
            k.dma("sp", buf[:, k0:k0 + 4, 0:ncols], src[:, k0:k0 + 4, :], writes=[kk])
        return buf, keys

    def rms_rstd(sq, src, srck, n):
        k.op("act", lambda e: e.activation(out=sq, in_=src, func=AF.Square), reads=[srck], writes=["sq"])
        k.op("dve", lambda e: e.reduce_sum(out=ss[:], in_=sq, axis=AX.X), reads=["sq"], writes=["ss"])
        k.op("act", lambda e: e.activation(out=ss[:], in_=ss[:], func=AF.Sqrt, scale=1.0 / n, bias=EPS), reads=["ss"], writes=["ss"])
        k.op("dve", lambda e: e.reciprocal(ss[:], ss[:]), reads=["ss"], writes=["ss"])

    zs = Scope(nc, k)
    zb = zs.sb([128, D], BF16, "zb")
    zf = zs.sb([128, D], F32, "zf")
    k.op("pool", lambda e: e.memset(zb[:], 0.0), writes=["zb"])
    k.op("pool", lambda e: e.memset(zf[:], 0.0), writes=["zf"])
    for r in range(NSLOT // 128 + 1):
        k.dma("sp", XS[r * 128:(r + 1) * 128, :], zb[:], reads=["zb"], writes=["XSz"])
    k.dma("sp", YSLa[NSLOT:NSLOT + 128, :], zf[:, 0:D // 2], reads=["zf"], writes=["YSLz"])
    k.dma("sp", YSLb[NSLOT:NSLOT + 128, :], zf[:, 0:D // 2], reads=["zf"], writes=["YSLz2"])
    zs.close()
    w_ada, t_ada = dist_weight("w_ada", D, 6 * D)
    w_in, t_in = dist_weight("w_in", D, 16448)
    pool_w2, t_pw = dist_weight("pool_w", 2048, 512)
    w_bp, t_bp = dist_weight("w_bp", D, D)
    w_bs, t_bs = dist_weight("w_bs", 4096, D)
    w_out, t_out = dist_weight("w_out", D, D)
    if stage > 7:
        cw_gu = [cast_weight("w_gu%d" % gq, 8 * D, 4096, lazy=True) for gq in range(4)]
        cw_dn = [cast_weight("w_dn%d" % gq, 8 * D, D, lazy=True) for gq in range(4)]
    WT = {id(w_ada): t_ada[-1], id(w_in): t_in[-1], id(pool_w2): t_pw[-1], id(w_bp): t_bp[-1], id(w_bs): t_bs[-1], id(w_out): t_out[-1]}


    s1 = Scope(nc, k)
    cts = s1.sb([128, 16], F32, "cts")
    k.dma("sp", cts[:], cT, writes=["cts"])
    k.op("act", lambda e: e.activation(out=cts[:], in_=cts[:], func=AF.Silu), reads=["cts"], writes=["cts"])
    lhsc = s1.sb([128, 16, 128], BF16, "lhsc")
    for kc in range(16):
        k.op("dve", lambda e, kc=kc: e.tensor_copy(lhsc[:, kc, :], cts[:, kc:kc + 1].to_broadcast([128, 128])),
             reads=["cts"], writes=["lhsc"])
    bb = [s1.sb([128, 512], F32, "bb%d" % i) for i in range(2)]
    mo = [s1.sb([128, 512], F32, "mo%d" % i) for i in range(2)]
    for s in range(24):
        pump(4)
        buf, wkeys = load_slab(w_ada, s * 512, 512, 16)
        bt = bb[s % 2]
        bk = "bb%d" % (s % 2)
        mt_ = mo[s % 2]
        mk_ = "mo%d" % (s % 2)
        k.dma("sp", bt[:], b_ada[0:1, s * 512:(s + 1) * 512].partition_broadcast(128), writes=[bk])
        ps, pk = next_ps()
        for kc in range(16):
            k.mm(lambda e, kc=kc: e.matmul(ps[:], lhsT=lhsc[:, kc, :], rhs=buf[:, kc, :], start=(kc == 0), stop=(kc == 15)),
                 reads=["lhsc"] + wkeys, writes=[pk], last=(kc == 15))
        addc = 1.0 if (s // 4) in (1, 4) else 0.0
        k.op("dve", lambda e: e.scalar_tensor_tensor(out=mt_[:], in0=ps[:], scalar=addc, in1=bt[:],
                                                     op0=ALU.add, op1=ALU.add), reads=[pk, bk], writes=[mk_])
        k.dma("sp", MODB[:, s * 512:(s + 1) * 512], mt_[:], reads=[mk_], writes=["MODB"])
    s1.close()
    if stage <= 1:
        return nc, k

    s3 = Scope(nc, k)
    uT = s3.sb([128, 16, TT], BF16, "uT")
    s2 = Scope(nc, k)
    sh1 = s2.sb([128, D], F32, "sh1")
    sc1 = s2.sb([128, D], F32, "sc1")
    k.dma("sp", sh1[:], MODB[:, 0:D], reads=["MODB"], writes=["sh1"])
    k.dma("sp", sc1[:], MODB[:, D:2 * D], reads=["MODB"], writes=["sc1"])
    hm = s2.sb([128, 1], F32, "hm")
    k.dma("sp", hm[:], hmask, writes=["hm"])
    xt = [s2.sb([128, D], F32, "xt%d" % i) for i in range(2)]
    sq = s2.sb([128, D], F32, "sq")
    ub = s2.sb([128, D], BF16, "ub")
    for ti in range(17):
        pump(4)
        x_t = xt[ti % 2]
        xk = "xt%d" % (ti % 2)
        k.dma("sp", x_t[:], xh[ti * 128:(ti + 1) * 128, :], writes=[xk])
        rms_rstd(sq[:], x_t[:], xk, D)
        k.op("dve", lambda e: e.scalar_tensor_tensor(out=sq[:], in0=x_t[:], scalar=ss[:, 0:1], in1=sc1[:], op0=ALU.mult, op1=ALU.mult),
             reads=[xk, "ss", "sc1"], writes=["sq"])
        k.op("dve", lambda e: e.tensor_tensor(out=ub[:], in0=sq[:], in1=sh1[:], op=ALU.add), reads=["sq", "sh1"], writes=["ub"])
        if ti == 0:
            k.op("dve", lambda e: e.tensor_scalar(out=ub[:], in0=ub[:], scalar1=hm[:, 0:1], scalar2=None, op0=ALU.mult),
                 reads=["ub", "hm"], writes=["ub"])
        for kq in range(4):
            pb, pbk = next_psb()
            for j in range(4):
                k.op("pe", lambda e, j=j: e.transpose(pb[:, j * 128:(j + 1) * 128], ub[:, (4 * kq + j) * 128:(4 * kq + j + 1) * 128], ident[:]),
                     reads=["ub", "ident"], writes=[pbk])
            k.op("act", lambda e: e.copy(uT[:, 4 * kq:4 * kq + 4, ti * 128:(ti + 1) * 128], pb[:].rearrange("p (j t) -> p j t", j=4)),
                 reads=[pbk], writes=["uT"])
    s2.close()

    TB = [(0, 512), (512, 512), (1024, 512), (1536, 512), (2048, 128)]
    TBO = [(128, 512), (640, 512), (1152, 512), (1664, 512)]
    rowf = [s3.sb([128, TT], F32, "rowf%d" % i) for i in range(2)]
    rfi = [0]

    def proj_fm(buf, wkeys, fc, KC=16, src=None, srck="uT", tbs=TB):
        pump(4)
        i = rfi[0] % 2
        rfi[0] += 1
        rf = rowf[i]
        rk = "rowf%d" % i
        sr = src if src is not None else uT
        for (t0, n) in tbs:
            ps, pk = next_ps()
            for kc in range(KC):
                k.mm(lambda e, kc=kc: e.matmul(ps[:, 0:n], lhsT=buf[:, kc, fc * 128:(fc + 1) * 128], rhs=sr[:, kc, t0:t0 + n],
                                               start=(kc == 0), stop=(kc == KC - 1)),
                     reads=wkeys + [srck], writes=[pk], last=(kc == KC - 1))
            k.op("act", lambda e: e.copy(rf[:, t0:t0 + n], ps[:, 0:n]), reads=[pk], writes=[rk])
        return rf, rk

    cw = s3.sb([128, 48, 4], F32, "cw")
    cb = s3.sb([128, 48], F32, "cb")
    k.dma("sp", cw[:], cwT, writes=["cw"])
    k.dma("sp", cb[:], cbT, writes=["cb"])
    acc = s3.sb([128, T], F32, "acc")
    obf = [s3.sb([128, T], BF16, "obf%d" % i) for i in range(2)]
    obi = [0]

    def next_ob():
        i = obi[0] % 2
        obi[0] += 1
        return obf[i], "obf%d" % i

    stg = s3.sb([128, 16, 128], BF16, "stg")

    for sl in range(12):
        buf, wkeys = load_slab(w_in, 6144 + sl * 512, 512, 16)
        for fc in range(4):
            ch = sl * 4 + fc
            rf, rk = proj_fm(buf, wkeys, fc)
            k.op("act", lambda e: e.activation(out=acc[:], in_=rf[:, 128:TT], func=AF.Identity, scale=cw[:, ch, 3:4], bias=cb[:, ch:ch + 1]),
                 reads=[rk, "cw", "cb"], writes=["acc"])
            for j in (1, 2, 3):
                k.op("dve", lambda e, j=j: e.scalar_tensor_tensor(out=acc[:], in0=rf[:, 128 - j:TT - j], scalar=cw[:, ch, 3 - j:4 - j], in1=acc[:],
                                                                 op0=ALU.mult, op1=ALU.add), reads=[rk, "cw", "acc"], writes=["acc"])
            ob, ok_ = next_ob()
            k.op("act", lambda e: e.activation(out=ob[:], in_=acc[:], func=AF.Silu), reads=["acc"], writes=[ok_])
            if ch >= 32:
                k.dma("sp", BCT[(ch - 32) * 128:(ch - 31) * 128, :], ob[:], reads=[ok_], writes=["BCT"])
            if ch < 40:
                for tq in range(4):
                    pb, pbk = next_psb()
                    for j in range(4):
                        tt = tq * 4 + j
                        k.op("pe", lambda e, j=j, tt=tt: e.transpose(pb[:, j * 128:(j + 1) * 128], ob[:, tt * 128:(tt + 1) * 128], ident[:]),
                             reads=[ok_, "ident"], writes=[pbk])
                    k.op("dve", lambda e: e.tensor_copy(stg[:, tq * 4:tq * 4 + 4, :], pb[:].rearrange("p (j t) -> p j t", j=4)),
                         reads=[pbk], writes=["stg"])
                dst = XTOK[:, ch * 128:(ch + 1) * 128].rearrange("(ti p) c -> p ti c", p=128)
                for tq in range(4):
                    k.dma("sp", dst[:, tq * 4:tq * 4 + 4, :], stg[:, tq * 4:tq * 4 + 4, :], reads=["stg"], writes=["XTOK"])

    dtb = s3.sb([128, 64], F32, "dtb")
    k.dma("sp", dtb[:], dt_bias.partition_broadcast(128), writes=["dtb"])
    buf, wkeys = load_slab(w_in, 12288, 64, 16)
    for ti in range(16):
        ps, pk = next_ps()
        for kc in range(16):
            k.mm(lambda e, kc=kc: e.matmul(ps[:, 0:64], lhsT=uT[:, kc, 128 + ti * 128:256 + ti * 128], rhs=buf[:, kc, 0:64],
                                           start=(kc == 0), stop=(kc == 15)), reads=wkeys + ["uT"], writes=[pk], last=(kc == 15))
        k.op("dve", lambda e: e.tensor_tensor(out=dt_all[:, ti, :], in0=ps[:, 0:64], in1=dtb[:], op=ALU.add), reads=[pk, "dtb"], writes=["dt_all"])
    k.op("act", lambda e: e.activation(out=dt_all[:], in_=dt_all[:], func=AF.Exp), reads=["dt_all"], writes=["dt_all"])
    k.op("act", lambda e: e.activation(out=dt_all[:], in_=dt_all[:], func=AF.Ln, bias=1.0), reads=["dt_all"], writes=["dt_all"])

    zt = [s3.sb([128, 512], BF16, "zt%d" % i) for i in range(2)]
    zi = [0]
    for sl in range(8):
        buf, wkeys = load_slab(w_in, 2048 + sl * 512, 512, 16)
        for ti in range(16):
            ps, pk = next_ps()
            for kc in range(16):
                k.mm(lambda e, kc=kc: e.matmul(ps[:], lhsT=uT[:, kc, 128 + ti * 128:256 + ti * 128], rhs=buf[:, kc, :],
                                               start=(kc == 0), stop=(kc == 15)), reads=wkeys + ["uT"], writes=[pk], last=(kc == 15))
            z_t = zt[zi[0] % 2]
            zk = "zt%d" % (zi[0] % 2)
            zi[0] += 1
            k.op("act", lambda e: e.activation(out=z_t[:], in_=ps[:], func=AF.Silu), reads=[pk], writes=[zk])
            k.dma("sp", ZT[ti * 128:(ti + 1) * 128, sl * 512:(sl + 1) * 512], z_t[:], reads=[zk], writes=["ZT"])

    for gi, (c0, dst) in enumerate(((12352, GP), (14400, GS))):
        for sl in range(4):
            buf, wkeys = load_slab(w_in, c0 + sl * 512, 512, 16)
            for fc in range(4):
                rf, rk = proj_fm(buf, wkeys, fc, tbs=TBO)
                ob, ok_ = next_ob()
                k.op("act", lambda e: e.activation(out=ob[:], in_=rf[:, 128:TT], func=AF.Sigmoid), reads=[rk], writes=[ok_])
                k.dma("sp", dst[(sl * 4 + fc) * 128:(sl * 4 + fc + 1) * 128, :], ob[:], reads=[ok_], writes=["G%d" % gi])

    icn = s3.sb([128, T], F32, "icn")
    diffT = s3.sb([128, 4, T], BF16, "diffT")
    pwa = s3.sb([128, TT], F32, "pwa")
    pwb = s3.sb([128, TT], F32, "pwb")
    pscT = s3.sb([128, 16], F32, "pscT")
    k.dma("sp", pscT[:], pool_scaleT, writes=["pscT"])
    for g in range(4):
        w = (2, 4, 8, 16)[g]
        k.dma("sp", icn[:], invcnt[g:g + 1, :].partition_broadcast(128), writes=["icn"])
        buf, wkeys = load_slab(w_in, g * 512, 512, 16)
        for fc in range(4):
            rf, rk = proj_fm(buf, wkeys, fc)
            cur, curk = rf, rk
            sh = 1
            pp = [(pwa, "pwa"), (pwb, "pwb")]
            pi = 0
            while sh < w:
                nx, nxk = pp[pi % 2]
                pi += 1
                k.op("dve", lambda e, cur=cur, nx=nx, sh=sh: e.tensor_tensor(out=nx[:, 16:TT], in0=cur[:, 16:TT], in1=cur[:, 16 - sh:TT - sh], op=ALU.add),
                     reads=[curk], writes=[nxk])
                cur, curk = nx, nxk
                sh *= 2
            k.op("dve", lambda e, cur=cur: e.tensor_tensor(out=acc[:], in0=cur[:, 128:TT], in1=icn[:], op=ALU.mult), reads=[curk, "icn"], writes=["acc"])
            k.op("dve", lambda e: e.tensor_tensor(out=diffT[:, fc, :], in0=acc[:], in1=rf[:, 128:TT], op=ALU.subtract), reads=["acc", rk], writes=["diffT"])
        pbuf, pkeys = load_slab(pool_w2, 0, 512, 4, row0=g * 512)
        for dc in range(4):
            rf, rk = proj_fm(pbuf, pkeys, dc, KC=4, src=diffT, srck="diffT", tbs=[(a, 512) for a in (0, 512, 1024, 1536)])
            ob, ok_ = next_ob()
            k.op("act", lambda e: e.activation(out=ob[:], in_=rf[:, 0:T], func=AF.Copy, scale=pscT[:, g * 4 + dc:g * 4 + dc + 1]), reads=[rk, "pscT"], writes=[ok_])
            k.dma("sp", YP[(g * 4 + dc) * 128:(g * 4 + dc + 1) * 128, :], ob[:], reads=[ok_], writes=["YP"])
    s3.close()
    if stage <= 3:
        return nc, k

    s4 = Scope(nc, k)
    abc = s4.sb([128, 64], F32, "abc")
    dsk = s4.sb([128, 64], F32, "dsk")
    k.dma("sp", abc[:], a_log.partition_broadcast(128), writes=["abc"])
    k.op("act", lambda e: e.activation(out=abc[:], in_=abc[:], func=AF.Exp), reads=["abc"], writes=["abc"])
    k.op("dve", lambda e: e.tensor_scalar(out=abc[:], in0=abc[:], scalar1=-1.0, scalar2=None, op0=ALU.mult), reads=["abc"], writes=["abc"])
    k.dma("sp", dsk[:], d_skip.partition_broadcast(128), writes=["dsk"])
    nwb = s4.sb([128, 4096], F32, "nwb")
    k.dma("sp", nwb[:], ssd_norm_w.partition_broadcast(128), writes=["nwb"])
    dacs_all = s4.sb([128, 16, 64], F32, "dacs_all")
    tot_all = s4.sb([128, 16, 64], F32, "tot_all")
    S = s4.sb([128, 4096], F32, "S")
    Sbf = s4.sb([128, 4096], BF16, "Sbf")
    dsum = s4.sb([128, 64], F32, "dsum")
    xb_t = [s4.sb([128, 5120], BF16, "xb_t%d" % i) for i in range(2)]
    xdtd = s4.sb([128, 4096], BF16, "xdtd")
    xdt = s4.sb([128, 4096], BF16, "xdt")
    tmp64 = s4.sb([128, 64], F32, "tmp64")
    wend = s4.sb([128, 64], F32, "wend")
    cdec = s4.sb([128, 64], F32, "cdec")
    da = s4.sb([128, 64], F32, "da")
    k.op("pool", lambda e: e.memset(S[:], 0.0), writes=["S"])
    k.op("pool", lambda e: e.memset(dsum[:], 0.0), writes=["dsum"])

    def tile_dt_stuff(ti, x_t, xk, first_pass):
        dtt = dt_all[:, ti, :]
        if first_pass:
            k.op("dve", lambda e: e.tensor_tensor(out=da[:], in0=dtt, in1=abc[:], op=ALU.mult), reads=["dt_all", "abc"], writes=["da"])
            ps, pk = next_ps()
            k.mm(lambda e: e.matmul(ps[:, 0:64], lhsT=tri[:], rhs=da[:], start=True, stop=True), reads=["tri", "da"], writes=[pk], last=True)
            k.op("dve", lambda e: e.tensor_copy(dacs_all[:, ti, :], ps[:, 0:64]), reads=[pk], writes=["dacs_all"])
            ps2, pk2 = next_ps()
            k.mm(lambda e: e.matmul(ps2[:, 0:64], lhsT=onesf[:], rhs=da[:], start=True, stop=True), reads=["onesf", "da"], writes=[pk2], last=True)
            k.op("dve", lambda e: e.tensor_copy(tot_all[:, ti, :], ps2[:, 0:64]), reads=[pk2], writes=["tot_all"])
            k.op("dve", lambda e: e.tensor_tensor(out=dsum[:], in0=dsum[:], in1=tot_all[:, ti, :], op=ALU.add), reads=["dsum", "tot_all"], writes=["dsum"])
        k.op("dve", lambda e: e.tensor_tensor(out=tmp64[:], in0=tot_all[:, ti, :], in1=dacs_all[:, ti, :], op=ALU.subtract),
             reads=["tot_all", "dacs_all"], writes=["tmp64"])
        k.op("act", lambda e: e.activation(out=tmp64[:], in_=tmp64[:], func=AF.Exp), reads=["tmp64"], writes=["tmp64"])
        k.op("dve", lambda e: e.tensor_tensor(out=wend[:], in0=tmp64[:], in1=dtt, op=ALU.mult), reads=["tmp64", "dt_all"], writes=["wend"])
        k.op("act", lambda e: e.activation(out=cdec[:], in_=tot_all[:, ti, :], func=AF.Exp), reads=["tot_all"], writes=["cdec"])
        k.op("dve", lambda e: e.tensor_tensor(out=xdtd[:].rearrange("p (h j) -> p h j", h=64), in0=x_t[:, 0:4096].rearrange("p (h j) -> p h j", h=64),
                                              in1=wend[:].unsqueeze(2).to_broadcast([128, 64, 64]), op=ALU.mult), reads=[xk, "wend"], writes=["xdtd"])

    def state_update(x_t, xk):
        for g in range(8):
            ps, pk = next_ps()
            k.mm(lambda e: e.matmul(ps[:], lhsT=x_t[:, 4096 + 128 * g:4096 + 128 * (g + 1)], rhs=xdtd[:, 512 * g:512 * (g + 1)], start=True, stop=True),
                 reads=[xk, "xdtd"], writes=[pk], last=True)
            Sg = S[:, 512 * g:512 * (g + 1)]
            k.op("dve", lambda e: e.tensor_tensor(out=Sg.rearrange("p (h j) -> p h j", h=8), in0=Sg.rearrange("p (h j) -> p h j", h=8),
                                                  in1=cdec[:, 8 * g:8 * g + 8].unsqueeze(2).to_broadcast([128, 8, 64]), op=ALU.mult),
                 reads=["S", "cdec"], writes=["S"])
            k.op("dve", lambda e: e.tensor_tensor(out=Sg, in0=Sg, in1=ps[:], op=ALU.add), reads=["S", pk], writes=["S"])

    for ti in range(16):
        x_t = xb_t[ti % 2]
        xk = "xb_t%d" % (ti % 2)
        k.dma("sp", x_t[:], XTOK[ti * 128:(ti + 1) * 128, :], writes=[xk])
        pump(8)
        tile_dt_stuff(ti, x_t, xk, True)
        state_update(x_t, xk)
    agi = [nc.dram_tensor("agi%d" % i, [128, 1024], F32) for i in range(4)] + [nc.dram_tensor("agi4", [128, 64], F32)]
    ago = [nc.dram_tensor("ago%d" % i, [512, 1024], F32) for i in range(4)] + [nc.dram_tensor("ago4", [512, 64], F32)]
    for i in range(5):
        srcS = S[:, 1024 * i:1024 * (i + 1)] if i < 4 else dsum[:]
        k.dma("sp", agi[i][:, :], srcS, reads=["S", "dsum"], writes=["agi%d" % i])
        k._deps("pool", ["agi%d" % i], [])
        nc.gpsimd.collective_compute("AllGather", ALU.bypass, replica_groups=RG,
                                     ins=[agi[i].ap().opt()], outs=[ago[i].ap().opt()]).then_inc(k.cc_sem)
        cc_n[0] += 1
    k._wait_tok("pool", ("cc", cc_n[0]))
    k.op("pool", lambda e: e.memset(tmp64[:, 0:1], 0.0), reads=["tmp64"], writes=["ag_out", "tmp64"])
    cms = s4.sb([128, 16], F32, "cms")
    cfs = s4.sb([128, 4], F32, "cfs")
    k.dma("sp", cms[:], cm, writes=["cms"])
    k.dma("sp", cfs[:], cf, writes=["cfs"])
    dsj = s4.sb([128, 4, 64], F32, "dsj")
    for j in range(4):
        k.dma("sp", dsj[:, j, :], ago[4][j * 128:(j + 1) * 128, :], reads=["ag_out"], writes=["dsj%d" % j])
    dsjk = ["dsj%d" % j for j in range(4)]
    k.op("pool", lambda e: e.memset(S[:], 0.0), reads=["S"], writes=["S"])
    Fj = s4.sb([128, 4096], F32, "Fj")
    coef = s4.sb([128, 64], F32, "coef")
    for j in range(3):
        for i4 in range(4):
            k.dma("sp", Fj[:, 1024 * i4:1024 * (i4 + 1)], ago[i4][j * 128:(j + 1) * 128, :], reads=["ag_out"], writes=["Fj"] if i4 == 0 else ["Fjx%d" % i4])
        k.op("dve", lambda e: e.tensor_scalar(out=coef[:], in0=dsj[:, 0, :], scalar1=cms[:, 4 * j:4 * j + 1], scalar2=None, op0=ALU.mult),
             reads=dsjk + ["cms"], writes=["coef"])
        for m in range(1, 4):
            k.op("dve", lambda e, m=m: e.scalar_tensor_tensor(out=coef[:], in0=dsj[:, m, :], scalar=cms[:, 4 * j + m:4 * j + m + 1], in1=coef[:],
                                                             op0=ALU.mult, op1=ALU.add), reads=dsjk + ["cms", "coef"], writes=["coef"])
        k.op("act", lambda e: e.activation(out=coef[:], in_=coef[:], func=AF.Exp), reads=["coef"], writes=["coef"])
        k.op("dve", lambda e: e.tensor_scalar(out=coef[:], in0=coef[:], scalar1=cfs[:, j:j + 1], scalar2=None, op0=ALU.mult), reads=["coef", "cfs"], writes=["coef"])
        k.op("dve", lambda e: e.tensor_tensor(out=Fj[:].rearrange("p (h j) -> p h j", h=64), in0=Fj[:].rearrange("p (h j) -> p h j", h=64),
                                              in1=coef[:].unsqueeze(2).to_broadcast([128, 64, 64]), op=ALU.mult), reads=["Fj", "Fjx1", "Fjx2", "Fjx3", "coef"], writes=["Fj", "Fjx1", "Fjx2", "Fjx3"])
        k.op("dve", lambda e: e.tensor_tensor(out=S[:], in0=S[:], in1=Fj[:], op=ALU.add), reads=["S", "Fj", "Fjx1", "Fjx2", "Fjx3"], writes=["S"])

    bct = [s4.sb([128, 16, 128], BF16, "bct%d" % i) for i in range(2)]
    z_t = [s4.sb([128, 4096], BF16, "z_t%d" % i) for i in range(2)]
    edacs = s4.sb([128, 64], F32, "edacs")
    cbm = s4.sb([128, 128], F32, "cbm")
    Xd = s4.sb([128, 1024], F32, "Xd")
    Lt8 = s4.sb([128, 1024], F32, "Lt8")
    Mt8 = s4.sb([128, 1024], BF16, "Mt8")
    yg = s4.sb([128, 512], F32, "yg")
    yo = s4.sb([128, 512], F32, "yo")
    ybf = s4.sb([128, 512], BF16, "ybf")
    ysq = s4.sb([128, 512], F32, "ysq")
    ystg = s4.sb([128, 4, 128], BF16, "ystg")
    BCTv = BCT.rearrange("(c p) t -> p c t", p=128)
    for ti in range(16):
        x_t = xb_t[ti % 2]
        xk = "xb_t%d" % (ti % 2)
        k.dma("sp", x_t[:], XTOK[ti * 128:(ti + 1) * 128, :], writes=[xk])
        bc = bct[ti % 2]
        bck = "bct%d" % (ti % 2)
        bckeys = []
        for c0 in range(0, 16, 4):
            kk = bck if c0 == 0 else bck + "x%d" % c0
            bckeys.append(kk)
            k.dma("sp", bc[:, c0:c0 + 4, :], BCTv[:, c0:c0 + 4, ti * 128:(ti + 1) * 128], writes=[kk])
        zz = z_t[ti % 2]
        zk = "z_t%d" % (ti % 2)
        k.dma("sp", zz[:], ZT[ti * 128:(ti + 1) * 128, :], writes=[zk])
        tile_dt_stuff(ti, x_t, xk, False)
        k.op("dve", lambda e: e.tensor_tensor(out=xdt[:].rearrange("p (h j) -> p h j", h=64), in0=x_t[:, 0:4096].rearrange("p (h j) -> p h j", h=64),
                                              in1=dt_all[:, ti, :].unsqueeze(2).to_broadcast([128, 64, 64]), op=ALU.mult), reads=[xk, "dt_all"], writes=["xdt"])
        k.op("act", lambda e: e.activation(out=edacs[:], in_=dacs_all[:, ti, :], func=AF.Exp), reads=["dacs_all"], writes=["edacs"])
        k.op("act", lambda e: e.copy(Sbf[:], S[:]), reads=["S"], writes=["Sbf"])
        for g in range(8):
            ps, pk = next_ps()
            k.mm(lambda e: e.matmul(ps[:, 0:128], lhsT=bc[:, g, :], rhs=bc[:, 8 + g, :], start=True, stop=True), reads=bckeys, writes=[pk], last=True)
            k.op("dve", lambda e: e.tensor_tensor(out=cbm[:], in0=ps[:, 0:128], in1=tri[:], op=ALU.mult), reads=[pk, "tri"], writes=["cbm"])
            psy, pyk = PSY, "psy"
            k.op("pool", lambda e: e.tensor_tensor(out=Xd[:].rearrange("p (h l) -> p h l", h=8), in0=identf[:].unsqueeze(1).to_broadcast([128, 8, 128]),
                                                  in1=dacs_all[:, ti, 8 * g:8 * g + 8].unsqueeze(2).to_broadcast([128, 8, 128]), op=ALU.mult),
                 reads=["identf", "dacs_all"], writes=["Xd"])
            for hq in range(2):
                psr, prk = next_ps()
                k.mm(lambda e: e.matmul(psr[:], lhsT=onesf[:], rhs=Xd[:, hq * 512:(hq + 1) * 512], start=True, stop=True), reads=["onesf", "Xd"], writes=[prk], last=True)
                Lh = Lt8[:, hq * 512:(hq + 1) * 512]
                k.op("dve", lambda e: e.tensor_tensor(out=Lh.rearrange("p (h l) -> p h l", h=4), in0=psr[:].rearrange("p (h l) -> p h l", h=4),
                                                      in1=dacs_all[:, ti, 8 * g + 4 * hq:8 * g + 4 * hq + 4].unsqueeze(2).to_broadcast([128, 4, 128]), op=ALU.subtract),
                     reads=[prk, "dacs_all"], writes=["Lt8_%d" % hq])
                k.op("dve", lambda e: e.tensor_scalar(out=Lh, in0=Lh, scalar1=0.0, scalar2=None, op0=ALU.min), reads=["Lt8_%d" % hq], writes=["Lt8_%d" % hq])
                k.op("act", lambda e: e.activation(out=Lh, in_=Lh, func=AF.Exp), reads=["Lt8_%d" % hq], writes=["Lt8_%d" % hq])
                Mh = Mt8[:, hq * 512:(hq + 1) * 512]
                k.op("dve", lambda e: e.tensor_tensor(out=Mh.rearrange("p (h l) -> p h l", h=4), in0=Lh.rearrange("p (h l) -> p h l", h=4),
                                                      in1=cbm[:].unsqueeze(1).to_broadcast([128, 4, 128]), op=ALU.mult),
                     reads=["Lt8_%d" % hq, "cbm"], writes=["Mt8_%d" % hq])
                for h4 in range(4):
                    hh = hq * 4 + h4
                    h = 8 * g + hh
                    k.mm(lambda e, h=h, hh=hh: e.matmul(psy[:, hh * 64:(hh + 1) * 64], lhsT=Mt8[:, hh * 128:(hh + 1) * 128], rhs=xdt[:, h * 64:(h + 1) * 64], start=True, stop=True),
                         reads=["Mt8_%d" % hq, "xdt"], writes=[pyk], last=True)
            pso, pok = next_ps()
            k.mm(lambda e: e.matmul(pso[:], lhsT=bc[:, 8 + g, :], rhs=Sbf[:, 512 * g:512 * (g + 1)], start=True, stop=True), reads=bckeys + ["Sbf"], writes=[pok], last=True)
            k.op("dve", lambda e: e.tensor_tensor(out=yo[:].rearrange("p (h j) -> p h j", h=8), in0=pso[:].rearrange("p (h j) -> p h j", h=8),
                                                  in1=edacs[:, 8 * g:8 * g + 8].unsqueeze(2).to_broadcast([128, 8, 64]), op=ALU.mult), reads=[pok, "edacs"], writes=["yo"])
            k.op("dve", lambda e: e.tensor_tensor(out=yg[:], in0=psy[:], in1=yo[:], op=ALU.add), reads=[pyk, "yo"], writes=["yg"])
            k.op("dve", lambda e: e.tensor_tensor(out=yo[:].rearrange("p (h j) -> p h j", h=8), in0=x_t[:, 512 * g:512 * (g + 1)].rearrange("p (h j) -> p h j", h=8),
                                                  in1=dsk[:, 8 * g:8 * g + 8].unsqueeze(2).to_broadcast([128, 8, 64]), op=ALU.mult), reads=[xk, "dsk", "yg"], writes=["yo"])
            k.op("dve", lambda e: e.tensor_tensor(out=yg[:], in0=yg[:], in1=yo[:], op=ALU.add), reads=["yg", "yo"], writes=["yg"])
            k.op("dve", lambda e: e.tensor_tensor(out=yg[:], in0=yg[:], in1=zz[:, 512 * g:512 * (g + 1)], op=ALU.mult), reads=["yg", zk], writes=["yg"])
            rms_rstd(ysq[:], yg[:], "yg", 512)
            k.op("dve", lambda e: e.scalar_tensor_tensor(out=ybf[:], in0=yg[:], scalar=ss[:, 0:1], in1=nwb[:, 512 * g:512 * (g + 1)], op0=ALU.mult, op1=ALU.mult),
                 reads=["yg", "ss", "nwb"], writes=["ybf"])
            pb, pbk = next_psb()
            for j in range(4):
                k.op("pe", lambda e, j=j: e.transpose(pb[:, j * 128:(j + 1) * 128], ybf[:, j * 128:(j + 1) * 128], ident[:]), reads=["ybf", "ident"], writes=[pbk])
            k.op("act", lambda e: e.copy(ystg[:], pb[:].rearrange("p (j t) -> p j t", j=4)), reads=[pbk], writes=["ystg"])
            k.dma("sp", YS[512 * g:512 * (g + 1), ti * 128:(ti + 1) * 128].rearrange("(j p) t -> p j t", p=128), ystg[:], reads=["ystg"], writes=["YS"])
        state_update(x_t, xk)
    if stage > 7:
        pump(10 ** 9)
        w_gu_p, t_gu, w_dn_p, t_dn = [], [], [], []
        for gq in range(4):
            wv_, tv_ = gather_weight(cw_gu[gq])
            w_gu_p.append(wv_)
            t_gu += tv_
        for gq in range(4):
            wv_, tv_ = gather_weight(cw_dn[gq])
            w_dn_p.append(wv_)
            t_dn += tv_
    s4.close()
    if stage <= 4:
        return nc, k

    s7 = Scope(nc, k)
    g1b = s7.sb([128, D], F32, "g1b")
    k.dma("sp", g1b[:], MODB[:, 2 * D:3 * D], writes=["g1b"])
    YPs = s7.sb([128, 16, 512], BF16, "YPs")
    YSs = s7.sb([128, 32, 512], BF16, "YSs")
    mgT = s7.sb([128, 16, 512], BF16, "mgT")
    wsb = s7.sb([128, 32, 512], BF16, "wsb")
    gpt = [s7.sb([128, 512], BF16, "gpt%d" % i) for i in range(2)]
    gst = [s7.sb([128, 512], BF16, "gst%d" % i) for i in range(2)]
    m1 = s7.sb([128, 512], F32, "m1")
    m2 = s7.sb([128, 512], F32, "m2")
    xres = [s7.sb([128, 512], F32, "xres%d" % i) for i in range(2)]
    hres = [s7.sb([128, 512], F32, "hres%d" % i) for i in range(2)]
    YPv = YP.rearrange("(c p) t -> p c t", p=128)
    YSv = YS.rearrange("(c p) t -> p c t", p=128)
    it7 = [0]
    for qt in range(4):
        t0 = qt * 512
        for c0 in range(0, 16, 4):
            k.dma("sp", YPs[:, c0:c0 + 4, :], YPv[:, c0:c0 + 4, t0:t0 + 512], writes=["YPs%d" % c0])
        for c0 in range(0, 32, 4):
            k.dma("sp", YSs[:, c0:c0 + 4, :], YSv[:, c0:c0 + 4, t0:t0 + 512], writes=["YSs%d" % c0])
        ypk = ["YPs%d" % c0 for c0 in range(0, 16, 4)]
        ysk = ["YSs%d" % c0 for c0 in range(0, 32, 4)]
        for fs in range(4):
            wp, wpk = load_slab(w_bp, fs * 512, 512, 16)
            wsk = []
            _, k1 = load_slab(w_bs, fs * 512, 512, 16, buf=wsb, key="wsb")
            wsk += k1
            src2 = w_bs[2048:4096, fs * 512:(fs + 1) * 512].rearrange("(kc p) n -> p kc n", p=128)
            for k0 in range(0, 16, 4):
                kk = "wsbh%d" % k0
                wsk.append(kk)
                k.dma("sp", wsb[:, 16 + k0:16 + k0 + 4, :], src2[:, k0:k0 + 4, :], writes=[kk])
            for fc in range(4):
                i = it7[0] % 2
                it7[0] += 1
                fr = (fs * 4 + fc) * 128
                k.dma("sp", gpt[i][:], GP[fr:fr + 128, t0:t0 + 512], writes=["gpt%d" % i])
                k.dma("sp", gst[i][:], GS[fr:fr + 128, t0:t0 + 512], writes=["gst%d" % i])
                psA, pak = next_ps()
                for kc in range(16):
                    k.mm(lambda e, kc=kc: e.matmul(psA[:], lhsT=wp[:, kc, fc * 128:(fc + 1) * 128], rhs=YPs[:, kc, :], start=(kc == 0), stop=(kc == 15)),
                         reads=wpk + ypk, writes=[pak], last=(kc == 15))
                psB, pbk_ = next_ps()
                for kc in range(32):
                    k.mm(lambda e, kc=kc: e.matmul(psB[:], lhsT=wsb[:, kc, fc * 128:(fc + 1) * 128], rhs=YSs[:, kc, :], start=(kc == 0), stop=(kc == 31)),
                         reads=wsk + ysk, writes=[pbk_], last=(kc == 31))
                k.op("dve", lambda e: e.tensor_tensor(out=m1[:], in0=psA[:], in1=gpt[i][:], op=ALU.mult), reads=[pak, "gpt%d" % i], writes=["m1"])
                k.op("dve", lambda e: e.tensor_tensor(out=m2[:], in0=psB[:], in1=gst[i][:], op=ALU.mult), reads=[pbk_, "gst%d" % i], writes=["m2"])
                k.op("dve", lambda e: e.tensor_tensor(out=mgT[:, fs * 4 + fc, :], in0=m1[:], in1=m2[:], op=ALU.add), reads=["m1", "m2"], writes=["mgT"])
        for fs in range(4):
            wo, wok = load_slab(w_out, fs * 512, 512, 16)
            for tt in range(4):
                i = it7[0] % 2
                it7[0] += 1
                tok0 = t0 + tt * 128
                k.dma("sp", xres[i][:], xh[128 + tok0:128 + tok0 + 128, fs * 512:(fs + 1) * 512], writes=["xres%d" % i])
                ps, pk = next_ps()
                for kc in range(16):
                    k.mm(lambda e, kc=kc: e.matmul(ps[:], lhsT=mgT[:, kc, tt * 128:(tt + 1) * 128], rhs=wo[:, kc, :], start=(kc == 0), stop=(kc == 15)),
                         reads=wok + ["mgT"], writes=[pk], last=(kc == 15))
                k.op("dve", lambda e: e.tensor_tensor(out=hres[i][:], in0=ps[:], in1=g1b[:, fs * 512:(fs + 1) * 512], op=ALU.mult), reads=[pk, "g1b"], writes=["hres%d" % i])
                k.op("dve", lambda e: e.tensor_tensor(out=hres[i][:], in0=hres[i][:], in1=xres[i][:], op=ALU.add), reads=["hres%d" % i, "xres%d" % i], writes=["hres%d" % i])
                k.dma("sp", H1[tok0:tok0 + 128, fs * 512:(fs + 1) * 512], hres[i][:], reads=["hres%d" % i], writes=["H1"])
                if dbg_out:
                    k.dma("sp", out[tok0:tok0 + 128, fs * 512:(fs + 1) * 512], hres[i][:], reads=["hres%d" % i], writes=["out"])
    s7.close()
    if stage <= 7:
        k.finish([])
        return nc, k

    s8 = Scope(nc, k)
    slot_i = s8.sb([128, 16, 4], I32, "slot_i")
    gate_k = s8.sb([128, 16, 4], F32, "gate_k")
    cntacc = s8.sb([128, NE], F32, "cntacc")
    ebase = s8.sb([128, NE], F32, "ebase")
    trash = s8.sb([128, 1], F32, "trash")
    brb = s8.sb([128, NE], F32, "brb")
    wr32 = s8.sb([128, 16, NE], F32, "wr32")
    whi = s8.sb([128, 16, NE], BF16, "whi")
    wlo = s8.sb([128, 16, NE], BF16, "wlo")
    k.dma("sp", ebase[:], ebase_in, writes=["ebase"])
    k.dma("sp", trash[:], trash_in, writes=["trash"])
    k.dma("sp", brb[:], b_router.partition_broadcast(128), writes=["brb"])
    k.dma("sp", wr32[:], w_router.rearrange("(kc p) e -> p kc e", p=128), writes=["wr32"])
    k.op("dve", lambda e: e.tensor_copy(whi[:], wr32[:]), reads=["wr32"], writes=["whi"])
    k.op("dve", lambda e: e.tensor_tensor(out=wr32[:], in0=wr32[:], in1=whi[:], op=ALU.subtract), reads=["wr32", "whi"], writes=["wr32"])
    k.op("dve", lambda e: e.tensor_copy(wlo[:], wr32[:]), reads=["wr32"], writes=["wlo"])
    k.op("pool", lambda e: e.memset(cntacc[:], 0.0), writes=["cntacc"])

    sA = Scope(nc, k)
    sh2 = sA.sb([128, D], F32, "sh2")
    sc2 = sA.sb([128, D], F32, "sc2")
    k.dma("sp", sh2[:], MODB[:, 3 * D:4 * D], writes=["sh2"])
    k.dma("sp", sc2[:], MODB[:, 4 * D:5 * D], writes=["sc2"])
    h1t = [sA.sb([128, D], F32, "h1t%d" % i) for i in range(2)]
    sqA = sA.sb([128, D], F32, "sqA")
    u2f = sA.sb([128, D], F32, "u2f")
    u2b = [sA.sb([128, D], BF16, "u2b%d" % i) for i in range(2)]
    ulo = sA.sb([128, D], BF16, "ulo")
    uTh = sA.sb([128, 16, 128], BF16, "uTh")
    uTl = sA.sb([128, 16, 128], BF16, "uTl")
    lg = sA.sb([128, NE], F32, "lg")
    m8 = sA.sb([128, 8], F32, "m8")
    mask = sA.sb([128, NE], F32, "mask")
    maskb = sA.sb([128, NE], BF16, "maskb")
    eg = sA.sb([128, NE], F32, "eg")
    rank = sA.sb([128, NE], F32, "rank")
    valid = sA.sb([128, NE], F32, "valid")
    inval = sA.sb([128, NE], F32, "inval")
    slotf = sA.sb([128, NE], F32, "slotf")
    gatev = sA.sb([128, NE], F32, "gatev")
    oh = sA.sb([128, NE], F32, "oh")
    t1 = sA.sb([128, NE], F32, "t1")
    sm1 = sA.sb([128, 1], F32, "sm1")
    negm = sA.sb([128, 1], F32, "negm")
    for ti in range(16):
        h_t = h1t[ti % 2]
        hk = "h1t%d" % (ti % 2)
        ub2 = u2b[ti % 2]
        ubk = "u2b%d" % (ti % 2)
        k.dma("sp", h_t[:], H1[ti * 128:(ti + 1) * 128, :], writes=[hk])
        rms_rstd(sqA[:], h_t[:], hk, D)
        k.op("dve", lambda e: e.scalar_tensor_tensor(out=u2f[:], in0=h_t[:], scalar=ss[:, 0:1], in1=sc2[:], op0=ALU.mult, op1=ALU.mult),
             reads=[hk, "ss", "sc2"], writes=["u2f"])
        k.op("dve", lambda e: e.tensor_tensor(out=u2f[:], in0=u2f[:], in1=sh2[:], op=ALU.add), reads=["u2f", "sh2"], writes=["u2f"])
        k.op("act", lambda e: e.copy(ub2[:], u2f[:]), reads=["u2f"], writes=[ubk])
        k.op("dve", lambda e: e.tensor_tensor(out=ulo[:], in0=u2f[:], in1=ub2[:], op=ALU.subtract), reads=["u2f", ubk], writes=["ulo"])
        for (srcb, srck, dstT, dstk) in ((ub2, ubk, uTh, "uTh"), (ulo, "ulo", uTl, "uTl")):
            for kq in range(4):
                pb, pbk = next_psb()
                for j in range(4):
                    k.op("pe", lambda e, j=j, srcb=srcb: e.transpose(pb[:, j * 128:(j + 1) * 128], srcb[:, (4 * kq + j) * 128:(4 * kq + j + 1) * 128], ident[:]),
                         reads=[srck, "ident"], writes=[pbk])
                k.op("act", lambda e, dstT=dstT: e.copy(dstT[:, 4 * kq:4 * kq + 4, :], pb[:].rearrange("p (j t) -> p j t", j=4)), reads=[pbk], writes=[dstk])
        ps, pk = next_ps()
        n_mm = 0
        for (aT, ak, wv, wk) in ((uTh, "uTh", whi, "whi"), (uTh, "uTh", wlo, "wlo"), (uTl, "uTl", whi, "whi")):
            for kc in range(16):
                n_mm += 1
                k.mm(lambda e, kc=kc, aT=aT, wv=wv, n_mm=n_mm: e.matmul(ps[:, 0:NE], lhsT=aT[:, kc, :], rhs=wv[:, kc, :], start=(n_mm == 1), stop=(n_mm == 48)),
                     reads=[ak, wk], writes=[pk], last=(n_mm == 48))
        k.op("dve", lambda e: e.tensor_tensor(out=lg[:], in0=ps[:, 0:NE], in1=brb[:], op=ALU.add), reads=[pk, "brb"], writes=["lg"])
        k.op("dve", lambda e: e.max(m8[:], lg[:]), reads=["lg"], writes=["m8"])
        k.op("dve", lambda e: e.tensor_scalar(out=mask[:], in0=lg[:], scalar1=m8[:, 3:4], scalar2=None, op0=ALU.is_ge), reads=["lg", "m8"], writes=["mask"])
        k.op("dve", lambda e: e.tensor_scalar(out=negm[:], in0=m8[:, 0:1], scalar1=-1.0, scalar2=None, op0=ALU.mult), reads=["m8"], writes=["negm"])
        k.op("act", lambda e: e.activation(out=eg[:], in_=lg[:], func=AF.Exp, bias=negm[:, 0:1], scale=1.0), reads=["lg", "negm"], writes=["eg"])
        k.op("dve", lambda e: e.tensor_tensor(out=eg[:], in0=eg[:], in1=mask[:], op=ALU.mult), reads=["eg", "mask"], writes=["eg"])
        k.op("dve", lambda e: e.reduce_sum(out=sm1[:], in_=eg[:], axis=AX.X), reads=["eg"], writes=["sm1"])
        k.op("dve", lambda e: e.reciprocal(sm1[:], sm1[:]), reads=["sm1"], writes=["sm1"])
        k.op("dve", lambda e: e.tensor_copy(maskb[:], mask[:]), reads=["mask"], writes=["maskb"])
        psr, prk = next_ps()
        k.mm(lambda e: e.matmul(psr[:, 0:NE], lhsT=ustr[:], rhs=maskb[:], start=True, stop=True), reads=["ustr", "maskb"], writes=[prk], last=True)
        k.op("dve", lambda e: e.tensor_tensor(out=rank[:], in0=psr[:, 0:NE], in1=cntacc[:], op=ALU.add), reads=[prk, "cntacc"], writes=["rank"])
        psc, pck = next_ps()
        k.mm(lambda e: e.matmul(psc[:, 0:NE], lhsT=onesb[:], rhs=maskb[:], start=True, stop=True), reads=["onesb", "maskb"], writes=[pck], last=True)
        k.op("dve", lambda e: e.tensor_tensor(out=cntacc[:], in0=cntacc[:], in1=psc[:, 0:NE], op=ALU.add), reads=[pck, "cntacc", "rank"], writes=["cntacc"])
        k.op("dve", lambda e: e.tensor_scalar(out=valid[:], in0=rank[:], scalar1=float(CAP), scalar2=None, op0=ALU.is_lt), reads=["rank"], writes=["valid"])
        k.op("dve", lambda e: e.tensor_tensor(out=valid[:], in0=valid[:], in1=mask[:], op=ALU.mult), reads=["valid", "mask"], writes=["valid"])
        k.op("dve", lambda e: e.tensor_scalar(out=inval[:], in0=valid[:], scalar1=-1.0, scalar2=1.0, op0=ALU.mult, op1=ALU.add), reads=["valid"], writes=["inval"])
        k.op("dve", lambda e: e.tensor_tensor(out=slotf[:], in0=rank[:], in1=ebase[:], op=ALU.add), reads=["rank", "ebase"], writes=["slotf"])
        k.op("dve", lambda e: e.tensor_tensor(out=slotf[:], in0=slotf[:], in1=valid[:], op=ALU.mult), reads=["slotf", "valid"], writes=["slotf"])
        k.op("dve", lambda e: e.scalar_tensor_tensor(out=slotf[:], in0=inval[:], scalar=trash[:, 0:1], in1=slotf[:], op0=ALU.mult, op1=ALU.add),
             reads=["inval", "trash", "slotf"], writes=["slotf"])
        k.op("dve", lambda e: e.tensor_tensor(out=gatev[:], in0=eg[:], in1=valid[:], op=ALU.mult), reads=["eg", "valid"], writes=["gatev"])
        k.op("dve", lambda e: e.tensor_scalar(out=gatev[:], in0=gatev[:], scalar1=sm1[:, 0:1], scalar2=None, op0=ALU.mult), reads=["gatev", "sm1"], writes=["gatev"])
        for kk in range(4):
            k.op("dve", lambda e, kk=kk: e.tensor_scalar(out=oh[:], in0=lg[:], scalar1=m8[:, kk:kk + 1], scalar2=None, op0=ALU.is_equal), reads=["lg", "m8"], writes=["oh"])
            k.op("dve", lambda e: e.tensor_tensor(out=t1[:], in0=oh[:], in1=slotf[:], op=ALU.mult), reads=["oh", "slotf"], writes=["t1"])
            k.op("dve", lambda e: e.reduce_sum(out=sm1[:], in_=t1[:], axis=AX.X), reads=["t1", "gatev"], writes=["sm1"])
            k.op("dve", lambda e, kk=kk: e.tensor_copy(slot_i[:, ti, kk:kk + 1], sm1[:]), reads=["sm1"], writes=["slot_i"])
            k.op("dve", lambda e: e.tensor_tensor(out=t1[:], in0=oh[:], in1=gatev[:], op=ALU.mult), reads=["oh", "gatev"], writes=["t1"])
            k.op("dve", lambda e, kk=kk: e.reduce_sum(out=gate_k[:, ti, kk:kk + 1], in_=t1[:], axis=AX.X), reads=["t1"], writes=["gate_k"])
            k.dma_custom("pool", lambda e, kk=kk: e.indirect_dma_start(
                out=XS[:, :], out_offset=bass.IndirectOffsetOnAxis(ap=slot_i[:, ti, kk:kk + 1], axis=0),
                in_=ub2[:, :], in_offset=None), reads=[ubk, "slot_i"], writes=["XS"])
    sA.close()

    sB = Scope(nc, k)
    Xg = sB.sb([128, NB, D], BF16, "Xg")
    XeT = sB.sb([128, 16, CAP], BF16, "XeT")
    actT = sB.sb([128, 16, CAP], BF16, "actT")
    bgu = sB.sb([128, NE, 32], F32, "bgu")
    k.dma("sp", bgu[:], b_guT, writes=["bgu"])
    gsb = sB.sb([128, 512], F32, "gsb")
    usb = sB.sb([128, 512], F32, "usb")
    sgb = sB.sb([128, 512], F32, "sgb")
    bdn_b = [sB.sb([128, 512], F32, "bdn%d" % i) for i in range(2)]
    yout = [sB.sb([128, 512], F32, "yout%d" % i) for i in range(2)]
    yi = [0]
    ppe_gu = D // (4 * piece_rows(8 * D, 4096))
    ppe_dn = D // (4 * piece_rows(8 * D, D))
    for ex in range(NE):
        XSv = XS[ex * CAP:(ex + 1) * CAP, :].rearrange("(b p) d -> p b d", p=128)
        for bq in range(NB):
            k.dma("sp", Xg[:, bq, :], XSv[:, bq, :], writes=["Xg%d" % bq])
        for blk in range(NB):
            for kq in range(4):
                pb, pbk = next_psb()
                for j in range(4):
                    k.op("pe", lambda e, j=j: e.transpose(pb[:, j * 128:(j + 1) * 128], Xg[:, blk, (4 * kq + j) * 128:(4 * kq + j + 1) * 128], ident[:]),
                         reads=["Xg%d" % blk, "ident"], writes=[pbk])
                k.op("act", lambda e: e.copy(XeT[:, 4 * kq:4 * kq + 4, blk * 128:(blk + 1) * 128], pb[:].rearrange("p (j t) -> p j t", j=4)),
                     reads=[pbk], writes=["XeT"])
        for s in range(4):
            wg, wgk = load_slab(w_gu_p[ex // 8], s * 512, 512, 16, row0=(ex % 8) * D, cc=t_gu[(ex + 1) * ppe_gu - 1])
            wu, wuk = load_slab(w_gu_p[ex // 8], 2048 + s * 512, 512, 16, row0=(ex % 8) * D, cc=t_gu[(ex + 1) * ppe_gu - 1])
            for fc in range(4):
                f = s * 4 + fc
                for (c0, cn) in ((0, 512), (512, CAP - 512)):
                    psG, pgk = next_ps()
                    for kc in range(16):
                        k.mm(lambda e, kc=kc: e.matmul(psG[:, 0:cn], lhsT=wg[:, kc, fc * 128:(fc + 1) * 128], rhs=XeT[:, kc, c0:c0 + cn], start=(kc == 0), stop=(kc == 15)),
                             reads=wgk + ["XeT"], writes=[pgk], last=(kc == 15))
                    psU, puk = next_ps()
                    for kc in range(16):
                        k.mm(lambda e, kc=kc: e.matmul(psU[:, 0:cn], lhsT=wu[:, kc, fc * 128:(fc + 1) * 128], rhs=XeT[:, kc, c0:c0 + cn], start=(kc == 0), stop=(kc == 15)),
                             reads=wuk + ["XeT"], writes=[puk], last=(kc == 15))
                    k.op("dve", lambda e: e.tensor_scalar(out=gsb[:, 0:cn], in0=psG[:, 0:cn], scalar1=bgu[:, ex, f:f + 1], scalar2=SWL, op0=ALU.add, op1=ALU.min),
                         reads=[pgk, "bgu"], writes=["gsb"])
                    k.op("act", lambda e: e.activation(out=sgb[:, 0:cn], in_=gsb[:, 0:cn], func=AF.Sigmoid, scale=SWA), reads=["gsb"], writes=["sgb"])
                    k.op("dve", lambda e: e.tensor_scalar(out=usb[:, 0:cn], in0=psU[:, 0:cn], scalar1=bgu[:, ex, 16 + f:17 + f], scalar2=SWL, op0=ALU.add, op1=ALU.min),
                         reads=[puk, "bgu"], writes=["usb"])
                    k.op("dve", lambda e: e.tensor_scalar(out=usb[:, 0:cn], in0=usb[:, 0:cn], scalar1=-SWL, scalar2=1.0, op0=ALU.max, op1=ALU.add), reads=["usb"], writes=["usb"])
                    k.op("dve", lambda e: e.tensor_tensor(out=gsb[:, 0:cn], in0=gsb[:, 0:cn], in1=sgb[:, 0:cn], op=ALU.mult), reads=["gsb", "sgb"], writes=["gsb"])
                    k.op("dve", lambda e: e.tensor_tensor(out=actT[:, f, c0:c0 + cn], in0=gsb[:, 0:cn], in1=usb[:, 0:cn], op=ALU.mult), reads=["gsb", "usb"], writes=["actT"])
        for ds in range(4):
            wd, wdk = load_slab(w_dn_p[ex // 8], ds * 512, 512, 16, row0=(ex % 8) * D, cc=t_dn[(ex + 1) * ppe_dn - 1])
            bd_ = bdn_b[ds % 2]
            bdk = "bdn%d" % (ds % 2)
            k.dma("sp", bd_[:], b_dn[ex:ex + 1, ds * 512:(ds + 1) * 512].partition_broadcast(128), writes=[bdk])
            for blk in range(NB):
                ps, pk = next_ps()
                for kc in range(16):
                    k.mm(lambda e, kc=kc: e.matmul(ps[:], lhsT=actT[:, kc, blk * 128:(blk + 1) * 128], rhs=wd[:, kc, :], start=(kc == 0), stop=(kc == 15)),
                         reads=wdk + ["actT"], writes=[pk], last=(kc == 15))
                yo_ = yout[yi[0] % 2]
                yk_ = "yout%d" % (yi[0] % 2)
                yi[0] += 1
                k.op("dve", lambda e: e.tensor_tensor(out=yo_[:], in0=ps[:], in1=bd_[:], op=ALU.add), reads=[pk, bdk], writes=[yk_])
                r0 = ex * CAP + blk * 128
                Yd = YSLa if ds < 2 else YSLb
                k.dma("sp", Yd[r0:r0 + 128, (ds % 2) * 512:(ds % 2 + 1) * 512], yo_[:], reads=[yk_], writes=["YSL"])
    sB.close()

    sC = Scope(nc, k)
    g2b = sC.sb([128, D], F32, "g2b")
    fnwb = sC.sb([128, D], F32, "fnwb")
    k.dma("sp", g2b[:], MODB[:, 5 * D:6 * D], writes=["g2b"])
    k.dma("sp", fnwb[:], fnw.partition_broadcast(128), writes=["fnwb"])
    h1c = [sC.sb([128, D], F32, "h1c%d" % i) for i in range(2)]
    Yg = [sC.sb([128, D], F32, "Yg%d" % i) for i in range(2)]
    accm = sC.sb([128, D], F32, "accm")
    sqC = sC.sb([128, D], F32, "sqC")
    outt = sC.sb([128, D], F32, "outt")
    gi_ = [0]
    for ti in range(16):
        h_t = h1c[ti % 2]
        hk = "h1c%d" % (ti % 2)
        k.dma("sp", h_t[:], H1[ti * 128:(ti + 1) * 128, :], writes=[hk])
        for kk in range(4):
            yg_ = Yg[gi_[0] % 2]
            ygk = "Yg%d" % (gi_[0] % 2)
            gi_[0] += 1
            k.dma_custom("pool", lambda e, kk=kk, yg_=yg_: e.indirect_dma_start(
                out=yg_[:, 0:D // 2], out_offset=None, in_=YSLa[:, :],
                in_offset=bass.IndirectOffsetOnAxis(ap=slot_i[:, ti, kk:kk + 1], axis=0)), reads=["slot_i"], writes=[ygk])
            k.dma_custom("pool", lambda e, kk=kk, yg_=yg_: e.indirect_dma_start(
                out=yg_[:, D // 2:D], out_offset=None, in_=YSLb[:, :],
                in_offset=bass.IndirectOffsetOnAxis(ap=slot_i[:, ti, kk:kk + 1], axis=0)), reads=["slot_i"], writes=[ygk + "b"])
            if kk == 0:
                k.op("dve", lambda e, yg_=yg_: e.tensor_scalar(out=accm[:], in0=yg_[:], scalar1=gate_k[:, ti, 0:1], scalar2=None, op0=ALU.mult),
                     reads=[ygk, ygk + "b", "gate_k"], writes=["accm"])
            else:
                k.op("dve", lambda e, kk=kk, yg_=yg_: e.scalar_tensor_tensor(out=accm[:], in0=yg_[:], scalar=gate_k[:, ti, kk:kk + 1], in1=accm[:],
                                                                             op0=ALU.mult, op1=ALU.add), reads=[ygk, ygk + "b", "gate_k", "accm"], writes=["accm"])
        k.op("dve", lambda e: e.tensor_tensor(out=accm[:], in0=accm[:], in1=g2b[:], op=ALU.mult), reads=["accm", "g2b"], writes=["accm"])
        k.op("dve", lambda e: e.tensor_tensor(out=accm[:], in0=accm[:], in1=h_t[:], op=ALU.add), reads=["accm", hk], writes=["accm"])
        rms_rstd(sqC[:], accm[:], "accm", D)
        k.op("dve", lambda e: e.scalar_tensor_tensor(out=outt[:], in0=accm[:], scalar=ss[:, 0:1], in1=fnwb[:], op0=ALU.mult, op1=ALU.mult),
             reads=["accm", "ss", "fnwb"], writes=["outt"])
        k.dma("sp", out[ti * 128:(ti + 1) * 128, :], outt[:], reads=["outt"], writes=["out"])
    sC.close()
    s8.close()
    k.finish([])
    return nc, k


def host_inputs(inputs, full=True):
    f = lambda a: np.ascontiguousarray(np.asarray(a, dtype=np.float32))
    x = f(inputs["x"])
    c = f(inputs["c"])
    def quarters(W2d, pr=None):
        R, C = W2d.shape
        rq = 1
        while rq * 2 * C * 2 <= (1 << 20) and (R // 4) % (rq * 2) == 0:
            rq *= 2
        pr = 4 * rq
        Wr = W2d.reshape(R // pr, 4, pr // 4, C)
        NP = (R // 4) // rq
        F = rq * C // 128
        return [np.ascontiguousarray(Wr[:, q].reshape(NP * 128, F)) for q in range(4)]

    big = {
        "w_ada_q": quarters(f(inputs["w_ada"][0]), 512), "w_in_q": quarters(f(inputs["w_in"][0]), 512),
        "pool_w_q": quarters(f(inputs["pool_w"][0]).reshape(2048, 512), 2048),
        "w_bp_q": quarters(f(inputs["w_branch_pool"][0]), 2048), "w_bs_q": quarters(f(inputs["w_branch_ssd"][0]), 4096),
        "w_out_q": quarters(f(inputs["w_out"][0]), 2048),
    }
    if full:
        wgu = np.asarray(inputs["w_gate_up"][0], dtype=np.float32)
        wdn = np.asarray(inputs["w_down"][0], dtype=np.float32)
        for gq in range(4):
            big["w_gu%d_q" % gq] = quarters(wgu[8 * gq:8 * gq + 8].reshape(8 * D, 4096))
            big["w_dn%d_q" % gq] = quarters(wdn[8 * gq:8 * gq + 8].reshape(8 * D, D))
    shared = {
        "b_ada": f(inputs["b_ada"][0]).reshape(1, -1),
        "pool_scaleT": f(np.asarray(inputs["pool_scale"][0]).reshape(16, 128).T),
        "cwT": f(np.asarray(inputs["conv_w"][0]).reshape(4, 48, 128).transpose(2, 1, 0)),
        "cbT": f(np.asarray(inputs["conv_b"][0]).reshape(48, 128).T),
        "dt_bias": f(inputs["dt_bias"][0]).reshape(1, 64), "a_log": f(inputs["a_log"][0]).reshape(1, 64),
        "d_skip": f(inputs["d_skip"][0]).reshape(1, 64), "ssd_norm_w": f(inputs["ssd_norm_w"][0]).reshape(1, 4096),
        "w_router": f(inputs["w_router"][0]), "b_router": f(inputs["b_router"][0]).reshape(1, NE),
        "b_guT": f(np.asarray(inputs["b_gate_up"][0]).reshape(NE, 32, 128).transpose(2, 0, 1)),
        "b_dn": f(inputs["b_down"][0]),
        "fnw": f(inputs["final_norm_w"]).reshape(1, D),
        "ebase": f(np.broadcast_to((np.arange(NE) * CAP)[None, :], (128, NE))),
        "trash": f((NSLOT + np.arange(128)).reshape(128, 1)),
    }
    maps = []
    for cid in range(NCORES):
        b, q = cid // 4, cid % 4
        xh = np.zeros((TT, D), np.float32)
        xh[HT:] = x[b, q * T:(q + 1) * T]
        if q > 0:
            xh[:HT] = x[b, q * T - HT:q * T]
        pos = np.arange(q * T + 1, (q + 1) * T + 1, dtype=np.float32)
        invcnt = np.stack([1.0 / np.minimum(pos, float(w)) for w in (2, 4, 8, 16)]).astype(np.float32)
        cm = np.zeros((128, 16), np.float32)
        cfv = np.zeros((128, 4), np.float32)
        for j in range(4):
            cfv[:, j] = 1.0 if j < q else 0.0
            for m in range(4):
                cm[:, 4 * j + m] = 1.0 if (j < m < q) else 0.0
        d = dict(shared)
        for kk, v in big.items():
            d[kk] = v[q]
        d.update({
            "xh": xh, "cT": f(c[b].reshape(16, 128).T), "hmask": np.full((128, 1), 1.0 if q > 0 else 0.0, np.float32),
            "invcnt": invcnt, "cm": cm, "cf": cfv,
        })
        maps.append(d)
    return maps


_CACHE = {}


def kernel(**inputs):
    maps = host_inputs(inputs)
    if "nc" not in _CACHE:
        _CACHE["nc"] = build()[0]
    res = run_bass_kernel_spmd(_CACHE["nc"], maps, core_ids=list(range(NCORES)))
    outs = [np.asarray(r["out"]) for r in res.results]
    o = np.stack(outs).reshape(2, 4 * T, D).astype(np.float32)
    return o
```

```python
import numpy as np
import concourse.bass as bass
import concourse.mybir as mybir

F32 = mybir.dt.float32
BF16 = mybir.dt.bfloat16
I32 = mybir.dt.int32
U32 = mybir.dt.uint32
AF = mybir.ActivationFunctionType
ALU = mybir.AluOpType
AX = mybir.AxisListType


class K:
    def __init__(self, nc, n_dma_sems=72, same_engine_sync=True):
        self.nc = nc
        self.eng = {"pe": nc.tensor, "act": nc.scalar, "dve": nc.vector, "pool": nc.gpsimd, "sp": nc.sync}
        self.sem = {e: nc.alloc_semaphore("c_" + e) for e in self.eng}
        self.cnt = {e: 0 for e in self.eng}
        self.seen = {e: {} for e in self.eng}
        self.dsem = [nc.alloc_semaphore("d%d" % i) for i in range(n_dma_sems)]
        self.dcnt = [0] * n_dma_sems
        self.dnext = 0
        self.last_w = {}
        self.readers = {}
        self.pe_pending_reads = []
        self.pe_pending_writes = []
        self.same_engine_sync = same_engine_sync
        self.n_wait = 0
        self.n_ins = 0

    def _wait_tok(self, e, tok):
        if tok is None:
            return
        kind = tok[0]
        if kind == "eng":
            _, e2, c = tok
            if e2 == e and (e == "pe" or not self.same_engine_sync):
                return
            key = ("e", e2)
            if self.seen[e].get(key, 0) >= c:
                return
            self.eng[e].wait_ge(self.sem[e2], c)
            self.seen[e][key] = c
            self.n_wait += 1
        elif kind == "cc":
            _, c = tok
            key = ("cc",)
            if self.seen[e].get(key, 0) >= c:
                return
            self.eng[e].wait_ge(self.cc_sem, c)
            self.seen[e][key] = c
            self.n_wait += 1
        else:
            _, i, c = tok
            key = ("d", i)
            if self.seen[e].get(key, 0) >= c:
                return
            self.eng[e].wait_ge(self.dsem[i], c)
            self.seen[e][key] = c
            self.n_wait += 1

    def _deps(self, e, reads, writes):
        for r in reads:
            self._wait_tok(e, self.last_w.get(r))
        for w in writes:
            self._wait_tok(e, self.last_w.get(w))
            for t in self.readers.get(w, ()):
                self._wait_tok(e, t)

    def _commit(self, tok, reads, writes):
        for r in reads:
            self.readers.setdefault(r, []).append(tok)
        for w in writes:
            self.last_w[w] = tok
            self.readers[w] = []

    def op(self, e, fn, reads=(), writes=()):
        self._deps(e, reads, writes)
        ins = fn(self.eng[e])
        ins.then_inc(self.sem[e], 1)
        self.cnt[e] += 1
        self.n_ins += 1
        tok = ("eng", e, self.cnt[e])
        if e == "pe":
            self._flush_pe(tok)
        self._commit(tok, reads, writes)
        return tok

    def _flush_pe(self, tok):
        if self.pe_pending_reads or self.pe_pending_writes:
            self._commit(tok, self.pe_pending_reads, self.pe_pending_writes)
            self.pe_pending_reads = []
            self.pe_pending_writes = []

    def mm(self, fn, reads=(), writes=(), last=False):
        for w in writes:
            if w in self.pe_pending_writes:
                continue
            self._wait_tok("pe", self.last_w.get(w))
            for t in self.readers.get(w, ()):
                self._wait_tok("pe", t)
        for r in reads:
            self._wait_tok("pe", self.last_w.get(r))
        ins = fn(self.eng["pe"])
        self.n_ins += 1
        for r in reads:
            if r not in self.pe_pending_reads:
                self.pe_pending_reads.append(r)
        for w in writes:
            if w not in self.pe_pending_writes:
                self.pe_pending_writes.append(w)
        if last:
            ins.then_inc(self.sem["pe"], 1)
            self.cnt["pe"] += 1
            tok = ("eng", "pe", self.cnt["pe"])
            self._flush_pe(tok)
            return tok
        return None

    def dma(self, q, out, in_, reads=(), writes=(), **kw):
        self._deps(q, reads, writes)
        i = self.dnext
        self.dnext = (self.dnext + 1) % len(self.dsem)
        self._wait_tok(q, ("dma", i, self.dcnt[i])) if self.dcnt[i] else None
        ins = self.eng[q].dma_start(out=out, in_=in_, **kw)
        ins.then_inc(self.dsem[i], 16)
        self.dcnt[i] += 16
        self.n_ins += 1
        tok = ("dma", i, self.dcnt[i])
        self._commit(tok, reads, writes)
        return tok

    def dma_custom(self, q, fn, reads=(), writes=(), inc=16):
        self._deps(q, reads, writes)
        i = self.dnext
        self.dnext = (self.dnext + 1) % len(self.dsem)
        self._wait_tok(q, ("dma", i, self.dcnt[i])) if self.dcnt[i] else None
        ins = fn(self.eng[q])
        ins.then_inc(self.dsem[i], inc)
        self.dcnt[i] += inc
        self.n_ins += 1
        tok = ("dma", i, self.dcnt[i])
        self._commit(tok, reads, writes)
        return tok

    def finish(self, keys):
        for k_ in keys:
            self._wait_tok("sp", self.last_w.get(k_))
        for i, c in enumerate(self.dcnt):
            if c:
                self._wait_tok("sp", ("dma", i, c))


from contextlib import ExitStack
from concourse.bass_utils import run_bass_kernel_spmd

NCORES = 8
T = 2048
NT = 16
D = 2048
HT = 128
TT = T + HT
EPS = 1e-6
NE = 32
CAP = 768
NB = CAP // 128
NSLOT = NE * CAP
SWA = 1.702
SWL = 7.0


def barrier(k):
    toks = []
    for e in k.eng:
        if k.cnt[e]:
            toks.append(("eng", e, k.cnt[e]))
    for i, c in enumerate(k.dcnt):
        if c:
            toks.append(("dma", i, c))
    for e in k.eng:
        for t in toks:
            if t[0] == "eng" and t[1] == e:
                continue
            k._wait_tok(e, t)


class Scope:
    def __init__(self, nc, k):
        self.nc, self.k, self.es = nc, k, ExitStack()

    def sb(self, shape, dt, name):
        Scope.uid += 1
        return self.es.enter_context(self.nc.sbuf_tensor("%s_%d" % (name, Scope.uid), shape, dt))

    def close(self):
        barrier(self.k)
        self.es.close()


Scope.uid = 0


def build(stage=99, dbg_out=False):
    nc = bass.Bass("TRN2", target_bir_lowering=False)
    din = lambda n, s, dt=F32: nc.dram_tensor(n, s, dt, kind="ExternalInput").ap()
    xh = din("xh", [TT, D])
    cT = din("cT", [128, 16])
    hmask = din("hmask", [128, 1])
    invcnt = din("invcnt", [4, T])
    cm = din("cm", [128, 16])
    cf = din("cf", [128, 4])
    b_ada = din("b_ada", [1, 6 * D])
    pool_scaleT = din("pool_scaleT", [128, 16])
    cwT = din("cwT", [128, 48, 4])
    cbT = din("cbT", [128, 48])
    dt_bias = din("dt_bias", [1, 64])
    a_log = din("a_log", [1, 64])
    d_skip = din("d_skip", [1, 64])
    ssd_norm_w = din("ssd_norm_w", [1, 4096])
    if stage > 7:
        b_guT = din("b_guT", [128, NE, 32])
        b_dn = din("b_dn", [NE, D])
        w_router = din("w_router", [D, NE])
        b_router = din("b_router", [1, NE])
        fnw = din("fnw", [1, D])
        ebase_in = din("ebase", [128, NE])
        trash_in = din("trash", [128, 1])
    out = nc.dram_tensor("out", [T, D], F32, kind="ExternalOutput").ap()

    dk = "Internal"
    MODB = nc.dram_tensor("MODB", [128, 6 * D], F32, kind=dk).ap()
    YP = nc.dram_tensor("YP", [D, T], BF16, kind=dk).ap()
    GP = nc.dram_tensor("GP", [D, T], BF16, kind=dk).ap()
    GS = nc.dram_tensor("GS", [D, T], BF16).ap()
    ZT = nc.dram_tensor("ZT", [T, 4096], BF16, kind=dk).ap()
    XTOK = nc.dram_tensor("XTOK", [T, 5120], BF16, kind=dk).ap()
    BCT = nc.dram_tensor("BCT", [2048, T], BF16, kind=dk).ap()
    YS = nc.dram_tensor("YS", [4096, T], BF16, kind=dk).ap()
    H1 = nc.dram_tensor("H1", [T, D], F32).ap()
    XS = nc.dram_tensor("XS", [NSLOT + 128, D], BF16).ap()
    YSLa = nc.dram_tensor("YSLa", [NSLOT + 128, D // 2], F32).ap()
    YSLb = nc.dram_tensor("YSLb", [NSLOT + 128, D // 2], F32).ap()

    k = K(nc)
    P = Scope(nc, k)
    k.cc_sem = nc.alloc_semaphore("cc_sem")
    cc_n = [0]
    RG = [[0, 1, 2, 3], [4, 5, 6, 7]]

    def piece_rows(R, C):
        rq = 1
        while rq * 2 * C * 2 <= (1 << 20) and (R // 4) % (rq * 2) == 0:
            rq *= 2
        return rq

    CW = 1028
    cf32 = [P.sb([128, CW], F32, "cf32_%d" % i) for i in range(2)]
    cbf = [P.sb([128, CW], BF16, "cbf_%d" % i) for i in range(2)]
    cu = [0]

    def cast_weight(name, R, C, lazy=False):
        rq = piece_rows(R, C)
        NP = (R // 4) // rq
        F = rq * C // 128
        nch = -(-F // CW)
        assert F % nch == 0
        fw_ = F // nch
        qin = nc.dram_tensor(name + "_q", [NP * 128, F], F32, kind="ExternalInput")
        bnc = nc.dram_tensor(name + "_b", [NP * 128, F], BF16)
        full = nc.dram_tensor(name + "_f", [R, C], BF16)
        units = [(i, c) for i in range(NP) for c in range(nch)]
        base = cu[0]
        cu[0] += len(units)

        def load(u):
            i, c = units[u]
            bi = (base + u) % 2
            k.dma("pool", cf32[bi][:, 0:fw_], qin[i * 128:(i + 1) * 128, c * fw_:(c + 1) * fw_], writes=["cf32_%d" % bi])

        def unit(u):
            i, c = units[u]
            bi = (base + u) % 2
            if u == 0:
                for u2 in range(min(2, len(units))):
                    load(u2)
            k.op("pool", lambda e: e.tensor_copy(cbf[bi][:, 0:fw_], cf32[bi][:, 0:fw_]), reads=["cf32_%d" % bi], writes=["cbf_%d" % bi])
            k.dma("pool", bnc[i * 128:(i + 1) * 128, c * fw_:(c + 1) * fw_], cbf[bi][:, 0:fw_], reads=["cbf_%d" % bi], writes=["%s_b%d_%d" % (name, i, c)])
            if u + 2 < len(units):
                load(u + 2)

        for u in range(len(units)):
            if lazy:
                lazyq.append(lambda u=u: unit(u))
            else:
                unit(u)
        return dict(name=name, rq=rq, NP=NP, nch=nch, bnc=bnc, full=full)

    lazyq = []

    def pump(n):
        while n > 0 and lazyq:
            lazyq.pop(0)()
            n -= 1

    def gather_weight(cw_):
        toks = []
        name, rq = cw_["name"], cw_["rq"]
        for i in range(cw_["NP"]):
            k._deps("pool", ["%s_b%d_%d" % (name, i, c) for c in range(cw_["nch"])], [])
            nc.gpsimd.collective_compute("AllGather", ALU.bypass, replica_groups=RG,
                                         ins=[cw_["bnc"][i * 128:(i + 1) * 128, :].opt()],
                                         outs=[cw_["full"][i * 4 * rq:(i + 1) * 4 * rq, :].opt()]).then_inc(k.cc_sem)
            cc_n[0] += 1
            toks.append(cc_n[0])
        return cw_["full"].ap(), toks

    def dist_weight(name, R, C):
        return gather_weight(cast_weight(name, R, C))

    PS = [nc.alloc_psum_tensor("ps%d" % i, [128, 512], F32) for i in range(5)]
    PSB = [nc.alloc_psum_tensor("psb%d" % i, [128, 512], BF16) for i in range(2)]
    PSY = nc.alloc_psum_tensor("psy", [128, 512], F32)
    psi = [0]

    def next_ps():
        i = psi[0] % len(PS)
        psi[0] += 1
        return PS[i], "ps%d" % i

    psbi = [0]

    def next_psb():
        i = psbi[0] % len(PSB)
        psbi[0] += 1
        return PSB[i], "psb%d" % i

    identf = P.sb([128, 128], F32, "identf")
    ident = P.sb([128, 128], BF16, "ident")
    tri = P.sb([128, 128], F32, "tri")
    ustr = P.sb([128, 128], BF16, "ustr")
    onesf = P.sb([128, 128], F32, "onesf")
    onesb = P.sb([128, 128], BF16, "onesb")
    ss = P.sb([128, 1], F32, "ss")
    dt_all = P.sb([128, 16, 64], F32, "dt_all")
    k.op("pool", lambda e: e.memset(identf[:], 1.0), writes=["identf"])
    k.op("pool", lambda e: e.affine_select(out=identf[:], in_=identf[:], pattern=[[-1, 128]],
                                           compare_op=ALU.is_equal, fill=0.0, base=0, channel_multiplier=1),
         reads=["identf"], writes=["identf"])
    k.op("dve", lambda e: e.tensor_copy(ident[:], identf[:]), reads=["identf"], writes=["ident"])
    k.op("pool", lambda e: e.memset(tri[:], 1.0), writes=["tri"])
    k.op("pool", lambda e: e.affine_select(out=tri[:], in_=tri[:], pattern=[[1, 128]],
                                           compare_op=ALU.is_ge, fill=0.0, base=0, channel_multiplier=-1),
         reads=["tri"], writes=["tri"])
    k.op("pool", lambda e: e.memset(onesf[:], 1.0), writes=["onesf"])
    k.op("pool", lambda e: e.affine_select(out=onesf[:], in_=onesf[:], pattern=[[1, 128]],
                                           compare_op=ALU.is_gt, fill=0.0, base=0, channel_multiplier=-1),
         reads=["onesf"], writes=["onesf"])
    k.op("dve", lambda e: e.tensor_copy(ustr[:], onesf[:]), reads=["onesf"], writes=["ustr"])
    k.op("pool", lambda e: e.memset(onesf[:], 1.0), reads=["ustr"], writes=["onesf"])
    k.op("dve", lambda e: e.tensor_copy(onesb[:], onesf[:]), reads=["onesf"], writes=["onesb"])

    wslab = [P.sb([128, 16, 512], BF16, "wslab%d" % i) for i in range(2)]
    wsi = [0]

    def load_slab(W2d, col0, ncols, KC, buf=None, key=None, row0=0, cc=None):
        if buf is None:
            i = wsi[0] % 2
            wsi[0] += 1
            buf = wslab[i]
            key = "wslab%d" % i
        src = W2d[row0:row0 + KC * 128, col0:col0 + ncols].rearrange("(kc p) n -> p kc n", p=128)
        keys = []
        if cc is None:
            cc = WT[id(W2d)]
        k._wait_tok("sp", ("cc", cc))
        for k0 in range(0, KC, 4):
            kk = key if k0 == 0 else key + "x%d" % k0
            keys.append(kk)
            k.dma("sp", buf[:, k0:k0 + 4, 0:ncols], src[:, k0:k0 + 4, :], writes=[kk])
        return buf, keys

    def rms_rstd(sq, src, srck, n):
        k.op("act", lambda e: e.activation(out=sq, in_=src, func=AF.Square), reads=[srck], writes=["sq"])
        k.op("dve", lambda e: e.reduce_sum(out=ss[:], in_=sq, axis=AX.X), reads=["sq"], writes=["ss"])
        k.op("act", lambda e: e.activation(out=ss[:], in_=ss[:], func=AF.Sqrt, scale=1.0 / n, bias=EPS), reads=["ss"], writes=["ss"])
        k.op("dve", lambda e: e.reciprocal(ss[:], ss[:]), reads=["ss"], writes=["ss"])

    zs = Scope(nc, k)
    zb = zs.sb([128, D], BF16, "zb")
    zf = zs.sb([128, D], F32, "zf")
    k.op("pool", lambda e: e.memset(zb[:], 0.0), writes=["zb"])
    k.op("pool", lambda e: e.memset(zf[:], 0.0), writes=["zf"])
    for r in range(NSLOT // 128 + 1):
        k.dma("sp", XS[r * 128:(r + 1) * 128, :], zb[:], reads=["zb"], writes=["XSz"])
    k.dma("sp", YSLa[NSLOT:NSLOT + 128, :], zf[:, 0:D // 2], reads=["zf"], writes=["YSLz"])
    k.dma("sp", YSLb[NSLOT:NSLOT + 128, :], zf[:, 0:D // 2], reads=["zf"], writes=["YSLz2"])
    zs.close()
    w_ada, t_ada = dist_weight("w_ada", D, 6 * D)
    w_in, t_in = dist_weight("w_in", D, 16448)
    pool_w2, t_pw = dist_weight("pool_w", 2048, 512)
    w_bp, t_bp = dist_weight("w_bp", D, D)
    w_bs, t_bs = dist_weight("w_bs", 4096, D)
    w_out, t_out = dist_weight("w_out", D, D)
    if stage > 7:
        cw_gu = [cast_weight("w_gu%d" % gq, 8 * D, 4096, lazy=True) for gq in range(4)]
        cw_dn = [cast_weight("w_dn%d" % gq, 8 * D, D, lazy=True) for gq in range(4)]
    WT = {id(w_ada): t_ada[-1], id(w_in): t_in[-1], id(pool_w2): t_pw[-1], id(w_bp): t_bp[-1], id(w_bs): t_bs[-1], id(w_out): t_out[-1]}


    s1 = Scope(nc, k)
    cts = s1.sb([128, 16], F32, "cts")
    k.dma("sp", cts[:], cT, writes=["cts"])
    k.op("act", lambda e: e.activation(out=cts[:], in_=cts[:], func=AF.Silu), reads=["cts"], writes=["cts"])
    lhsc = s1.sb([128, 16, 128], BF16, "lhsc")
    for kc in range(16):
        k.op("dve", lambda e, kc=kc: e.tensor_copy(lhsc[:, kc, :], cts[:, kc:kc + 1].to_broadcast([128, 128])),
             reads=["cts"], writes=["lhsc"])
    bb = [s1.sb([128, 512], F32, "bb%d" % i) for i in range(2)]
    mo = [s1.sb([128, 512], F32, "mo%d" % i) for i in range(2)]
    for s in range(24):
        pump(4)
        buf, wkeys = load_slab(w_ada, s * 512, 512, 16)
        bt = bb[s % 2]
        bk = "bb%d" % (s % 2)
        mt_ = mo[s % 2]
        mk_ = "mo%d" % (s % 2)
        k.dma("sp", bt[:], b_ada[0:1, s * 512:(s + 1) * 512].partition_broadcast(128), writes=[bk])
        ps, pk = next_ps()
        for kc in range(16):
            k.mm(lambda e, kc=kc: e.matmul(ps[:], lhsT=lhsc[:, kc, :], rhs=buf[:, kc, :], start=(kc == 0), stop=(kc == 15)),
                 reads=["lhsc"] + wkeys, writes=[pk], last=(kc == 15))
        addc = 1.0 if (s // 4) in (1, 4) else 0.0
        k.op("dve", lambda e: e.scalar_tensor_tensor(out=mt_[:], in0=ps[:], scalar=addc, in1=bt[:],
                                                     op0=ALU.add, op1=ALU.add), reads=[pk, bk], writes=[mk_])
        k.dma("sp", MODB[:, s * 512:(s + 1) * 512], mt_[:], reads=[mk_], writes=["MODB"])
    s1.close()
    if stage <= 1:
        return nc, k

    s3 = Scope(nc, k)
    uT = s3.sb([128, 16, TT], BF16, "uT")
    s2 = Scope(nc, k)
    sh1 = s2.sb([128, D], F32, "sh1")
    sc1 = s2.sb([128, D], F32, "sc1")
    k.dma("sp", sh1[:], MODB[:, 0:D], reads=["MODB"], writes=["sh1"])
    k.dma("sp", sc1[:], MODB[:, D:2 * D], reads=["MODB"], writes=["sc1"])
    hm = s2.sb([128, 1], F32, "hm")
    k.dma("sp", hm[:], hmask, writes=["hm"])
    xt = [s2.sb([128, D], F32, "xt%d" % i) for i in range(2)]
    sq = s2.sb([128, D], F32, "sq")
    ub = s2.sb([128, D], BF16, "ub")
    for ti in range(17):
        pump(4)
        x_t = xt[ti % 2]
        xk = "xt%d" % (ti % 2)
        k.dma("sp", x_t[:], xh[ti * 128:(ti + 1) * 128, :], writes=[xk])
        rms_rstd(sq[:], x_t[:], xk, D)
        k.op("dve", lambda e: e.scalar_tensor_tensor(out=sq[:], in0=x_t[:], scalar=ss[:, 0:1], in1=sc1[:], op0=ALU.mult, op1=ALU.mult),
             reads=[xk, "ss", "sc1"], writes=["sq"])
        k.op("dve", lambda e: e.tensor_tensor(out=ub[:], in0=sq[:], in1=sh1[:], op=ALU.add), reads=["sq", "sh1"], writes=["ub"])
        if ti == 0:
            k.op("dve", lambda e: e.tensor_scalar(out=ub[:], in0=ub[:], scalar1=hm[:, 0:1], scalar2=None, op0=ALU.mult),
                 reads=["ub", "hm"], writes=["ub"])
        for kq in range(4):
            pb, pbk = next_psb()
            for j in range(4):
                k.op("pe", lambda e, j=j: e.transpose(pb[:, j * 128:(j + 1) * 128], ub[:, (4 * kq + j) * 128:(4 * kq + j + 1) * 128], ident[:]),
                     reads=["ub", "ident"], writes=[pbk])
            k.op("act", lambda e: e.copy(uT[:, 4 * kq:4 * kq + 4, ti * 128:(ti + 1) * 128], pb[:].rearrange("p (j t) -> p j t", j=4)),
                 reads=[pbk], writes=["uT"])
    s2.close()

    TB = [(0, 512), (512, 512), (1024, 512), (1536, 512), (2048, 128)]
    TBO = [(128, 512), (640, 512), (1152, 512), (1664, 512)]
    rowf = [s3.sb([128, TT], F32, "rowf%d" % i) for i in range(2)]
    rfi = [0]

    def proj_fm(buf, wkeys, fc, KC=16, src=None, srck="uT", tbs=TB):
        pump(4)
        i = rfi[0] % 2
        rfi[0] += 1
        rf = rowf[i]
        rk = "rowf%d" % i
        sr = src if src is not None else uT
        for (t0, n) in tbs:
            ps, pk = next_ps()
            for kc in range(KC):
                k.mm(lambda e, kc=kc: e.matmul(ps[:, 0:n], lhsT=buf[:, kc, fc * 128:(fc + 1) * 128], rhs=sr[:, kc, t0:t0 + n],
                                               start=(kc == 0), stop=(kc == KC - 1)),
                     reads=wkeys + [srck], writes=[pk], last=(kc == KC - 1))
            k.op("act", lambda e: e.copy(rf[:, t0:t0 + n], ps[:, 0:n]), reads=[pk], writes=[rk])
        return rf, rk

    cw = s3.sb([128, 48, 4], F32, "cw")
    cb = s3.sb([128, 48], F32, "cb")
    k.dma("sp", cw[:], cwT, writes=["cw"])
    k.dma("sp", cb[:], cbT, writes=["cb"])
    acc = s3.sb([128, T], F32, "acc")
    obf = [s3.sb([128, T], BF16, "obf%d" % i) for i in range(2)]
    obi = [0]

    def next_ob():
        i = obi[0] % 2
        obi[0] += 1
        return obf[i], "obf%d" % i

    stg = s3.sb([128, 16, 128], BF16, "stg")

    for sl in range(12):
        buf, wkeys = load_slab(w_in, 6144 + sl * 512, 512, 16)
        for fc in range(4):
            ch = sl * 4 + fc
            rf, rk = proj_fm(buf, wkeys, fc)
            k.op("act", lambda e: e.activation(out=acc[:], in_=rf[:, 128:TT], func=AF.Identity, scale=cw[:, ch, 3:4], bias=cb[:, ch:ch + 1]),
                 reads=[rk, "cw", "cb"], writes=["acc"])
            for j in (1, 2, 3):
                k.op("dve", lambda e, j=j: e.scalar_tensor_tensor(out=acc[:], in0=rf[:, 128 - j:TT - j], scalar=cw[:, ch, 3 - j:4 - j], in1=acc[:],
                                                                 op0=ALU.mult, op1=ALU.add), reads=[rk, "cw", "acc"], writes=["acc"])
            ob, ok_ = next_ob()
            k.op("act", lambda e: e.activation(out=ob[:], in_=acc[:], func=AF.Silu), reads=["acc"], writes=[ok_])
            if ch >= 32:
                k.dma("sp", BCT[(ch - 32) * 128:(ch - 31) * 128, :], ob[:], reads=[ok_], writes=["BCT"])
            if ch < 40:
                for tq in range(4):
                    pb, pbk = next_psb()
                    for j in range(4):
                        tt = tq * 4 + j
                        k.op("pe", lambda e, j=j, tt=tt: e.transpose(pb[:, j * 128:(j + 1) * 128], ob[:, tt * 128:(tt + 1) * 128], ident[:]),
                             reads=[ok_, "ident"], writes=[pbk])
                    k.op("dve", lambda e: e.tensor_copy(stg[:, tq * 4:tq * 4 + 4, :], pb[:].rearrange("p (j t) -> p j t", j=4)),
                         reads=[pbk], writes=["stg"])
                dst = XTOK[:, ch * 128:(ch + 1) * 128].rearrange("(ti p) c -> p ti c", p=128)
                for tq in range(4):
                    k.dma("sp", dst[:, tq * 4:tq * 4 + 4, :], stg[:, tq * 4:tq * 4 + 4, :], reads=["stg"], writes=["XTOK"])

    dtb = s3.sb([128, 64], F32, "dtb")
    k.dma("sp", dtb[:], dt_bias.partition_broadcast(128), writes=["dtb"])
    buf, wkeys = load_slab(w_in, 12288, 64, 16)
    for ti in range(16):
        ps, pk = next_ps()
        for kc in range(16):
            k.mm(lambda e, kc=kc: e.matmul(ps[:, 0:64], lhsT=uT[:, kc, 128 + ti * 128:256 + ti * 128], rhs=buf[:, kc, 0:64],
                                           start=(kc == 0), stop=(kc == 15)), reads=wkeys + ["uT"], writes=[pk], last=(kc == 15))
        k.op("dve", lambda e: e.tensor_tensor(out=dt_all[:, ti, :], in0=ps[:, 0:64], in1=dtb[:], op=ALU.add), reads=[pk, "dtb"], writes=["dt_all"])
    k.op("act", lambda e: e.activation(out=dt_all[:], in_=dt_all[:], func=AF.Exp), reads=["dt_all"], writes=["dt_all"])
    k.op("act", lambda e: e.activation(out=dt_all[:], in_=dt_all[:], func=AF.Ln, bias=1.0), reads=["dt_all"], writes=["dt_all"])

    zt = [s3.sb([128, 512], BF16, "zt%d" % i) for i in range(2)]
    zi = [0]
    for sl in range(8):
        buf, wkeys = load_slab(w_in, 2048 + sl * 512, 512, 16)
        for ti in range(16):
            ps, pk = next_ps()
            for kc in range(16):
                k.mm(lambda e, kc=kc: e.matmul(ps[:], lhsT=uT[:, kc, 128 + ti * 128:256 + ti * 128], rhs=buf[:, kc, :],
                                               start=(kc == 0), stop=(kc == 15)), reads=wkeys + ["uT"], writes=[pk], last=(kc == 15))
            z_t = zt[zi[0] % 2]
            zk = "zt%d" % (zi[0] % 2)
            zi[0] += 1
            k.op("act", lambda e: e.activation(out=z_t[:], in_=ps[:], func=AF.Silu), reads=[pk], writes=[zk])
            k.dma("sp", ZT[ti * 128:(ti + 1) * 128, sl * 512:(sl + 1) * 512], z_t[:], reads=[zk], writes=["ZT"])

    for gi, (c0, dst) in enumerate(((12352, GP), (14400, GS))):
        for sl in range(4):
            buf, wkeys = load_slab(w_in, c0 + sl * 512, 512, 16)
            for fc in range(4):
                rf, rk = proj_fm(buf, wkeys, fc, tbs=TBO)
                ob, ok_ = next_ob()
                k.op("act", lambda e: e.activation(out=ob[:], in_=rf[:, 128:TT], func=AF.Sigmoid), reads=[rk], writes=[ok_])
                k.dma("sp", dst[(sl * 4 + fc) * 128:(sl * 4 + fc + 1) * 128, :], ob[:], reads=[ok_], writes=["G%d" % gi])

    icn = s3.sb([128, T], F32, "icn")
    diffT = s3.sb([128, 4, T], BF16, "diffT")
    pwa = s3.sb([128, TT], F32, "pwa")
    pwb = s3.sb([128, TT], F32, "pwb")
    pscT = s3.sb([128, 16], F32, "pscT")
    k.dma("sp", pscT[:], pool_scaleT, writes=["pscT"])
    for g in range(4):
        w = (2, 4, 8, 16)[g]
        k.dma("sp", icn[:], invcnt[g:g + 1, :].partition_broadcast(128), writes=["icn"])
        buf, wkeys = load_slab(w_in, g * 512, 512, 16)
        for fc in range(4):
            rf, rk = proj_fm(buf, wkeys, fc)
            cur, curk = rf, rk
            sh = 1
            pp = [(pwa, "pwa"), (pwb, "pwb")]
            pi = 0
            while sh < w:
                nx, nxk = pp[pi % 2]
                pi += 1
                k.op("dve", lambda e, cur=cur, nx=nx, sh=sh: e.tensor_tensor(out=nx[:, 16:TT], in0=cur[:, 16:TT], in1=cur[:, 16 - sh:TT - sh], op=ALU.add),
                     reads=[curk], writes=[nxk])
                cur, curk = nx, nxk
                sh *= 2
            k.op("dve", lambda e, cur=cur: e.tensor_tensor(out=acc[:], in0=cur[:, 128:TT], in1=icn[:], op=ALU.mult), reads=[curk, "icn"], writes=["acc"])
            k.op("dve", lambda e: e.tensor_tensor(out=diffT[:, fc, :], in0=acc[:], in1=rf[:, 128:TT], op=ALU.subtract), reads=["acc", rk], writes=["diffT"])
        pbuf, pkeys = load_slab(pool_w2, 0, 512, 4, row0=g * 512)
        for dc in range(4):
            rf, rk = proj_fm(pbuf, pkeys, dc, KC=4, src=diffT, srck="diffT", tbs=[(a, 512) for a in (0, 512, 1024, 1536)])
            ob, ok_ = next_ob()
            k.op("act", lambda e: e.activation(out=ob[:], in_=rf[:, 0:T], func=AF.Copy, scale=pscT[:, g * 4 + dc:g * 4 + dc + 1]), reads=[rk, "pscT"], writes=[ok_])
            k.dma("sp", YP[(g * 4 + dc) * 128:(g * 4 + dc + 1) * 128, :], ob[:], reads=[ok_], writes=["YP"])
    s3.close()
    if stage <= 3:
        return nc, k

    s4 = Scope(nc, k)
    abc = s4.sb([128, 64], F32, "abc")
    dsk = s4.sb([128, 64], F32, "dsk")
    k.dma("sp", abc[:], a_log.partition_broadcast(128), writes=["abc"])
    k.op("act", lambda e: e.activation(out=abc[:], in_=abc[:], func=AF.Exp), reads=["abc"], writes=["abc"])
    k.op("dve", lambda e: e.tensor_scalar(out=abc[:], in0=abc[:], scalar1=-1.0, scalar2=None, op0=ALU.mult), reads=["abc"], writes=["abc"])
    k.dma("sp", dsk[:], d_skip.partition_broadcast(128), writes=["dsk"])
    nwb = s4.sb([128, 4096], F32, "nwb")
    k.dma("sp", nwb[:], ssd_norm_w.partition_broadcast(128), writes=["nwb"])
    dacs_all = s4.sb([128, 16, 64], F32, "dacs_all")
    tot_all = s4.sb([128, 16, 64], F32, "tot_all")
    S = s4.sb([128, 4096], F32, "S")
    Sbf = s4.sb([128, 4096], BF16, "Sbf")
    dsum = s4.sb([128, 64], F32, "dsum")
    xb_t = [s4.sb([128, 5120], BF16, "xb_t%d" % i) for i in range(2)]
    xdtd = s4.sb([128, 4096], BF16, "xdtd")
    xdt = s4.sb([128, 4096], BF16, "xdt")
    tmp64 = s4.sb([128, 64], F32, "tmp64")
    wend = s4.sb([128, 64], F32, "wend")
    cdec = s4.sb([128, 64], F32, "cdec")
    da = s4.sb([128, 64], F32, "da")
    k.op("pool", lambda e: e.memset(S[:], 0.0), writes=["S"])
    k.op("pool", lambda e: e.memset(dsum[:], 0.0), writes=["dsum"])

    def tile_dt_stuff(ti, x_t, xk, first_pass):
        dtt = dt_all[:, ti, :]
        if first_pass:
            k.op("dve", lambda e: e.tensor_tensor(out=da[:], in0=dtt, in1=abc[:], op=ALU.mult), reads=["dt_all", "abc"], writes=["da"])
            ps, pk = next_ps()
            k.mm(lambda e: e.matmul(ps[:, 0:64], lhsT=tri[:], rhs=da[:], start=True, stop=True), reads=["tri", "da"], writes=[pk], last=True)
            k.op("dve", lambda e: e.tensor_copy(dacs_all[:, ti, :], ps[:, 0:64]), reads=[pk], writes=["dacs_all"])
            ps2, pk2 = next_ps()
            k.mm(lambda e: e.matmul(ps2[:, 0:64], lhsT=onesf[:], rhs=da[:], start=True, stop=True), reads=["onesf", "da"], writes=[pk2], last=True)
            k.op("dve", lambda e: e.tensor_copy(tot_all[:, ti, :], ps2[:, 0:64]), reads=[pk2], writes=["tot_all"])
            k.op("dve", lambda e: e.tensor_tensor(out=dsum[:], in0=dsum[:], in1=tot_all[:, ti, :], op=ALU.add), reads=["dsum", "tot_all"], writes=["dsum"])
        k.op("dve", lambda e: e.tensor_tensor(out=tmp64[:], in0=tot_all[:, ti, :], in1=dacs_all[:, ti, :], op=ALU.subtract),
             reads=["tot_all", "dacs_all"], writes=["tmp64"])
        k.op("act", lambda e: e.activation(out=tmp64[:], in_=tmp64[:], func=AF.Exp), reads=["tmp64"], writes=["tmp64"])
        k.op("dve", lambda e: e.tensor_tensor(out=wend[:], in0=tmp64[:], in1=dtt, op=ALU.mult), reads=["tmp64", "dt_all"], writes=["wend"])
        k.op("act", lambda e: e.activation(out=cdec[:], in_=tot_all[:, ti, :], func=AF.Exp), reads=["tot_all"], writes=["cdec"])
        k.op("dve", lambda e: e.tensor_tensor(out=xdtd[:].rearrange("p (h j) -> p h j", h=64), in0=x_t[:, 0:4096].rearrange("p (h j) -> p h j", h=64),
                                              in1=wend[:].unsqueeze(2).to_broadcast([128, 64, 64]), op=ALU.mult), reads=[xk, "wend"], writes=["xdtd"])

    def state_update(x_t, xk):
        for g in range(8):
            ps, pk = next_ps()
            k.mm(lambda e: e.matmul(ps[:], lhsT=x_t[:, 4096 + 128 * g:4096 + 128 * (g + 1)], rhs=xdtd[:, 512 * g:512 * (g + 1)], start=True, stop=True),
                 reads=[xk, "xdtd"], writes=[pk], last=True)
            Sg = S[:, 512 * g:512 * (g + 1)]
            k.op("dve", lambda e: e.tensor_tensor(out=Sg.rearrange("p (h j) -> p h j", h=8), in0=Sg.rearrange("p (h j) -> p h j", h=8),
                                                  in1=cdec[:, 8 * g:8 * g + 8].unsqueeze(2).to_broadcast([128, 8, 64]), op=ALU.mult),
                 reads=["S", "cdec"], writes=["S"])
            k.op("dve", lambda e: e.tensor_tensor(out=Sg, in0=Sg, in1=ps[:], op=ALU.add), reads=["S", pk], writes=["S"])

    for ti in range(16):
        x_t = xb_t[ti % 2]
        xk = "xb_t%d" % (ti % 2)
        k.dma("sp", x_t[:], XTOK[ti * 128:(ti + 1) * 128, :], writes=[xk])
        pump(8)
        tile_dt_stuff(ti, x_t, xk, True)
        state_update(x_t, xk)
    agi = [nc.dram_tensor("agi%d" % i, [128, 1024], F32) for i in range(4)] + [nc.dram_tensor("agi4", [128, 64], F32)]
    ago = [nc.dram_tensor("ago%d" % i, [512, 1024], F32) for i in range(4)] + [nc.dram_tensor("ago4", [512, 64], F32)]
    for i in range(5):
        srcS = S[:, 1024 * i:1024 * (i + 1)] if i < 4 else dsum[:]
        k.dma("sp", agi[i][:, :], srcS, reads=["S", "dsum"], writes=["agi%d" % i])
        k._deps("pool", ["agi%d" % i], [])
        nc.gpsimd.collective_compute("AllGather", ALU.bypass, replica_groups=RG,
                                     ins=[agi[i].ap().opt()], outs=[ago[i].ap().opt()]).then_inc(k.cc_sem)
        cc_n[0] += 1
    k._wait_tok("pool", ("cc", cc_n[0]))
    k.op("pool", lambda e: e.memset(tmp64[:, 0:1], 0.0), reads=["tmp64"], writes=["ag_out", "tmp64"])
    if stage > 7:
        pump(10 ** 9)
        w_gu_p, t_gu, w_dn_p, t_dn = [], [], [], []
        for gq in range(4):
            wv_, tv_ = gather_weight(cw_gu[gq])
            w_gu_p.append(wv_)
            t_gu += tv_
        for gq in range(4):
            wv_, tv_ = gather_weight(cw_dn[gq])
            w_dn_p.append(wv_)
            t_dn += tv_
    cms = s4.sb([128, 16], F32, "cms")
    cfs = s4.sb([128, 4], F32, "cfs")
    k.dma("sp", cms[:], cm, writes=["cms"])
    k.dma("sp", cfs[:], cf, writes=["cfs"])
    dsj = s4.sb([128, 4, 64], F32, "dsj")
    for j in range(4):
        k.dma("sp", dsj[:, j, :], ago[4][j * 128:(j + 1) * 128, :], reads=["ag_out"], writes=["dsj%d" % j])
    dsjk = ["dsj%d" % j for j in range(4)]
    k.op("pool", lambda e: e.memset(S[:], 0.0), reads=["S"], writes=["S"])
    Fj = s4.sb([128, 4096], F32, "Fj")
    coef = s4.sb([128, 64], F32, "coef")
    for j in range(3):
        for i4 in range(4):
            k.dma("sp", Fj[:, 1024 * i4:1024 * (i4 + 1)], ago[i4][j * 128:(j + 1) * 128, :], reads=["ag_out"], writes=["Fj"] if i4 == 0 else ["Fjx%d" % i4])
        k.op("dve", lambda e: e.tensor_scalar(out=coef[:], in0=dsj[:, 0, :], scalar1=cms[:, 4 * j:4 * j + 1], scalar2=None, op0=ALU.mult),
             reads=dsjk + ["cms"], writes=["coef"])
        for m in range(1, 4):
            k.op("dve", lambda e, m=m: e.scalar_tensor_tensor(out=coef[:], in0=dsj[:, m, :], scalar=cms[:, 4 * j + m:4 * j + m + 1], in1=coef[:],
                                                             op0=ALU.mult, op1=ALU.add), reads=dsjk + ["cms", "coef"], writes=["coef"])
        k.op("act", lambda e: e.activation(out=coef[:], in_=coef[:], func=AF.Exp), reads=["coef"], writes=["coef"])
        k.op("dve", lambda e: e.tensor_scalar(out=coef[:], in0=coef[:], scalar1=cfs[:, j:j + 1], scalar2=None, op0=ALU.mult), reads=["coef", "cfs"], writes=["coef"])
        k.op("dve", lambda e: e.tensor_tensor(out=Fj[:].rearrange("p (h j) -> p h j", h=64), in0=Fj[:].rearrange("p (h j) -> p h j", h=64),
                                              in1=coef[:].unsqueeze(2).to_broadcast([128, 64, 64]), op=ALU.mult), reads=["Fj", "Fjx1", "Fjx2", "Fjx3", "coef"], writes=["Fj", "Fjx1", "Fjx2", "Fjx3"])
        k.op("dve", lambda e: e.tensor_tensor(out=S[:], in0=S[:], in1=Fj[:], op=ALU.add), reads=["S", "Fj", "Fjx1", "Fjx2", "Fjx3"], writes=["S"])

    bct = [s4.sb([128, 16, 128], BF16, "bct%d" % i) for i in range(2)]
    z_t = [s4.sb([128, 4096], BF16, "z_t%d" % i) for i in range(2)]
    edacs = s4.sb([128, 64], F32, "edacs")
    cbm = s4.sb([128, 128], F32, "cbm")
    Xd = s4.sb([128, 1024], F32, "Xd")
    Lt8 = s4.sb([128, 1024], F32, "Lt8")
    Mt8 = s4.sb([128, 1024], BF16, "Mt8")
    yg = s4.sb([128, 512], F32, "yg")
    yo = s4.sb([128, 512], F32, "yo")
    ybf = s4.sb([128, 512], BF16, "ybf")
    ysq = s4.sb([128, 512], F32, "ysq")
    ystg = s4.sb([128, 4, 128], BF16, "ystg")
    BCTv = BCT.rearrange("(c p) t -> p c t", p=128)
    for ti in range(16):
        x_t = xb_t[ti % 2]
        xk = "xb_t%d" % (ti % 2)
        k.dma("sp", x_t[:], XTOK[ti * 128:(ti + 1) * 128, :], writes=[xk])
        bc = bct[ti % 2]
        bck = "bct%d" % (ti % 2)
        bckeys = []
        for c0 in range(0, 16, 4):
            kk = bck if c0 == 0 else bck + "x%d" % c0
            bckeys.append(kk)
            k.dma("sp", bc[:, c0:c0 + 4, :], BCTv[:, c0:c0 + 4, ti * 128:(ti + 1) * 128], writes=[kk])
        zz = z_t[ti % 2]
        zk = "z_t%d" % (ti % 2)
        k.dma("sp", zz[:], ZT[ti * 128:(ti + 1) * 128, :], writes=[zk])
        tile_dt_stuff(ti, x_t, xk, False)
        k.op("dve", lambda e: e.tensor_tensor(out=xdt[:].rearrange("p (h j) -> p h j", h=64), in0=x_t[:, 0:4096].rearrange("p (h j) -> p h j", h=64),
                                              in1=dt_all[:, ti, :].unsqueeze(2).to_broadcast([128, 64, 64]), op=ALU.mult), reads=[xk, "dt_all"], writes=["xdt"])
        k.op("act", lambda e: e.activation(out=edacs[:], in_=dacs_all[:, ti, :], func=AF.Exp), reads=["dacs_all"], writes=["edacs"])
        k.op("act", lambda e: e.copy(Sbf[:], S[:]), reads=["S"], writes=["Sbf"])
        for g in range(8):
            ps, pk = next_ps()
            k.mm(lambda e: e.matmul(ps[:, 0:128], lhsT=bc[:, g, :], rhs=bc[:, 8 + g, :], start=True, stop=True), reads=bckeys, writes=[pk], last=True)
            k.op("dve", lambda e: e.tensor_tensor(out=cbm[:], in0=ps[:, 0:128], in1=tri[:], op=ALU.mult), reads=[pk, "tri"], writes=["cbm"])
            psy, pyk = PSY, "psy"
            k.op("pool", lambda e: e.tensor_tensor(out=Xd[:].rearrange("p (h l) -> p h l", h=8), in0=identf[:].unsqueeze(1).to_broadcast([128, 8, 128]),
                                                  in1=dacs_all[:, ti, 8 * g:8 * g + 8].unsqueeze(2).to_broadcast([128, 8, 128]), op=ALU.mult),
                 reads=["identf", "dacs_all"], writes=["Xd"])
            for hq in range(2):
                psr, prk = next_ps()
                k.mm(lambda e: e.matmul(psr[:], lhsT=onesf[:], rhs=Xd[:, hq * 512:(hq + 1) * 512], start=True, stop=True), reads=["onesf", "Xd"], writes=[prk], last=True)
                Lh = Lt8[:, hq * 512:(hq + 1) * 512]
                k.op("dve", lambda e: e.tensor_tensor(out=Lh.rearrange("p (h l) -> p h l", h=4), in0=psr[:].rearrange("p (h l) -> p h l", h=4),
                                                      in1=dacs_all[:, ti, 8 * g + 4 * hq:8 * g + 4 * hq + 4].unsqueeze(2).to_broadcast([128, 4, 128]), op=ALU.subtract),
                     reads=[prk, "dacs_all"], writes=["Lt8_%d" % hq])
                k.op("dve", lambda e: e.tensor_scalar(out=Lh, in0=Lh, scalar1=0.0, scalar2=None, op0=ALU.min), reads=["Lt8_%d" % hq], writes=["Lt8_%d" % hq])
                k.op("act", lambda e: e.activation(out=Lh, in_=Lh, func=AF.Exp), reads=["Lt8_%d" % hq], writes=["Lt8_%d" % hq])
                Mh = Mt8[:, hq * 512:(hq + 1) * 512]
                k.op("dve", lambda e: e.tensor_tensor(out=Mh.rearrange("p (h l) -> p h l", h=4), in0=Lh.rearrange("p (h l) -> p h l", h=4),
                                                      in1=cbm[:].unsqueeze(1).to_broadcast([128, 4, 128]), op=ALU.mult),
                     reads=["Lt8_%d" % hq, "cbm"], writes=["Mt8_%d" % hq])
                for h4 in range(4):
                    hh = hq * 4 + h4
                    h = 8 * g + hh
                    k.mm(lambda e, h=h, hh=hh: e.matmul(psy[:, hh * 64:(hh + 1) * 64], lhsT=Mt8[:, hh * 128:(hh + 1) * 128], rhs=xdt[:, h * 64:(h + 1) * 64], start=True, stop=True),
                         reads=["Mt8_%d" % hq, "xdt"], writes=[pyk], last=True)
            pso, pok = next_ps()
            k.mm(lambda e: e.matmul(pso[:], lhsT=bc[:, 8 + g, :], rhs=Sbf[:, 512 * g:512 * (g + 1)], start=True, stop=True), reads=bckeys + ["Sbf"], writes=[pok], last=True)
            k.op("dve", lambda e: e.tensor_tensor(out=yo[:].rearrange("p (h j) -> p h j", h=8), in0=pso[:].rearrange("p (h j) -> p h j", h=8),
                                                  in1=edacs[:, 8 * g:8 * g + 8].unsqueeze(2).to_broadcast([128, 8, 64]), op=ALU.mult), reads=[pok, "edacs"], writes=["yo"])
            k.op("dve", lambda e: e.tensor_tensor(out=yg[:], in0=psy[:], in1=yo[:], op=ALU.add), reads=[pyk, "yo"], writes=["yg"])
            k.op("dve", lambda e: e.tensor_tensor(out=yo[:].rearrange("p (h j) -> p h j", h=8), in0=x_t[:, 512 * g:512 * (g + 1)].rearrange("p (h j) -> p h j", h=8),
                                                  in1=dsk[:, 8 * g:8 * g + 8].unsqueeze(2).to_broadcast([128, 8, 64]), op=ALU.mult), reads=[xk, "dsk", "yg"], writes=["yo"])
            k.op("dve", lambda e: e.tensor_tensor(out=yg[:], in0=yg[:], in1=yo[:], op=ALU.add), reads=["yg", "yo"], writes=["yg"])
            k.op("dve", lambda e: e.tensor_tensor(out=yg[:], in0=yg[:], in1=zz[:, 512 * g:512 * (g + 1)], op=ALU.mult), reads=["yg", zk], writes=["yg"])
            rms_rstd(ysq[:], yg[:], "yg", 512)
            k.op("dve", lambda e: e.scalar_tensor_tensor(out=ybf[:], in0=yg[:], scalar=ss[:, 0:1], in1=nwb[:, 512 * g:512 * (g + 1)], op0=ALU.mult, op1=ALU.mult),
                 reads=["yg", "ss", "nwb"], writes=["ybf"])
            pb, pbk = next_psb()
            for j in range(4):
                k.op("pe", lambda e, j=j: e.transpose(pb[:, j * 128:(j + 1) * 128], ybf[:, j * 128:(j + 1) * 128], ident[:]), reads=["ybf", "ident"], writes=[pbk])
            k.op("act", lambda e: e.copy(ystg[:], pb[:].rearrange("p (j t) -> p j t", j=4)), reads=[pbk], writes=["ystg"])
            k.dma("sp", YS[512 * g:512 * (g + 1), ti * 128:(ti + 1) * 128].rearrange("(j p) t -> p j t", p=128), ystg[:], reads=["ystg"], writes=["YS"])
        state_update(x_t, xk)
    s4.close()
    if stage <= 4:
        return nc, k

    s7 = Scope(nc, k)
    g1b = s7.sb([128, D], F32, "g1b")
    k.dma("sp", g1b[:], MODB[:, 2 * D:3 * D], writes=["g1b"])
    YPs = s7.sb([128, 16, 512], BF16, "YPs")
    YSs = s7.sb([128, 32, 512], BF16, "YSs")
    mgT = s7.sb([128, 16, 512], BF16, "mgT")
    wsb = s7.sb([128, 32, 512], BF16, "wsb")
    gpt = [s7.sb([128, 512], BF16, "gpt%d" % i) for i in range(2)]
    gst = [s7.sb([128, 512], BF16, "gst%d" % i) for i in range(2)]
    m1 = s7.sb([128, 512], F32, "m1")
    m2 = s7.sb([128, 512], F32, "m2")
    xres = [s7.sb([128, 512], F32, "xres%d" % i) for i in range(2)]
    hres = [s7.sb([128, 512], F32, "hres%d" % i) for i in range(2)]
    YPv = YP.rearrange("(c p) t -> p c t", p=128)
    YSv = YS.rearrange("(c p) t -> p c t", p=128)
    it7 = [0]
    for qt in range(4):
        t0 = qt * 512
        for c0 in range(0, 16, 4):
            k.dma("sp", YPs[:, c0:c0 + 4, :], YPv[:, c0:c0 + 4, t0:t0 + 512], writes=["YPs%d" % c0])
        for c0 in range(0, 32, 4):
            k.dma("sp", YSs[:, c0:c0 + 4, :], YSv[:, c0:c0 + 4, t0:t0 + 512], writes=["YSs%d" % c0])
        ypk = ["YPs%d" % c0 for c0 in range(0, 16, 4)]
        ysk = ["YSs%d" % c0 for c0 in range(0, 32, 4)]
        for fs in range(4):
            wp, wpk = load_slab(w_bp, fs * 512, 512, 16)
            wsk = []
            _, k1 = load_slab(w_bs, fs * 512, 512, 16, buf=wsb, key="wsb")
            wsk += k1
            src2 = w_bs[2048:4096, fs * 512:(fs + 1) * 512].rearrange("(kc p) n -> p kc n", p=128)
            for k0 in range(0, 16, 4):
                kk = "wsbh%d" % k0
                wsk.append(kk)
                k.dma("sp", wsb[:, 16 + k0:16 + k0 + 4, :], src2[:, k0:k0 + 4, :], writes=[kk])
            for fc in range(4):
                i = it7[0] % 2
                it7[0] += 1
                fr = (fs * 4 + fc) * 128
                k.dma("sp", gpt[i][:], GP[fr:fr + 128, t0:t0 + 512], writes=["gpt%d" % i])
                k.dma("sp", gst[i][:], GS[fr:fr + 128, t0:t0 + 512], writes=["gst%d" % i])
                psA, pak = next_ps()
                for kc in range(16):
                    k.mm(lambda e, kc=kc: e.matmul(psA[:], lhsT=wp[:, kc, fc * 128:(fc + 1) * 128], rhs=YPs[:, kc, :], start=(kc == 0), stop=(kc == 15)),
                         reads=wpk + ypk, writes=[pak], last=(kc == 15))
                psB, pbk_ = next_ps()
                for kc in range(32):
                    k.mm(lambda e, kc=kc: e.matmul(psB[:], lhsT=wsb[:, kc, fc * 128:(fc + 1) * 128], rhs=YSs[:, kc, :], start=(kc == 0), stop=(kc == 31)),
                         reads=wsk + ysk, writes=[pbk_], last=(kc == 31))
                k.op("dve", lambda e: e.tensor_tensor(out=m1[:], in0=psA[:], in1=gpt[i][:], op=ALU.mult), reads=[pak, "gpt%d" % i], writes=["m1"])
                k.op("dve", lambda e: e.tensor_tensor(out=m2[:], in0=psB[:], in1=gst[i][:], op=ALU.mult), reads=[pbk_, "gst%d" % i], writes=["m2"])
                k.op("dve", lambda e: e.tensor_tensor(out=mgT[:, fs * 4 + fc, :], in0=m1[:], in1=m2[:], op=ALU.add), reads=["m1", "m2"], writes=["mgT"])
        for fs in range(4):
            wo, wok = load_slab(w_out, fs * 512, 512, 16)
            for tt in range(4):
                i = it7[0] % 2
                it7[0] += 1
                tok0 = t0 + tt * 128
                k.dma("sp", xres[i][:], xh[128 + tok0:128 + tok0 + 128, fs * 512:(fs + 1) * 512], writes=["xres%d" % i])
                ps, pk = next_ps()
                for kc in range(16):
                    k.mm(lambda e, kc=kc: e.matmul(ps[:], lhsT=mgT[:, kc, tt * 128:(tt + 1) * 128], rhs=wo[:, kc, :], start=(kc == 0), stop=(kc == 15)),
                         reads=wok + ["mgT"], writes=[pk], last=(kc == 15))
                k.op("dve", lambda e: e.tensor_tensor(out=hres[i][:], in0=ps[:], in1=g1b[:, fs * 512:(fs + 1) * 512], op=ALU.mult), reads=[pk, "g1b"], writes=["hres%d" % i])
                k.op("dve", lambda e: e.tensor_tensor(out=hres[i][:], in0=hres[i][:], in1=xres[i][:], op=ALU.add), reads=["hres%d" % i, "xres%d" % i], writes=["hres%d" % i])
                k.dma("sp", H1[tok0:tok0 + 128, fs * 512:(fs + 1) * 512], hres[i][:], reads=["hres%d" % i], writes=["H1"])
                if dbg_out:
                    k.dma("sp", out[tok0:tok0 + 128, fs * 512:(fs + 1) * 512], hres[i][:], reads=["hres%d" % i], writes=["out"])
    s7.close()
    if stage <= 7:
        k.finish([])
        return nc, k

    s8 = Scope(nc, k)
    slot_i = s8.sb([128, 16, 4], I32, "slot_i")
    gate_k = s8.sb([128, 16, 4], F32, "gate_k")
    cntacc = s8.sb([128, NE], F32, "cntacc")
    ebase = s8.sb([128, NE], F32, "ebase")
    trash = s8.sb([128, 1], F32, "trash")
    brb = s8.sb([128, NE], F32, "brb")
    wr32 = s8.sb([128, 16, NE], F32, "wr32")
    whi = s8.sb([128, 16, NE], BF16, "whi")
    wlo = s8.sb([128, 16, NE], BF16, "wlo")
    k.dma("sp", ebase[:], ebase_in, writes=["ebase"])
    k.dma("sp", trash[:], trash_in, writes=["trash"])
    k.dma("sp", brb[:], b_router.partition_broadcast(128), writes=["brb"])
    k.dma("sp", wr32[:], w_router.rearrange("(kc p) e -> p kc e", p=128), writes=["wr32"])
    k.op("dve", lambda e: e.tensor_copy(whi[:], wr32[:]), reads=["wr32"], writes=["whi"])
    k.op("dve", lambda e: e.tensor_tensor(out=wr32[:], in0=wr32[:], in1=whi[:], op=ALU.subtract), reads=["wr32", "whi"], writes=["wr32"])
    k.op("dve", lambda e: e.tensor_copy(wlo[:], wr32[:]), reads=["wr32"], writes=["wlo"])
    k.op("pool", lambda e: e.memset(cntacc[:], 0.0), writes=["cntacc"])

    sA = Scope(nc, k)
    sh2 = sA.sb([128, D], F32, "sh2")
    sc2 = sA.sb([128, D], F32, "sc2")
    k.dma("sp", sh2[:], MODB[:, 3 * D:4 * D], writes=["sh2"])
    k.dma("sp", sc2[:], MODB[:, 4 * D:5 * D], writes=["sc2"])
    h1t = [sA.sb([128, D], F32, "h1t%d" % i) for i in range(2)]
    sqA = sA.sb([128, D], F32, "sqA")
    u2f = sA.sb([128, D], F32, "u2f")
    u2b = [sA.sb([128, D], BF16, "u2b%d" % i) for i in range(2)]
    ulo = sA.sb([128, D], BF16, "ulo")
    uTh = sA.sb([128, 16, 128], BF16, "uTh")
    uTl = sA.sb([128, 16, 128], BF16, "uTl")
    lg = sA.sb([128, NE], F32, "lg")
    m8 = sA.sb([128, 8], F32, "m8")
    mask = sA.sb([128, NE], F32, "mask")
    maskb = sA.sb([128, NE], BF16, "maskb")
    eg = sA.sb([128, NE], F32, "eg")
    rank = sA.sb([128, NE], F32, "rank")
    valid = sA.sb([128, NE], F32, "valid")
    inval = sA.sb([128, NE], F32, "inval")
    slotf = sA.sb([128, NE], F32, "slotf")
    gatev = sA.sb([128, NE], F32, "gatev")
    oh = sA.sb([128, NE], F32, "oh")
    t1 = sA.sb([128, NE], F32, "t1")
    sm1 = sA.sb([128, 1], F32, "sm1")
    negm = sA.sb([128, 1], F32, "negm")
    for ti in range(16):
        h_t = h1t[ti % 2]
        hk = "h1t%d" % (ti % 2)
        ub2 = u2b[ti % 2]
        ubk = "u2b%d" % (ti % 2)
        k.dma("sp", h_t[:], H1[ti * 128:(ti + 1) * 128, :], writes=[hk])
        rms_rstd(sqA[:], h_t[:], hk, D)
        k.op("dve", lambda e: e.scalar_tensor_tensor(out=u2f[:], in0=h_t[:], scalar=ss[:, 0:1], in1=sc2[:], op0=ALU.mult, op1=ALU.mult),
             reads=[hk, "ss", "sc2"], writes=["u2f"])
        k.op("dve", lambda e: e.tensor_tensor(out=u2f[:], in0=u2f[:], in1=sh2[:], op=ALU.add), reads=["u2f", "sh2"], writes=["u2f"])
        k.op("act", lambda e: e.copy(ub2[:], u2f[:]), reads=["u2f"], writes=[ubk])
        k.op("dve", lambda e: e.tensor_tensor(out=ulo[:], in0=u2f[:], in1=ub2[:], op=ALU.subtract), reads=["u2f", ubk], writes=["ulo"])
        for (srcb, srck, dstT, dstk) in ((ub2, ubk, uTh, "uTh"), (ulo, "ulo", uTl, "uTl")):
            for kq in range(4):
                pb, pbk = next_psb()
                for j in range(4):
                    k.op("pe", lambda e, j=j, srcb=srcb: e.transpose(pb[:, j * 128:(j + 1) * 128], srcb[:, (4 * kq + j) * 128:(4 * kq + j + 1) * 128], ident[:]),
                         reads=[srck, "ident"], writes=[pbk])
                k.op("act", lambda e, dstT=dstT: e.copy(dstT[:, 4 * kq:4 * kq + 4, :], pb[:].rearrange("p (j t) -> p j t", j=4)), reads=[pbk], writes=[dstk])
        ps, pk = next_ps()
        n_mm = 0
        for (aT, ak, wv, wk) in ((uTh, "uTh", whi, "whi"), (uTh, "uTh", wlo, "wlo"), (uTl, "uTl", whi, "whi")):
            for kc in range(16):
                n_mm += 1
                k.mm(lambda e, kc=kc, aT=aT, wv=wv, n_mm=n_mm: e.matmul(ps[:, 0:NE], lhsT=aT[:, kc, :], rhs=wv[:, kc, :], start=(n_mm == 1), stop=(n_mm == 48)),
                     reads=[ak, wk], writes=[pk], last=(n_mm == 48))
        k.op("dve", lambda e: e.tensor_tensor(out=lg[:], in0=ps[:, 0:NE], in1=brb[:], op=ALU.add), reads=[pk, "brb"], writes=["lg"])
        k.op("dve", lambda e: e.max(m8[:], lg[:]), reads=["lg"], writes=["m8"])
        k.op("dve", lambda e: e.tensor_scalar(out=mask[:], in0=lg[:], scalar1=m8[:, 3:4], scalar2=None, op0=ALU.is_ge), reads=["lg", "m8"], writes=["mask"])
        k.op("dve", lambda e: e.tensor_scalar(out=negm[:], in0=m8[:, 0:1], scalar1=-1.0, scalar2=None, op0=ALU.mult), reads=["m8"], writes=["negm"])
        k.op("act", lambda e: e.activation(out=eg[:], in_=lg[:], func=AF.Exp, bias=negm[:, 0:1], scale=1.0), reads=["lg", "negm"], writes=["eg"])
        k.op("dve", lambda e: e.tensor_tensor(out=eg[:], in0=eg[:], in1=mask[:], op=ALU.mult), reads=["eg", "mask"], writes=["eg"])
        k.op("dve", lambda e: e.reduce_sum(out=sm1[:], in_=eg[:], axis=AX.X), reads=["eg"], writes=["sm1"])
        k.op("dve", lambda e: e.reciprocal(sm1[:], sm1[:]), reads=["sm1"], writes=["sm1"])
        k.op("dve", lambda e: e.tensor_copy(maskb[:], mask[:]), reads=["mask"], writes=["maskb"])
        psr, prk = next_ps()
        k.mm(lambda e: e.matmul(psr[:, 0:NE], lhsT=ustr[:], rhs=maskb[:], start=True, stop=True), reads=["ustr", "maskb"], writes=[prk], last=True)
        k.op("dve", lambda e: e.tensor_tensor(out=rank[:], in0=psr[:, 0:NE], in1=cntacc[:], op=ALU.add), reads=[prk, "cntacc"], writes=["rank"])
        psc, pck = next_ps()
        k.mm(lambda e: e.matmul(psc[:, 0:NE], lhsT=onesb[:], rhs=maskb[:], start=True, stop=True), reads=["onesb", "maskb"], writes=[pck], last=True)
        k.op("dve", lambda e: e.tensor_tensor(out=cntacc[:], in0=cntacc[:], in1=psc[:, 0:NE], op=ALU.add), reads=[pck, "cntacc", "rank"], writes=["cntacc"])
        k.op("dve", lambda e: e.tensor_scalar(out=valid[:], in0=rank[:], scalar1=float(CAP), scalar2=None, op0=ALU.is_lt), reads=["rank"], writes=["valid"])
        k.op("dve", lambda e: e.tensor_tensor(out=valid[:], in0=valid[:], in1=mask[:], op=ALU.mult), reads=["valid", "mask"], writes=["valid"])
        k.op("dve", lambda e: e.tensor_scalar(out=inval[:], in0=valid[:], scalar1=-1.0, scalar2=1.0, op0=ALU.mult, op1=ALU.add), reads=["valid"], writes=["inval"])
        k.op("dve", lambda e: e.tensor_tensor(out=slotf[:], in0=rank[:], in1=ebase[:], op=ALU.add), reads=["rank", "ebase"], writes=["slotf"])
        k.op("dve", lambda e: e.tensor_tensor(out=slotf[:], in0=slotf[:], in1=valid[:], op=ALU.mult), reads=["slotf", "valid"], writes=["slotf"])
        k.op("dve", lambda e: e.scalar_tensor_tensor(out=slotf[:], in0=inval[:], scalar=trash[:, 0:1], in1=slotf[:], op0=ALU.mult, op1=ALU.add),
             reads=["inval", "trash", "slotf"], writes=["slotf"])
        k.op("dve", lambda e: e.tensor_tensor(out=gatev[:], in0=eg[:], in1=valid[:], op=ALU.mult), reads=["eg", "valid"], writes=["gatev"])
        k.op("dve", lambda e: e.tensor_scalar(out=gatev[:], in0=gatev[:], scalar1=sm1[:, 0:1], scalar2=None, op0=ALU.mult), reads=["gatev", "sm1"], writes=["gatev"])
        for kk in range(4):
            k.op("dve", lambda e, kk=kk: e.tensor_scalar(out=oh[:], in0=lg[:], scalar1=m8[:, kk:kk + 1], scalar2=None, op0=ALU.is_equal), reads=["lg", "m8"], writes=["oh"])
            k.op("dve", lambda e: e.tensor_tensor(out=t1[:], in0=oh[:], in1=slotf[:], op=ALU.mult), reads=["oh", "slotf"], writes=["t1"])
            k.op("dve", lambda e: e.reduce_sum(out=sm1[:], in_=t1[:], axis=AX.X), reads=["t1", "gatev"], writes=["sm1"])
            k.op("dve", lambda e, kk=kk: e.tensor_copy(slot_i[:, ti, kk:kk + 1], sm1[:]), reads=["sm1"], writes=["slot_i"])
            k.op("dve", lambda e: e.tensor_tensor(out=t1[:], in0=oh[:], in1=gatev[:], op=ALU.mult), reads=["oh", "gatev"], writes=["t1"])
            k.op("dve", lambda e, kk=kk: e.reduce_sum(out=gate_k[:, ti, kk:kk + 1], in_=t1[:], axis=AX.X), reads=["t1"], writes=["gate_k"])
            k.dma_custom("pool", lambda e, kk=kk: e.indirect_dma_start(
                out=XS[:, :], out_offset=bass.IndirectOffsetOnAxis(ap=slot_i[:, ti, kk:kk + 1], axis=0),
                in_=ub2[:, :], in_offset=None), reads=[ubk, "slot_i"], writes=["XS"])
    sA.close()

    sB = Scope(nc, k)
    Xg = sB.sb([128, NB, D], BF16, "Xg")
    XeT = sB.sb([128, 16, CAP], BF16, "XeT")
    actT = sB.sb([128, 16, CAP], BF16, "actT")
    bgu = sB.sb([128, NE, 32], F32, "bgu")
    k.dma("sp", bgu[:], b_guT, writes=["bgu"])
    gsb = sB.sb([128, 512], F32, "gsb")
    usb = sB.sb([128, 512], F32, "usb")
    sgb = sB.sb([128, 512], F32, "sgb")
    bdn_b = [sB.sb([128, 512], F32, "bdn%d" % i) for i in range(2)]
    yout = [sB.sb([128, 512], F32, "yout%d" % i) for i in range(2)]
    yi = [0]
    ppe_gu = D // (4 * piece_rows(8 * D, 4096))
    ppe_dn = D // (4 * piece_rows(8 * D, D))
    for ex in range(NE):
        XSv = XS[ex * CAP:(ex + 1) * CAP, :].rearrange("(b p) d -> p b d", p=128)
        for bq in range(NB):
            k.dma("sp", Xg[:, bq, :], XSv[:, bq, :], writes=["Xg%d" % bq])
        for blk in range(NB):
            for kq in range(4):
                pb, pbk = next_psb()
                for j in range(4):
                    k.op("pe", lambda e, j=j: e.transpose(pb[:, j * 128:(j + 1) * 128], Xg[:, blk, (4 * kq + j) * 128:(4 * kq + j + 1) * 128], ident[:]),
                         reads=["Xg%d" % blk, "ident"], writes=[pbk])
                k.op("act", lambda e: e.copy(XeT[:, 4 * kq:4 * kq + 4, blk * 128:(blk + 1) * 128], pb[:].rearrange("p (j t) -> p j t", j=4)),
                     reads=[pbk], writes=["XeT"])
        for s in range(4):
            wg, wgk = load_slab(w_gu_p[ex // 8], s * 512, 512, 16, row0=(ex % 8) * D, cc=t_gu[(ex + 1) * ppe_gu - 1])
            wu, wuk = load_slab(w_gu_p[ex // 8], 2048 + s * 512, 512, 16, row0=(ex % 8) * D, cc=t_gu[(ex + 1) * ppe_gu - 1])
            for fc in range(4):
                f = s * 4 + fc
                for (c0, cn) in ((0, 512), (512, CAP - 512)):
                    psG, pgk = next_ps()
                    for kc in range(16):
                        k.mm(lambda e, kc=kc: e.matmul(psG[:, 0:cn], lhsT=wg[:, kc, fc * 128:(fc + 1) * 128], rhs=XeT[:, kc, c0:c0 + cn], start=(kc == 0), stop=(kc == 15)),
                             reads=wgk + ["XeT"], writes=[pgk], last=(kc == 15))
                    psU, puk = next_ps()
                    for kc in range(16):
                        k.mm(lambda e, kc=kc: e.matmul(psU[:, 0:cn], lhsT=wu[:, kc, fc * 128:(fc + 1) * 128], rhs=XeT[:, kc, c0:c0 + cn], start=(kc == 0), stop=(kc == 15)),
                             reads=wuk + ["XeT"], writes=[puk], last=(kc == 15))
                    k.op("dve", lambda e: e.tensor_scalar(out=gsb[:, 0:cn], in0=psG[:, 0:cn], scalar1=bgu[:, ex, f:f + 1], scalar2=SWL, op0=ALU.add, op1=ALU.min),
                         reads=[pgk, "bgu"], writes=["gsb"])
                    k.op("act", lambda e: e.activation(out=sgb[:, 0:cn], in_=gsb[:, 0:cn], func=AF.Sigmoid, scale=SWA), reads=["gsb"], writes=["sgb"])
                    k.op("dve", lambda e: e.tensor_scalar(out=usb[:, 0:cn], in0=psU[:, 0:cn], scalar1=bgu[:, ex, 16 + f:17 + f], scalar2=SWL, op0=ALU.add, op1=ALU.min),
                         reads=[puk, "bgu"], writes=["usb"])
                    k.op("dve", lambda e: e.tensor_scalar(out=usb[:, 0:cn], in0=usb[:, 0:cn], scalar1=-SWL, scalar2=1.0, op0=ALU.max, op1=ALU.add), reads=["usb"], writes=["usb"])
                    k.op("dve", lambda e: e.tensor_tensor(out=gsb[:, 0:cn], in0=gsb[:, 0:cn], in1=sgb[:, 0:cn], op=ALU.mult), reads=["gsb", "sgb"], writes=["gsb"])
                    k.op("dve", lambda e: e.tensor_tensor(out=actT[:, f, c0:c0 + cn], in0=gsb[:, 0:cn], in1=usb[:, 0:cn], op=ALU.mult), reads=["gsb", "usb"], writes=["actT"])
        for ds in range(4):
            wd, wdk = load_slab(w_dn_p[ex // 8], ds * 512, 512, 16, row0=(ex % 8) * D, cc=t_dn[(ex + 1) * ppe_dn - 1])
            bd_ = bdn_b[ds % 2]
            bdk = "bdn%d" % (ds % 2)
            k.dma("sp", bd_[:], b_dn[ex:ex + 1, ds * 512:(ds + 1) * 512].partition_broadcast(128), writes=[bdk])
            for blk in range(NB):
                ps, pk = next_ps()
                for kc in range(16):
                    k.mm(lambda e, kc=kc: e.matmul(ps[:], lhsT=actT[:, kc, blk * 128:(blk + 1) * 128], rhs=wd[:, kc, :], start=(kc == 0), stop=(kc == 15)),
                         reads=wdk + ["actT"], writes=[pk], last=(kc == 15))
                yo_ = yout[yi[0] % 2]
                yk_ = "yout%d" % (yi[0] % 2)
                yi[0] += 1
                k.op("dve", lambda e: e.tensor_tensor(out=yo_[:], in0=ps[:], in1=bd_[:], op=ALU.add), reads=[pk, bdk], writes=[yk_])
                r0 = ex * CAP + blk * 128
                Yd = YSLa if ds < 2 else YSLb
                k.dma("sp", Yd[r0:r0 + 128, (ds % 2) * 512:(ds % 2 + 1) * 512], yo_[:], reads=[yk_], writes=["YSL"])
    sB.close()

    sC = Scope(nc, k)
    g2b = sC.sb([128, D], F32, "g2b")
    fnwb = sC.sb([128, D], F32, "fnwb")
    k.dma("sp", g2b[:], MODB[:, 5 * D:6 * D], writes=["g2b"])
    k.dma("sp", fnwb[:], fnw.partition_broadcast(128), writes=["fnwb"])
    h1c = [sC.sb([128, D], F32, "h1c%d" % i) for i in range(2)]
    Yg = [sC.sb([128, D], F32, "Yg%d" % i) for i in range(2)]
    accm = sC.sb([128, D], F32, "accm")
    sqC = sC.sb([128, D], F32, "sqC")
    outt = sC.sb([128, D], F32, "outt")
    gi_ = [0]
    for ti in range(16):
        h_t = h1c[ti % 2]
        hk = "h1c%d" % (ti % 2)
        k.dma("sp", h_t[:], H1[ti * 128:(ti + 1) * 128, :], writes=[hk])
        for kk in range(4):
            yg_ = Yg[gi_[0] % 2]
            ygk = "Yg%d" % (gi_[0] % 2)
            gi_[0] += 1
            k.dma_custom("pool", lambda e, kk=kk, yg_=yg_: e.indirect_dma_start(
                out=yg_[:, 0:D // 2], out_offset=None, in_=YSLa[:, :],
                in_offset=bass.IndirectOffsetOnAxis(ap=slot_i[:, ti, kk:kk + 1], axis=0)), reads=["slot_i"], writes=[ygk])
            k.dma_custom("pool", lambda e, kk=kk, yg_=yg_: e.indirect_dma_start(
                out=yg_[:, D // 2:D], out_offset=None, in_=YSLb[:, :],
                in_offset=bass.IndirectOffsetOnAxis(ap=slot_i[:, ti, kk:kk + 1], axis=0)), reads=["slot_i"], writes=[ygk + "b"])
            if kk == 0:
                k.op("dve", lambda e, yg_=yg_: e.tensor_scalar(out=accm[:], in0=yg_[:], scalar1=gate_k[:, ti, 0:1], scalar2=None, op0=ALU.mult),
                     reads=[ygk, ygk + "b", "gate_k"], writes=["accm"])
            else:
                k.op("dve", lambda e, kk=kk, yg_=yg_: e.scalar_tensor_tensor(out=accm[:], in0=yg_[:], scalar=gate_k[:, ti, kk:kk + 1], in1=accm[:],
                                                                             op0=ALU.mult, op1=ALU.add), reads=[ygk, ygk + "b", "gate_k", "accm"], writes=["accm"])
        k.op("dve", lambda e: e.tensor_tensor(out=accm[:], in0=accm[:], in1=g2b[:], op=ALU.mult), reads=["accm", "g2b"], writes=["accm"])
        k.op("dve", lambda e: e.tensor_tensor(out=accm[:], in0=accm[:], in1=h_t[:], op=ALU.add), reads=["accm", hk], writes=["accm"])
        rms_rstd(sqC[:], accm[:], "accm", D)
        k.op("dve", lambda e: e.scalar_tensor_tensor(out=outt[:], in0=accm[:], scalar=ss[:, 0:1], in1=fnwb[:], op0=ALU.mult, op1=ALU.mult),
             reads=["accm", "ss", "fnwb"], writes=["outt"])
        k.dma("sp", out[ti * 128:(ti + 1) * 128, :], outt[:], reads=["outt"], writes=["out"])
    sC.close()
    s8.close()
    k.finish([])
    return nc, k


def host_inputs(inputs, full=True):
    f = lambda a: np.ascontiguousarray(np.asarray(a, dtype=np.float32))
    x = f(inputs["x"])
    c = f(inputs["c"])
    def quarters(W2d, pr=None):
        R, C = W2d.shape
        rq = 1
        while rq * 2 * C * 2 <= (1 << 20) and (R // 4) % (rq * 2) == 0:
            rq *= 2
        pr = 4 * rq
        Wr = W2d.reshape(R // pr, 4, pr // 4, C)
        NP = (R // 4) // rq
        F = rq * C // 128
        return [np.ascontiguousarray(Wr[:, q].reshape(NP * 128, F)) for q in range(4)]

    big = {
        "w_ada_q": quarters(f(inputs["w_ada"][0]), 512), "w_in_q": quarters(f(inputs["w_in"][0]), 512),
        "pool_w_q": quarters(f(inputs["pool_w"][0]).reshape(2048, 512), 2048),
        "w_bp_q": quarters(f(inputs["w_branch_pool"][0]), 2048), "w_bs_q": quarters(f(inputs["w_branch_ssd"][0]), 4096),
        "w_out_q": quarters(f(inputs["w_out"][0]), 2048),
    }
    if full:
        wgu = np.asarray(inputs["w_gate_up"][0], dtype=np.float32)
        wdn = np.asarray(inputs["w_down"][0], dtype=np.float32)
        for gq in range(4):
            big["w_gu%d_q" % gq] = quarters(wgu[8 * gq:8 * gq + 8].reshape(8 * D, 4096))
            big["w_dn%d_q" % gq] = quarters(wdn[8 * gq:8 * gq + 8].reshape(8 * D, D))
    shared = {
        "b_ada": f(inputs["b_ada"][0]).reshape(1, -1),
        "pool_scaleT": f(np.asarray(inputs["pool_scale"][0]).reshape(16, 128).T),
        "cwT": f(np.asarray(inputs["conv_w"][0]).reshape(4, 48, 128).transpose(2, 1, 0)),
        "cbT": f(np.asarray(inputs["conv_b"][0]).reshape(48, 128).T),
        "dt_bias": f(inputs["dt_bias"][0]).reshape(1, 64), "a_log": f(inputs["a_log"][0]).reshape(1, 64),
        "d_skip": f(inputs["d_skip"][0]).reshape(1, 64), "ssd_norm_w": f(inputs["ssd_norm_w"][0]).reshape(1, 4096),
        "w_router": f(inputs["w_router"][0]), "b_router": f(inputs["b_router"][0]).reshape(1, NE),
        "b_guT": f(np.asarray(inputs["b_gate_up"][0]).reshape(NE, 32, 128).transpose(2, 0, 1)),
        "b_dn": f(inputs["b_down"][0]),
        "fnw": f(inputs["final_norm_w"]).reshape(1, D),
        "ebase": f(np.broadcast_to((np.arange(NE) * CAP)[None, :], (128, NE))),
        "trash": f((NSLOT + np.arange(128)).reshape(128, 1)),
    }
    maps = []
    for cid in range(NCORES):
        b, q = cid // 4, cid % 4
        xh = np.zeros((TT, D), np.float32)
        xh[HT:] = x[b, q * T:(q + 1) * T]
        if q > 0:
            xh[:HT] = x[b, q * T - HT:q * T]
        pos = np.arange(q * T + 1, (q + 1) * T + 1, dtype=np.float32)
        invcnt = np.stack([1.0 / np.minimum(pos, float(w)) for w in (2, 4, 8, 16)]).astype(np.float32)
        cm = np.zeros((128, 16), np.float32)
        cfv = np.zeros((128, 4), np.float32)
        for j in range(4):
            cfv[:, j] = 1.0 if j < q else 0.0
            for m in range(4):
                cm[:, 4 * j + m] = 1.0 if (j < m < q) else 0.0
        d = dict(shared)
        for kk, v in big.items():
            d[kk] = v[q]
        d.update({
            "xh": xh, "cT": f(c[b].reshape(16, 128).T), "hmask": np.full((128, 1), 1.0 if q > 0 else 0.0, np.float32),
            "invcnt": invcnt, "cm": cm, "cf": cfv,
        })
        maps.append(d)
    return maps


_CACHE = {}


def kernel(**inputs):
    maps = host_inputs(inputs)
    if "nc" not in _CACHE:
        _CACHE["nc"] = build()[0]
    res = run_bass_kernel_spmd(_CACHE["nc"], maps, core_ids=list(range(NCORES)))
    outs = [np.asarray(r["out"]) for r in res.results]
    o = np.stack(outs).reshape(2, 4 * T, D).astype(np.float32)
    return o
```

```python
import numpy as np
import concourse.bass as bass
import concourse.mybir as mybir

F32 = mybir.dt.float32
BF16 = mybir.dt.bfloat16
I32 = mybir.dt.int32
U32 = mybir.dt.uint32
AF = mybir.ActivationFunctionType
ALU = mybir.AluOpType
AX = mybir.AxisListType


class K:
    def __init__(self, nc, n_dma_sems=72, same_engine_sync=True):
        self.nc = nc
        self.eng = {"pe": nc.tensor, "act": nc.scalar, "dve": nc.vector, "pool": nc.gpsimd, "sp": nc.sync}
        self.sem = {e: nc.alloc_semaphore("c_" + e) for e in self.eng}
        self.cnt = {e: 0 for e in self.eng}
        self.seen = {e: {} for e in self.eng}
        self.dsem = [nc.alloc_semaphore("d%d" % i) for i in range(n_dma_sems)]
        self.dcnt = [0] * n_dma_sems
        self.dnext = 0
        self.last_w = {}
        self.readers = {}
        self.pe_pending_reads = []
        self.pe_pending_writes = []
        self.same_engine_sync = same_engine_sync
        self.n_wait = 0
        self.n_ins = 0

    def _wait_tok(self, e, tok):
        if tok is None:
            return
        kind = tok[0]
        if kind == "eng":
            _, e2, c = tok
            if e2 == e and (e == "pe" or not self.same_engine_sync):
                return
            key = ("e", e2)
            if self.seen[e].get(key, 0) >= c:
                return
            self.eng[e].wait_ge(self.sem[e2], c)
            self.seen[e][key] = c
            self.n_wait += 1
        elif kind == "cc":
            _, c = tok
            key = ("cc",)
            if self.seen[e].get(key, 0) >= c:
                return
            self.eng[e].wait_ge(self.cc_sem, c)
            self.seen[e][key] = c
            self.n_wait += 1
        else:
            _, i, c = tok
            key = ("d", i)
            if self.seen[e].get(key, 0) >= c:
                return
            self.eng[e].wait_ge(self.dsem[i], c)
            self.seen[e][key] = c
            self.n_wait += 1

    def _deps(self, e, reads, writes):
        for r in reads:
            self._wait_tok(e, self.last_w.get(r))
        for w in writes:
            self._wait_tok(e, self.last_w.get(w))
            for t in self.readers.get(w, ()):
                self._wait_tok(e, t)

    def _commit(self, tok, reads, writes):
        for r in reads:
            self.readers.setdefault(r, []).append(tok)
        for w in writes:
            self.last_w[w] = tok
            self.readers[w] = []

    def op(self, e, fn, reads=(), writes=()):
        self._deps(e, reads, writes)
        ins = fn(self.eng[e])
        ins.then_inc(self.sem[e], 1)
        self.cnt[e] += 1
        self.n_ins += 1
        tok = ("eng", e, self.cnt[e])
        if e == "pe":
            self._flush_pe(tok)
        self._commit(tok, reads, writes)
        return tok

    def _flush_pe(self, tok):
        if self.pe_pending_reads or self.pe_pending_writes:
            self._commit(tok, self.pe_pending_reads, self.pe_pending_writes)
            self.pe_pending_reads = []
            self.pe_pending_writes = []

    def mm(self, fn, reads=(), writes=(), last=False):
        for w in writes:
            if w in self.pe_pending_writes:
                continue
            self._wait_tok("pe", self.last_w.get(w))
            for t in self.readers.get(w, ()):
                self._wait_tok("pe", t)
        for r in reads:
            self._wait_tok("pe", self.last_w.get(r))
        ins = fn(self.eng["pe"])
        self.n_ins += 1
        for r in reads:
            if r not in self.pe_pending_reads:
                self.pe_pending_reads.append(r)
        for w in writes:
            if w not in self.pe_pending_writes:
                self.pe_pending_writes.append(w)
        if last:
            ins.then_inc(self.sem["pe"], 1)
            self.cnt["pe"] += 1
            tok = ("eng", "pe", self.cnt["pe"])
            self._flush_pe(tok)
            return tok
        return None

    def dma(self, q, out, in_, reads=(), writes=(), **kw):
        self._deps(q, reads, writes)
        i = self.dnext
        self.dnext = (self.dnext + 1) % len(self.dsem)
        self._wait_tok(q, ("dma", i, self.dcnt[i])) if self.dcnt[i] else None
        ins = self.eng[q].dma_start(out=out, in_=in_, **kw)
        ins.then_inc(self.dsem[i], 16)
        self.dcnt[i] += 16
        self.n_ins += 1
        tok = ("dma", i, self.dcnt[i])
        self._commit(tok, reads, writes)
        return tok

    def dma_custom(self, q, fn, reads=(), writes=(), inc=16):
        self._deps(q, reads, writes)
        i = self.dnext
        self.dnext = (self.dnext + 1) % len(self.dsem)
        self._wait_tok(q, ("dma", i, self.dcnt[i])) if self.dcnt[i] else None
        ins = fn(self.eng[q])
        ins.then_inc(self.dsem[i], inc)
        self.dcnt[i] += inc
        self.n_ins += 1
        tok = ("dma", i, self.dcnt[i])
        self._commit(tok, reads, writes)
        return tok

    def finish(self, keys):
        for k_ in keys:
            self._wait_tok("sp", self.last_w.get(k_))
        for i, c in enumerate(self.dcnt):
            if c:
                self._wait_tok("sp", ("dma", i, c))


from contextlib import ExitStack
from concourse.bass_utils import run_bass_kernel_spmd

NCORES = 8
T = 2048
NT = 16
D = 2048
HT = 128
TT = T + HT
EPS = 1e-6
NE = 32
CAP = 768
NB = CAP // 128
NSLOT = NE * CAP
SWA = 1.702
SWL = 7.0


def barrier(k):
    toks = []
    for e in k.eng:
        if k.cnt[e]:
            toks.append(("eng", e, k.cnt[e]))
    for i, c in enumerate(k.dcnt):
        if c:
            toks.append(("dma", i, c))
    for e in k.eng:
        for t in toks:
            if t[0] == "eng" and t[1] == e:
                continue
            k._wait_tok(e, t)


class Scope:
    def __init__(self, nc, k):
        self.nc, self.k, self.es = nc, k, ExitStack()

    def sb(self, shape, dt, name):
        Scope.uid += 1
        return self.es.enter_context(self.nc.sbuf_tensor("%s_%d" % (name, Scope.uid), shape, dt))

    def close(self):
        barrier(self.k)
        self.es.close()


Scope.uid = 0


def build(stage=99, dbg_out=False):
    nc = bass.Bass("TRN2", target_bir_lowering=False)
    din = lambda n, s, dt=F32: nc.dram_tensor(n, s, dt, kind="ExternalInput").ap()
    xh = din("xh", [TT, D])
    cT = din("cT", [128, 16])
    hmask = din("hmask", [128, 1])
    invcnt = din("invcnt", [4, T])
    cm = din("cm", [128, 16])
    cf = din("cf", [128, 4])
    b_ada = din("b_ada", [1, 6 * D])
    pool_scaleT = din("pool_scaleT", [128, 16])
    cwT = din("cwT", [128, 48, 4])
    cbT = din("cbT", [128, 48])
    dt_bias = din("dt_bias", [1, 64])
    a_log = din("a_log", [1, 64])
    d_skip = din("d_skip", [1, 64])
    ssd_norm_w = din("ssd_norm_w", [1, 4096])
    if stage > 7:
        b_guT = din("b_guT", [128, NE, 32])
        b_dn = din("b_dn", [NE, D])
        w_router = din("w_router", [D, NE])
        b_router = din("b_router", [1, NE])
        fnw = din("fnw", [1, D])
        ebase_in = din("ebase", [128, NE])
        trash_in = din("trash", [128, 1])
    out = nc.dram_tensor("out", [T, D], F32, kind="ExternalOutput").ap()

    dk = "Internal"
    MODB = nc.dram_tensor("MODB", [128, 6 * D], F32, kind=dk).ap()
    YP = nc.dram_tensor("YP", [D, T], BF16, kind=dk).ap()
    GP = nc.dram_tensor("GP", [D, T], BF16, kind=dk).ap()
    GS = nc.dram_tensor("GS", [D, T], BF16).ap()
    ZT = nc.dram_tensor("ZT", [T, 4096], BF16, kind=dk).ap()
    XTOK = nc.dram_tensor("XTOK", [T, 5120], BF16, kind=dk).ap()
    BCT = nc.dram_tensor("BCT", [2048, T], BF16, kind=dk).ap()
    YS = nc.dram_tensor("YS", [4096, T], BF16, kind=dk).ap()
    H1 = nc.dram_tensor("H1", [T, D], F32).ap()
    XS = nc.dram_tensor("XS", [NSLOT + 128, D], BF16).ap()
    YSLa = nc.dram_tensor("YSLa", [NSLOT + 128, D // 2], F32).ap()
    YSLb = nc.dram_tensor("YSLb", [NSLOT + 128, D // 2], F32).ap()

    k = K(nc)
    P = Scope(nc, k)
    k.cc_sem = nc.alloc_semaphore("cc_sem")
    cc_n = [0]
    RG = [[0, 1, 2, 3], [4, 5, 6, 7]]

    def piece_rows(R, C):
        rq = 1
        while rq * 2 * C * 2 <= (1 << 20) and (R // 4) % (rq * 2) == 0:
            rq *= 2
        return rq

    CW = 1028
    cf32 = [P.sb([128, CW], F32, "cf32_%d" % i) for i in range(2)]
    cbf = [P.sb([128, CW], BF16, "cbf_%d" % i) for i in range(2)]
    cu = [0]

    def cast_weight(name, R, C, lazy=False):
        rq = piece_rows(R, C)
        NP = (R // 4) // rq
        F = rq * C // 128
        nch = -(-F // CW)
        assert F % nch == 0
        fw_ = F // nch
        qin = nc.dram_tensor(name + "_q", [NP * 128, F], F32, kind="ExternalInput")
        bnc = nc.dram_tensor(name + "_b", [NP * 128, F], BF16)
        full = nc.dram_tensor(name + "_f", [R, C], BF16)
        units = [(i, c) for i in range(NP) for c in range(nch)]
        base = cu[0]
        cu[0] += len(units)

        def load(u):
            i, c = units[u]
            bi = (base + u) % 2
            k.dma("pool", cf32[bi][:, 0:fw_], qin[i * 128:(i + 1) * 128, c * fw_:(c + 1) * fw_], writes=["cf32_%d" % bi])

        def unit(u):
            i, c = units[u]
            bi = (base + u) % 2
            if u == 0:
                for u2 in range(min(2, len(units))):
                    load(u2)
            k.op("pool", lambda e: e.tensor_copy(cbf[bi][:, 0:fw_], cf32[bi][:, 0:fw_]), reads=["cf32_%d" % bi], writes=["cbf_%d" % bi])
            k.dma("pool", bnc[i * 128:(i + 1) * 128, c * fw_:(c + 1) * fw_], cbf[bi][:, 0:fw_], reads=["cbf_%d" % bi], writes=["%s_b%d_%d" % (name, i, c)])
            if u + 2 < len(units):
                load(u + 2)

        for u in range(len(units)):
            if lazy:
                lazyq.append(lambda u=u: unit(u))
            else:
                unit(u)
        return dict(name=name, rq=rq, NP=NP, nch=nch, bnc=bnc, full=full)

    lazyq = []

    def pump(n):
        while n > 0 and lazyq:
            lazyq.pop(0)()
            n -= 1

    def gather_weight(cw_):
        toks = []
        name, rq = cw_["name"], cw_["rq"]
        for i in range(cw_["NP"]):
            k._deps("pool", ["%s_b%d_%d" % (name, i, c) for c in range(cw_["nch"])], [])
            nc.gpsimd.collective_compute("AllGather", ALU.bypass, replica_groups=RG,
                                         ins=[cw_["bnc"][i * 128:(i + 1) * 128, :].opt()],
                                         outs=[cw_["full"][i * 4 * rq:(i + 1) * 4 * rq, :].opt()]).then_inc(k.cc_sem)
            cc_n[0] += 1
            toks.append(cc_n[0])
        return cw_["full"].ap(), toks

    def dist_weight(name, R, C):
        return gather_weight(cast_weight(name, R, C))

    PS = [nc.alloc_psum_tensor("ps%d" % i, [128, 512], F32) for i in range(5)]
    PSB = [nc.alloc_psum_tensor("psb%d" % i, [128, 512], BF16) for i in range(2)]
    PSY = nc.alloc_psum_tensor("psy", [128, 512], F32)
    psi = [0]

    def next_ps():
        i = psi[0] % len(PS)
        psi[0] += 1
        return PS[i], "ps%d" % i

    psbi = [0]

    def next_psb():
        i = psbi[0] % len(PSB)
        psbi[0] += 1
        return PSB[i], "psb%d" % i

    identf = P.sb([128, 128], F32, "identf")
    ident = P.sb([128, 128], BF16, "ident")
    tri = P.sb([128, 128], F32, "tri")
    ustr = P.sb([128, 128], BF16, "ustr")
    onesf = P.sb([128, 128], F32, "onesf")
    onesb = P.sb([128, 128], BF16, "onesb")
    ss = P.sb([128, 1], F32, "ss")
    dt_all = P.sb([128, 16, 64], F32, "dt_all")
    k.op("pool", lambda e: e.memset(identf[:], 1.0), writes=["identf"])
    k.op("pool", lambda e: e.affine_select(out=identf[:], in_=identf[:], pattern=[[-1, 128]],
                                           compare_op=ALU.is_equal, fill=0.0, base=0, channel_multiplier=1),
         reads=["identf"], writes=["identf"])
    k.op("dve", lambda e: e.tensor_copy(ident[:], identf[:]), reads=["identf"], writes=["ident"])
    k.op("pool", lambda e: e.memset(tri[:], 1.0), writes=["tri"])
    k.op("pool", lambda e: e.affine_select(out=tri[:], in_=tri[:], pattern=[[1, 128]],
                                           compare_op=ALU.is_ge, fill=0.0, base=0, channel_multiplier=-1),
         reads=["tri"], writes=["tri"])
    k.op("pool", lambda e: e.memset(onesf[:], 1.0), writes=["onesf"])
    k.op("pool", lambda e: e.affine_select(out=onesf[:], in_=onesf[:], pattern=[[1, 128]],
                                           compare_op=ALU.is_gt, fill=0.0, base=0, channel_multiplier=-1),
         reads=["onesf"], writes=["onesf"])
    k.op("dve", lambda e: e.tensor_copy(ustr[:], onesf[:]), reads=["onesf"], writes=["ustr"])
    k.op("pool", lambda e: e.memset(onesf[:], 1.0), reads=["ustr"], writes=["onesf"])
    k.op("dve", lambda e: e.tensor_copy(onesb[:], onesf[:]), reads=["onesf"], writes=["onesb"])

    wslab = [P.sb([128, 16, 512], BF16, "wslab%d" % i) for i in range(2)]
    wsi = [0]

    def load_slab(W2d, col0, ncols, KC, buf=None, key=None, row0=0, cc=None):
        if buf is None:
            i = wsi[0] % 2
            wsi[0] += 1
            buf = wslab[i]
            key = "wslab%d" % i
        src = W2d[row0:row0 + KC * 128, col0:col0 + ncols].rearrange("(kc p) n -> p kc n", p=128)
        keys = []
        if cc is None:
            cc = WT[id(W2d)]
        k._wait_tok("sp", ("cc", cc))
        for k0 in range(0, KC, 4):
            kk = key if k0 == 0 else key + "x%d" % k0
            keys.append(kk)
            k.dma("sp", buf[:, k0:k0 + 4, 0:ncols], src[:, k0:k0 + 4, :], writes=[kk])
        return buf, keys

    def rms_rstd(sq, src, srck, n):
        k.op("act", lambda e: e.activation(out=sq, in_=src, func=AF.Square), reads=[srck], writes=["sq"])
        k.op("dve", lambda e: e.reduce_sum(out=ss[:], in_=sq, axis=AX.X), reads=["sq"], writes=["ss"])
        k.op("act", lambda e: e.activation(out=ss[:], in_=ss[:], func=AF.Sqrt, scale=1.0 / n, bias=EPS), reads=["ss"], writes=["ss"])
        k.op("dve", lambda e: e.reciprocal(ss[:], ss[:]), reads=["ss"], writes=["ss"])

    zs = Scope(nc, k)
    zb = zs.sb([128, D], BF16, "zb")
    zf = zs.sb([128, D], F32, "zf")
    k.op("pool", lambda e: e.memset(zb[:], 0.0), writes=["zb"])
    k.op("pool", lambda e: e.memset(zf[:], 0.0), writes=["zf"])
    for r in range(NSLOT // 128 + 1):
        k.dma("sp", XS[r * 128:(r + 1) * 128, :], zb[:], reads=["zb"], writes=["XSz"])
    k.dma("sp", YSLa[NSLOT:NSLOT + 128, :], zf[:, 0:D // 2], reads=["zf"], writes=["YSLz"])
    k.dma("sp", YSLb[NSLOT:NSLOT + 128, :], zf[:, 0:D // 2], reads=["zf"], writes=["YSLz2"])
    zs.close()
    w_ada, t_ada = dist_weight("w_ada", D, 6 * D)
    w_in, t_in = dist_weight("w_in", D, 16448)
    pool_w2, t_pw = dist_weight("pool_w", 2048, 512)
    w_bp, t_bp = dist_weight("w_bp", D, D)
    w_bs, t_bs = dist_weight("w_bs", 4096, D)
    w_out, t_out = dist_weight("w_out", D, D)
    if stage > 7:
        cw_gu = [cast_weight("w_gu%d" % gq, 8 * D, 4096, lazy=True) for gq in range(4)]
        cw_dn = [cast_weight("w_dn%d" % gq, 8 * D, D, lazy=True) for gq in range(4)]
    WT = {id(w_ada): t_ada[-1], id(w_in): t_in[-1], id(pool_w2): t_pw[-1], id(w_bp): t_bp[-1], id(w_bs): t_bs[-1], id(w_out): t_out[-1]}


    s1 = Scope(nc, k)
    cts = s1.sb([128, 16], F32, "cts")
    k.dma("sp", cts[:], cT, writes=["cts"])
    k.op("act", lambda e: e.activation(out=cts[:], in_=cts[:], func=AF.Silu), reads=["cts"], writes=["cts"])
    lhsc = s1.sb([128, 16, 128], BF16, "lhsc")
    for kc in range(16):
        k.op("dve", lambda e, kc=kc: e.tensor_copy(lhsc[:, kc, :], cts[:, kc:kc + 1].to_broadcast([128, 128])),
             reads=["cts"], writes=["lhsc"])
    bb = [s1.sb([128, 512], F32, "bb%d" % i) for i in range(2)]
    mo = [s1.sb([128, 512], F32, "mo%d" % i) for i in range(2)]
    for s in range(24):
        pump(4)
        buf, wkeys = load_slab(w_ada, s * 512, 512, 16)
        bt = bb[s % 2]
        bk = "bb%d" % (s % 2)
        mt_ = mo[s % 2]
        mk_ = "mo%d" % (s % 2)
        k.dma("sp", bt[:], b_ada[0:1, s * 512:(s + 1) * 512].partition_broadcast(128), writes=[bk])
        ps, pk = next_ps()
        for kc in range(16):
            k.mm(lambda e, kc=kc: e.matmul(ps[:], lhsT=lhsc[:, kc, :], rhs=buf[:, kc, :], start=(kc == 0), stop=(kc == 15)),
                 reads=["lhsc"] + wkeys, writes=[pk], last=(kc == 15))
        addc = 1.0 if (s // 4) in (1, 4) else 0.0
        k.op("dve", lambda e: e.scalar_tensor_tensor(out=mt_[:], in0=ps[:], scalar=addc, in1=bt[:],
                                                     op0=ALU.add, op1=ALU.add), reads=[pk, bk], writes=[mk_])
        k.dma("sp", MODB[:, s * 512:(s + 1) * 512], mt_[:], reads=[mk_], writes=["MODB"])
    s1.close()
    if stage <= 1:
        return nc, k

    s3 = Scope(nc, k)
    uT = s3.sb([128, 16, TT], BF16, "uT")
    s2 = Scope(nc, k)
    sh1 = s2.sb([128, D], F32, "sh1")
    sc1 = s2.sb([128, D], F32, "sc1")
    k.dma("sp", sh1[:], MODB[:, 0:D], reads=["MODB"], writes=["sh1"])
    k.dma("sp", sc1[:], MODB[:, D:2 * D], reads=["MODB"], writes=["sc1"])
    hm = s2.sb([128, 1], F32, "hm")
    k.dma("sp", hm[:], hmask, writes=["hm"])
    xt = [s2.sb([128, D], F32, "xt%d" % i) for i in range(2)]
    sq = s2.sb([128, D], F32, "sq")
    ub = s2.sb([128, D], BF16, "ub")
    for ti in range(17):
        pump(4)
        x_t = xt[ti % 2]
        xk = "xt%d" % (ti % 2)
        k.dma("sp", x_t[:], xh[ti * 128:(ti + 1) * 128, :], writes=[xk])
        rms_rstd(sq[:], x_t[:], xk, D)
        k.op("dve", lambda e: e.scalar_tensor_tensor(out=sq[:], in0=x_t[:], scalar=ss[:, 0:1], in1=sc1[:], op0=ALU.mult, op1=ALU.mult),
             reads=[xk, "ss", "sc1"], writes=["sq"])
        k.op("dve", lambda e: e.tensor_tensor(out=ub[:], in0=sq[:], in1=sh1[:], op=ALU.add), reads=["sq", "sh1"], writes=["ub"])
        if ti == 0:
            k.op("dve", lambda e: e.tensor_scalar(out=ub[:], in0=ub[:], scalar1=hm[:, 0:1], scalar2=None, op0=ALU.mult),
                 reads=["ub", "hm"], writes=["ub"])
        for kq in range(4):
            pb, pbk = next_psb()
            for j in range(4):
                k.op("pe", lambda e, j=j: e.transpose(pb[:, j * 128:(j + 1) * 128], ub[:, (4 * kq + j) * 128:(4 * kq + j + 1) * 128], ident[:]),
                     reads=["ub", "ident"], writes=[pbk])
            k.op("act", lambda e: e.copy(uT[:, 4 * kq:4 * kq + 4, ti * 128:(ti + 1) * 128], pb[:].rearrange("p (j t) -> p j t", j=4)),
                 reads=[pbk], writes=["uT"])
    s2.close()

    TB = [(0, 512), (512, 512), (1024, 512), (1536, 512), (2048, 128)]
    TBO = [(128, 512), (640, 512), (1152, 512), (1664, 512)]
    rowf = [s3.sb([128, TT], F32, "rowf%d" % i) for i in range(2)]
    rfi = [0]

    def proj_fm(buf, wkeys, fc, KC=16, src=None, srck="uT", tbs=TB):
        pump(4)
        i = rfi[0] % 2
        rfi[0] += 1
        rf = rowf[i]
        rk = "rowf%d" % i
        sr = src if src is not None else uT
        for (t0, n) in tbs:
            ps, pk = next_ps()
            for kc in range(KC):
                k.mm(lambda e, kc=kc: e.matmul(ps[:, 0:n], lhsT=buf[:, kc, fc * 128:(fc + 1) * 128], rhs=sr[:, kc, t0:t0 + n],
                                               start=(kc == 0), stop=(kc == KC - 1)),
                     reads=wkeys + [srck], writes=[pk], last=(kc == KC - 1))
            if (t0 // 512) % 2 == 0:
                k.op("act", lambda e: e.copy(rf[:, t0:t0 + n], ps[:, 0:n]), reads=[pk], writes=[rk])
            else:
                k.op("dve", lambda e: e.tensor_copy(rf[:, t0:t0 + n], ps[:, 0:n]), reads=[pk], writes=[rk])
        return rf, rk

    cw = s3.sb([128, 48, 4], F32, "cw")
    cb = s3.sb([128, 48], F32, "cb")
    k.dma("sp", cw[:], cwT, writes=["cw"])
    k.dma("sp", cb[:], cbT, writes=["cb"])
    acc = s3.sb([128, T], F32, "acc")
    obf = [s3.sb([128, T], BF16, "obf%d" % i) for i in range(2)]
    obi = [0]

    def next_ob():
        i = obi[0] % 2
        obi[0] += 1
        return obf[i], "obf%d" % i

    stg = s3.sb([128, 16, 128], BF16, "stg")

    for sl in range(12):
        buf, wkeys = load_slab(w_in, 6144 + sl * 512, 512, 16)
        for fc in range(4):
            ch = sl * 4 + fc
            rf, rk = proj_fm(buf, wkeys, fc)
            k.op("act", lambda e: e.activation(out=acc[:], in_=rf[:, 128:TT], func=AF.Identity, scale=cw[:, ch, 3:4], bias=cb[:, ch:ch + 1]),
                 reads=[rk, "cw", "cb"], writes=["acc"])
            for j in (1, 2, 3):
                k.op("dve", lambda e, j=j: e.scalar_tensor_tensor(out=acc[:], in0=rf[:, 128 - j:TT - j], scalar=cw[:, ch, 3 - j:4 - j], in1=acc[:],
                                                                 op0=ALU.mult, op1=ALU.add), reads=[rk, "cw", "acc"], writes=["acc"])
            ob, ok_ = next_ob()
            k.op("act", lambda e: e.activation(out=ob[:], in_=acc[:], func=AF.Silu), reads=["acc"], writes=[ok_])
            if ch >= 32:
                k.dma("sp", BCT[(ch - 32) * 128:(ch - 31) * 128, :], ob[:], reads=[ok_], writes=["BCT"])
            if ch < 40:
                for tq in range(4):
                    pb, pbk = next_psb()
                    for j in range(4):
                        tt = tq * 4 + j
                        k.op("pe", lambda e, j=j, tt=tt: e.transpose(pb[:, j * 128:(j + 1) * 128], ob[:, tt * 128:(tt + 1) * 128], ident[:]),
                             reads=[ok_, "ident"], writes=[pbk])
                    k.op("dve", lambda e: e.tensor_copy(stg[:, tq * 4:tq * 4 + 4, :], pb[:].rearrange("p (j t) -> p j t", j=4)),
                         reads=[pbk], writes=["stg"])
                dst = XTOK[:, ch * 128:(ch + 1) * 128].rearrange("(ti p) c -> p ti c", p=128)
                for tq in range(4):
                    k.dma("sp", dst[:, tq * 4:tq * 4 + 4, :], stg[:, tq * 4:tq * 4 + 4, :], reads=["stg"], writes=["XTOK"])

    dtb = s3.sb([128, 64], F32, "dtb")
    k.dma("sp", dtb[:], dt_bias.partition_broadcast(128), writes=["dtb"])
    buf, wkeys = load_slab(w_in, 12288, 64, 16)
    for ti in range(16):
        ps, pk = next_ps()
        for kc in range(16):
            k.mm(lambda e, kc=kc: e.matmul(ps[:, 0:64], lhsT=uT[:, kc, 128 + ti * 128:256 + ti * 128], rhs=buf[:, kc, 0:64],
                                           start=(kc == 0), stop=(kc == 15)), reads=wkeys + ["uT"], writes=[pk], last=(kc == 15))
        k.op("dve", lambda e: e.tensor_tensor(out=dt_all[:, ti, :], in0=ps[:, 0:64], in1=dtb[:], op=ALU.add), reads=[pk, "dtb"], writes=["dt_all"])
    k.op("act", lambda e: e.activation(out=dt_all[:], in_=dt_all[:], func=AF.Exp), reads=["dt_all"], writes=["dt_all"])
    k.op("act", lambda e: e.activation(out=dt_all[:], in_=dt_all[:], func=AF.Ln, bias=1.0), reads=["dt_all"], writes=["dt_all"])

    zt = [s3.sb([128, 512], BF16, "zt%d" % i) for i in range(2)]
    zi = [0]
    for sl in range(8):
        buf, wkeys = load_slab(w_in, 2048 + sl * 512, 512, 16)
        for ti in range(16):
            ps, pk = next_ps()
            for kc in range(16):
                k.mm(lambda e, kc=kc: e.matmul(ps[:], lhsT=uT[:, kc, 128 + ti * 128:256 + ti * 128], rhs=buf[:, kc, :],
                                               start=(kc == 0), stop=(kc == 15)), reads=wkeys + ["uT"], writes=[pk], last=(kc == 15))
            z_t = zt[zi[0] % 2]
            zk = "zt%d" % (zi[0] % 2)
            zi[0] += 1
            k.op("act", lambda e: e.activation(out=z_t[:], in_=ps[:], func=AF.Silu), reads=[pk], writes=[zk])
            k.dma("sp", ZT[ti * 128:(ti + 1) * 128, sl * 512:(sl + 1) * 512], z_t[:], reads=[zk], writes=["ZT"])

    for gi, (c0, dst) in enumerate(((12352, GP), (14400, GS))):
        for sl in range(4):
            buf, wkeys = load_slab(w_in, c0 + sl * 512, 512, 16)
            for fc in range(4):
                rf, rk = proj_fm(buf, wkeys, fc, tbs=TBO)
                ob, ok_ = next_ob()
                k.op("act", lambda e: e.activation(out=ob[:], in_=rf[:, 128:TT], func=AF.Sigmoid), reads=[rk], writes=[ok_])
                k.dma("sp", dst[(sl * 4 + fc) * 128:(sl * 4 + fc + 1) * 128, :], ob[:], reads=[ok_], writes=["G%d" % gi])

    icn = s3.sb([128, T], F32, "icn")
    diffT = s3.sb([128, 4, T], BF16, "diffT")
    pwa = s3.sb([128, TT], F32, "pwa")
    pwb = s3.sb([128, TT], F32, "pwb")
    pscT = s3.sb([128, 16], F32, "pscT")
    k.dma("sp", pscT[:], pool_scaleT, writes=["pscT"])
    for g in range(4):
        w = (2, 4, 8, 16)[g]
        k.dma("sp", icn[:], invcnt[g:g + 1, :].partition_broadcast(128), writes=["icn"])
        buf, wkeys = load_slab(w_in, g * 512, 512, 16)
        for fc in range(4):
            rf, rk = proj_fm(buf, wkeys, fc)
            cur, curk = rf, rk
            sh = 1
            pp = [(pwa, "pwa"), (pwb, "pwb")]
            pi = 0
            while sh < w:
                nx, nxk = pp[pi % 2]
                pi += 1
                k.op("dve", lambda e, cur=cur, nx=nx, sh=sh: e.tensor_tensor(out=nx[:, 16:TT], in0=cur[:, 16:TT], in1=cur[:, 16 - sh:TT - sh], op=ALU.add),
                     reads=[curk], writes=[nxk])
                cur, curk = nx, nxk
                sh *= 2
            k.op("dve", lambda e, cur=cur: e.tensor_tensor(out=acc[:], in0=cur[:, 128:TT], in1=icn[:], op=ALU.mult), reads=[curk, "icn"], writes=["acc"])
            k.op("dve", lambda e: e.tensor_tensor(out=diffT[:, fc, :], in0=acc[:], in1=rf[:, 128:TT], op=ALU.subtract), reads=["acc", rk], writes=["diffT"])
        pbuf, pkeys = load_slab(pool_w2, 0, 512, 4, row0=g * 512)
        for dc in range(4):
            rf, rk = proj_fm(pbuf, pkeys, dc, KC=4, src=diffT, srck="diffT", tbs=[(a, 512) for a in (0, 512, 1024, 1536)])
            ob, ok_ = next_ob()
            k.op("act", lambda e: e.activation(out=ob[:], in_=rf[:, 0:T], func=AF.Copy, scale=pscT[:, g * 4 + dc:g * 4 + dc + 1]), reads=[rk, "pscT"], writes=[ok_])
            k.dma("sp", YP[(g * 4 + dc) * 128:(g * 4 + dc + 1) * 128, :], ob[:], reads=[ok_], writes=["YP"])
    s3.close()
    if stage <= 3:
        return nc, k

    s4 = Scope(nc, k)
    abc = s4.sb([128, 64], F32, "abc")
    dsk = s4.sb([128, 64], F32, "dsk")
    k.dma("sp", abc[:], a_log.partition_broadcast(128), writes=["abc"])
    k.op("act", lambda e: e.activation(out=abc[:], in_=abc[:], func=AF.Exp), reads=["abc"], writes=["abc"])
    k.op("dve", lambda e: e.tensor_scalar(out=abc[:], in0=abc[:], scalar1=-1.0, scalar2=None, op0=ALU.mult), reads=["abc"], writes=["abc"])
    k.dma("sp", dsk[:], d_skip.partition_broadcast(128), writes=["dsk"])
    nwb = s4.sb([128, 4096], F32, "nwb")
    k.dma("sp", nwb[:], ssd_norm_w.partition_broadcast(128), writes=["nwb"])
    dacs_all = s4.sb([128, 16, 64], F32, "dacs_all")
    tot_all = s4.sb([128, 16, 64], F32, "tot_all")
    S = s4.sb([128, 4096], F32, "S")
    Sbf = s4.sb([128, 4096], BF16, "Sbf")
    dsum = s4.sb([128, 64], F32, "dsum")
    xb_t = [s4.sb([128, 5120], BF16, "xb_t%d" % i) for i in range(2)]
    xdtd = s4.sb([128, 4096], BF16, "xdtd")
    xdt = s4.sb([128, 4096], BF16, "xdt")
    tmp64 = s4.sb([128, 64], F32, "tmp64")
    wend = s4.sb([128, 64], F32, "wend")
    cdec = s4.sb([128, 64], F32, "cdec")
    da = s4.sb([128, 64], F32, "da")
    k.op("pool", lambda e: e.memset(S[:], 0.0), writes=["S"])
    k.op("pool", lambda e: e.memset(dsum[:], 0.0), writes=["dsum"])

    def tile_dt_stuff(ti, x_t, xk, first_pass):
        dtt = dt_all[:, ti, :]
        if first_pass:
            k.op("dve", lambda e: e.tensor_tensor(out=da[:], in0=dtt, in1=abc[:], op=ALU.mult), reads=["dt_all", "abc"], writes=["da"])
            ps, pk = next_ps()
            k.mm(lambda e: e.matmul(ps[:, 0:64], lhsT=tri[:], rhs=da[:], start=True, stop=True), reads=["tri", "da"], writes=[pk], last=True)
            k.op("dve", lambda e: e.tensor_copy(dacs_all[:, ti, :], ps[:, 0:64]), reads=[pk], writes=["dacs_all"])
            ps2, pk2 = next_ps()
            k.mm(lambda e: e.matmul(ps2[:, 0:64], lhsT=onesf[:], rhs=da[:], start=True, stop=True), reads=["onesf", "da"], writes=[pk2], last=True)
            k.op("dve", lambda e: e.tensor_copy(tot_all[:, ti, :], ps2[:, 0:64]), reads=[pk2], writes=["tot_all"])
            k.op("dve", lambda e: e.tensor_tensor(out=dsum[:], in0=dsum[:], in1=tot_all[:, ti, :], op=ALU.add), reads=["dsum", "tot_all"], writes=["dsum"])
        k.op("dve", lambda e: e.tensor_tensor(out=tmp64[:], in0=tot_all[:, ti, :], in1=dacs_all[:, ti, :], op=ALU.subtract),
             reads=["tot_all", "dacs_all"], writes=["tmp64"])
        k.op("act", lambda e: e.activation(out=tmp64[:], in_=tmp64[:], func=AF.Exp), reads=["tmp64"], writes=["tmp64"])
        k.op("dve", lambda e: e.tensor_tensor(out=wend[:], in0=tmp64[:], in1=dtt, op=ALU.mult), reads=["tmp64", "dt_all"], writes=["wend"])
        k.op("act", lambda e: e.activation(out=cdec[:], in_=tot_all[:, ti, :], func=AF.Exp), reads=["tot_all"], writes=["cdec"])
        k.op("dve", lambda e: e.tensor_tensor(out=xdtd[:].rearrange("p (h j) -> p h j", h=64), in0=x_t[:, 0:4096].rearrange("p (h j) -> p h j", h=64),
                                              in1=wend[:].unsqueeze(2).to_broadcast([128, 64, 64]), op=ALU.mult), reads=[xk, "wend"], writes=["xdtd"])

    def state_update(x_t, xk):
        for g in range(8):
            ps, pk = next_ps()
            k.mm(lambda e: e.matmul(ps[:], lhsT=x_t[:, 4096 + 128 * g:4096 + 128 * (g + 1)], rhs=xdtd[:, 512 * g:512 * (g + 1)], start=True, stop=True),
                 reads=[xk, "xdtd"], writes=[pk], last=True)
            Sg = S[:, 512 * g:512 * (g + 1)]
            k.op("dve", lambda e: e.tensor_tensor(out=Sg.rearrange("p (h j) -> p h j", h=8), in0=Sg.rearrange("p (h j) -> p h j", h=8),
                                                  in1=cdec[:, 8 * g:8 * g + 8].unsqueeze(2).to_broadcast([128, 8, 64]), op=ALU.mult),
                 reads=["S", "cdec"], writes=["S"])
            k.op("dve", lambda e: e.tensor_tensor(out=Sg, in0=Sg, in1=ps[:], op=ALU.add), reads=["S", pk], writes=["S"])

    for ti in range(16):
        x_t = xb_t[ti % 2]
        xk = "xb_t%d" % (ti % 2)
        k.dma("sp", x_t[:], XTOK[ti * 128:(ti + 1) * 128, :], writes=[xk])
        pump(8)
        tile_dt_stuff(ti, x_t, xk, True)
        state_update(x_t, xk)
    agi = [nc.dram_tensor("agi%d" % i, [128, 1024], F32) for i in range(4)] + [nc.dram_tensor("agi4", [128, 64], F32)]
    ago = [nc.dram_tensor("ago%d" % i, [512, 1024], F32) for i in range(4)] + [nc.dram_tensor("ago4", [512, 64], F32)]
    for i in range(5):
        srcS = S[:, 1024 * i:1024 * (i + 1)] if i < 4 else dsum[:]
        k.dma("sp", agi[i][:, :], srcS, reads=["S", "dsum"], writes=["agi%d" % i])
        k._deps("pool", ["agi%d" % i], [])
        nc.gpsimd.collective_compute("AllGather", ALU.bypass, replica_groups=RG,
                                     ins=[agi[i].ap().opt()], outs=[ago[i].ap().opt()]).then_inc(k.cc_sem)
        cc_n[0] += 1
    k._wait_tok("pool", ("cc", cc_n[0]))
    k.op("pool", lambda e: e.memset(tmp64[:, 0:1], 0.0), reads=["tmp64"], writes=["ag_out", "tmp64"])
    if stage > 7:
        pump(10 ** 9)
        w_gu_p, t_gu, w_dn_p, t_dn = [], [], [], []
        for gq in range(4):
            wv_, tv_ = gather_weight(cw_gu[gq])
            w_gu_p.append(wv_)
            t_gu += tv_
        for gq in range(4):
            wv_, tv_ = gather_weight(cw_dn[gq])
            w_dn_p.append(wv_)
            t_dn += tv_
    cms = s4.sb([128, 16], F32, "cms")
    cfs = s4.sb([128, 4], F32, "cfs")
    k.dma("sp", cms[:], cm, writes=["cms"])
    k.dma("sp", cfs[:], cf, writes=["cfs"])
    dsj = s4.sb([128, 4, 64], F32, "dsj")
    for j in range(4):
        k.dma("sp", dsj[:, j, :], ago[4][j * 128:(j + 1) * 128, :], reads=["ag_out"], writes=["dsj%d" % j])
    dsjk = ["dsj%d" % j for j in range(4)]
    k.op("pool", lambda e: e.memset(S[:], 0.0), reads=["S"], writes=["S"])
    Fj = s4.sb([128, 4096], F32, "Fj")
    coef = s4.sb([128, 64], F32, "coef")
    for j in range(3):
        for i4 in range(4):
            k.dma("sp", Fj[:, 1024 * i4:1024 * (i4 + 1)], ago[i4][j * 128:(j + 1) * 128, :], reads=["ag_out"], writes=["Fj"] if i4 == 0 else ["Fjx%d" % i4])
        k.op("dve", lambda e: e.tensor_scalar(out=coef[:], in0=dsj[:, 0, :], scalar1=cms[:, 4 * j:4 * j + 1], scalar2=None, op0=ALU.mult),
             reads=dsjk + ["cms"], writes=["coef"])
        for m in range(1, 4):
            k.op("dve", lambda e, m=m: e.scalar_tensor_tensor(out=coef[:], in0=dsj[:, m, :], scalar=cms[:, 4 * j + m:4 * j + m + 1], in1=coef[:],
                                                             op0=ALU.mult, op1=ALU.add), reads=dsjk + ["cms", "coef"], writes=["coef"])
        k.op("act", lambda e: e.activation(out=coef[:], in_=coef[:], func=AF.Exp), reads=["coef"], writes=["coef"])
        k.op("dve", lambda e: e.tensor_scalar(out=coef[:], in0=coef[:], scalar1=cfs[:, j:j + 1], scalar2=None, op0=ALU.mult), reads=["coef", "cfs"], writes=["coef"])
        k.op("dve", lambda e: e.tensor_tensor(out=Fj[:].rearrange("p (h j) -> p h j", h=64), in0=Fj[:].rearrange("p (h j) -> p h j", h=64),
                                              in1=coef[:].unsqueeze(2).to_broadcast([128, 64, 64]), op=ALU.mult), reads=["Fj", "Fjx1", "Fjx2", "Fjx3", "coef"], writes=["Fj", "Fjx1", "Fjx2", "Fjx3"])
        k.op("dve", lambda e: e.tensor_tensor(out=S[:], in0=S[:], in1=Fj[:], op=ALU.add), reads=["S", "Fj", "Fjx1", "Fjx2", "Fjx3"], writes=["S"])

    bct = [s4.sb([128, 16, 128], BF16, "bct%d" % i) for i in range(2)]
    z_t = [s4.sb([128, 4096], BF16, "z_t%d" % i) for i in range(2)]
    edacs = s4.sb([128, 64], F32, "edacs")
    cbm = s4.sb([128, 128], F32, "cbm")
    Xd = s4.sb([128, 1024], F32, "Xd")
    Lt8 = s4.sb([128, 1024], F32, "Lt8")
    Mt8 = s4.sb([128, 1024], BF16, "Mt8")
    yg = s4.sb([128, 512], F32, "yg")
    yo = s4.sb([128, 512], F32, "yo")
    ybf = s4.sb([128, 512], BF16, "ybf")
    ysq = s4.sb([128, 512], F32, "ysq")
    ystg = s4.sb([128, 4, 128], BF16, "ystg")
    BCTv = BCT.rearrange("(c p) t -> p c t", p=128)
    for ti in range(16):
        x_t = xb_t[ti % 2]
        xk = "xb_t%d" % (ti % 2)
        k.dma("sp", x_t[:], XTOK[ti * 128:(ti + 1) * 128, :], writes=[xk])
        bc = bct[ti % 2]
        bck = "bct%d" % (ti % 2)
        bckeys = []
        for c0 in range(0, 16, 4):
            kk = bck if c0 == 0 else bck + "x%d" % c0
            bckeys.append(kk)
            k.dma("sp", bc[:, c0:c0 + 4, :], BCTv[:, c0:c0 + 4, ti * 128:(ti + 1) * 128], writes=[kk])
        zz = z_t[ti % 2]
        zk = "z_t%d" % (ti % 2)
        k.dma("sp", zz[:], ZT[ti * 128:(ti + 1) * 128, :], writes=[zk])
        tile_dt_stuff(ti, x_t, xk, False)
        k.op("dve", lambda e: e.tensor_tensor(out=xdt[:].rearrange("p (h j) -> p h j", h=64), in0=x_t[:, 0:4096].rearrange("p (h j) -> p h j", h=64),
                                              in1=dt_all[:, ti, :].unsqueeze(2).to_broadcast([128, 64, 64]), op=ALU.mult), reads=[xk, "dt_all"], writes=["xdt"])
        k.op("act", lambda e: e.activation(out=edacs[:], in_=dacs_all[:, ti, :], func=AF.Exp), reads=["dacs_all"], writes=["edacs"])
        k.op("act", lambda e: e.copy(Sbf[:], S[:]), reads=["S"], writes=["Sbf"])
        for g in range(8):
            ps, pk = next_ps()
            k.mm(lambda e: e.matmul(ps[:, 0:128], lhsT=bc[:, g, :], rhs=bc[:, 8 + g, :], start=True, stop=True), reads=bckeys, writes=[pk], last=True)
            k.op("dve", lambda e: e.tensor_tensor(out=cbm[:], in0=ps[:, 0:128], in1=tri[:], op=ALU.mult), reads=[pk, "tri"], writes=["cbm"])
            psy, pyk = PSY, "psy"
            k.op("pool", lambda e: e.tensor_tensor(out=Xd[:].rearrange("p (h l) -> p h l", h=8), in0=identf[:].unsqueeze(1).to_broadcast([128, 8, 128]),
                                                  in1=dacs_all[:, ti, 8 * g:8 * g + 8].unsqueeze(2).to_broadcast([128, 8, 128]), op=ALU.mult),
                 reads=["identf", "dacs_all"], writes=["Xd"])
            for hq in range(2):
                psr, prk = next_ps()
                k.mm(lambda e: e.matmul(psr[:], lhsT=onesf[:], rhs=Xd[:, hq * 512:(hq + 1) * 512], start=True, stop=True), reads=["onesf", "Xd"], writes=[prk], last=True)
                Lh = Lt8[:, hq * 512:(hq + 1) * 512]
                k.op("dve", lambda e: e.tensor_tensor(out=Lh.rearrange("p (h l) -> p h l", h=4), in0=psr[:].rearrange("p (h l) -> p h l", h=4),
                                                      in1=dacs_all[:, ti, 8 * g + 4 * hq:8 * g + 4 * hq + 4].unsqueeze(2).to_broadcast([128, 4, 128]), op=ALU.subtract),
                     reads=[prk, "dacs_all"], writes=["Lt8_%d" % hq])
                k.op("dve", lambda e: e.tensor_scalar(out=Lh, in0=Lh, scalar1=0.0, scalar2=None, op0=ALU.min), reads=["Lt8_%d" % hq], writes=["Lt8_%d" % hq])
                k.op("act", lambda e: e.activation(out=Lh, in_=Lh, func=AF.Exp), reads=["Lt8_%d" % hq], writes=["Lt8_%d" % hq])
                Mh = Mt8[:, hq * 512:(hq + 1) * 512]
                k.op("dve", lambda e: e.tensor_tensor(out=Mh.rearrange("p (h l) -> p h l", h=4), in0=Lh.rearrange("p (h l) -> p h l", h=4),
                                                      in1=cbm[:].unsqueeze(1).to_broadcast([128, 4, 128]), op=ALU.mult),
                     reads=["Lt8_%d" % hq, "cbm"], writes=["Mt8_%d" % hq])
                for h4 in range(4):
                    hh = hq * 4 + h4
                    h = 8 * g + hh
                    k.mm(lambda e, h=h, hh=hh: e.matmul(psy[:, hh * 64:(hh + 1) * 64], lhsT=Mt8[:, hh * 128:(hh + 1) * 128], rhs=xdt[:, h * 64:(h + 1) * 64], start=True, stop=True),
                         reads=["Mt8_%d" % hq, "xdt"], writes=[pyk], last=True)
            pso, pok = next_ps()
            k.mm(lambda e: e.matmul(pso[:], lhsT=bc[:, 8 + g, :], rhs=Sbf[:, 512 * g:512 * (g + 1)], start=True, stop=True), reads=bckeys + ["Sbf"], writes=[pok], last=True)
            k.op("dve", lambda e: e.tensor_tensor(out=yo[:].rearrange("p (h j) -> p h j", h=8), in0=pso[:].rearrange("p (h j) -> p h j", h=8),
                                                  in1=edacs[:, 8 * g:8 * g + 8].unsqueeze(2).to_broadcast([128, 8, 64]), op=ALU.mult), reads=[pok, "edacs"], writes=["yo"])
            k.op("dve", lambda e: e.tensor_tensor(out=yg[:], in0=psy[:], in1=yo[:], op=ALU.add), reads=[pyk, "yo"], writes=["yg"])
            k.op("dve", lambda e: e.tensor_tensor(out=yo[:].rearrange("p (h j) -> p h j", h=8), in0=x_t[:, 512 * g:512 * (g + 1)].rearrange("p (h j) -> p h j", h=8),
                                                  in1=dsk[:, 8 * g:8 * g + 8].unsqueeze(2).to_broadcast([128, 8, 64]), op=ALU.mult), reads=[xk, "dsk", "yg"], writes=["yo"])
            k.op("dve", lambda e: e.tensor_tensor(out=yg[:], in0=yg[:], in1=yo[:], op=ALU.add), reads=["yg", "yo"], writes=["yg"])
            k.op("dve", lambda e: e.tensor_tensor(out=yg[:], in0=yg[:], in1=zz[:, 512 * g:512 * (g + 1)], op=ALU.mult), reads=["yg", zk], writes=["yg"])
            rms_rstd(ysq[:], yg[:], "yg", 512)
            k.op("dve", lambda e: e.scalar_tensor_tensor(out=ybf[:], in0=yg[:], scalar=ss[:, 0:1], in1=nwb[:, 512 * g:512 * (g + 1)], op0=ALU.mult, op1=ALU.mult),
                 reads=["yg", "ss", "nwb"], writes=["ybf"])
            pb, pbk = next_psb()
            for j in range(4):
                k.op("pe", lambda e, j=j: e.transpose(pb[:, j * 128:(j + 1) * 128], ybf[:, j * 128:(j + 1) * 128], ident[:]), reads=["ybf", "ident"], writes=[pbk])
            k.op("act", lambda e: e.copy(ystg[:], pb[:].rearrange("p (j t) -> p j t", j=4)), reads=[pbk], writes=["ystg"])
            k.dma("sp", YS[512 * g:512 * (g + 1), ti * 128:(ti + 1) * 128].rearrange("(j p) t -> p j t", p=128), ystg[:], reads=["ystg"], writes=["YS"])
        state_update(x_t, xk)
    s4.close()
    if stage <= 4:
        return nc, k

    s7 = Scope(nc, k)
    g1b = s7.sb([128, D], F32, "g1b")
    k.dma("sp", g1b[:], MODB[:, 2 * D:3 * D], writes=["g1b"])
    YPs = s7.sb([128, 16, 512], BF16, "YPs")
    YSs = s7.sb([128, 32, 512], BF16, "YSs")
    mgT = s7.sb([128, 16, 512], BF16, "mgT")
    wsb = s7.sb([128, 32, 512], BF16, "wsb")
    gpt = [s7.sb([128, 512], BF16, "gpt%d" % i) for i in range(2)]
    gst = [s7.sb([128, 512], BF16, "gst%d" % i) for i in range(2)]
    m1 = s7.sb([128, 512], F32, "m1")
    m2 = s7.sb([128, 512], F32, "m2")
    xres = [s7.sb([128, 512], F32, "xres%d" % i) for i in range(2)]
    hres = [s7.sb([128, 512], F32, "hres%d" % i) for i in range(2)]
    YPv = YP.rearrange("(c p) t -> p c t", p=128)
    YSv = YS.rearrange("(c p) t -> p c t", p=128)
    it7 = [0]
    for qt in range(4):
        t0 = qt * 512
        for c0 in range(0, 16, 4):
            k.dma("sp", YPs[:, c0:c0 + 4, :], YPv[:, c0:c0 + 4, t0:t0 + 512], writes=["YPs%d" % c0])
        for c0 in range(0, 32, 4):
            k.dma("sp", YSs[:, c0:c0 + 4, :], YSv[:, c0:c0 + 4, t0:t0 + 512], writes=["YSs%d" % c0])
        ypk = ["YPs%d" % c0 for c0 in range(0, 16, 4)]
        ysk = ["YSs%d" % c0 for c0 in range(0, 32, 4)]
        for fs in range(4):
            wp, wpk = load_slab(w_bp, fs * 512, 512, 16)
            wsk = []
            _, k1 = load_slab(w_bs, fs * 512, 512, 16, buf=wsb, key="wsb")
            wsk += k1
            src2 = w_bs[2048:4096, fs * 512:(fs + 1) * 512].rearrange("(kc p) n -> p kc n", p=128)
            for k0 in range(0, 16, 4):
                kk = "wsbh%d" % k0
                wsk.append(kk)
                k.dma("sp", wsb[:, 16 + k0:16 + k0 + 4, :], src2[:, k0:k0 + 4, :], writes=[kk])
            for fc in range(4):
                i = it7[0] % 2
                it7[0] += 1
                fr = (fs * 4 + fc) * 128
                k.dma("sp", gpt[i][:], GP[fr:fr + 128, t0:t0 + 512], writes=["gpt%d" % i])
                k.dma("sp", gst[i][:], GS[fr:fr + 128, t0:t0 + 512], writes=["gst%d" % i])
                psA, pak = next_ps()
                for kc in range(16):
                    k.mm(lambda e, kc=kc: e.matmul(psA[:], lhsT=wp[:, kc, fc * 128:(fc + 1) * 128], rhs=YPs[:, kc, :], start=(kc == 0), stop=(kc == 15)),
                         reads=wpk + ypk, writes=[pak], last=(kc == 15))
                psB, pbk_ = next_ps()
                for kc in range(32):
                    k.mm(lambda e, kc=kc: e.matmul(psB[:], lhsT=wsb[:, kc, fc * 128:(fc + 1) * 128], rhs=YSs[:, kc, :], start=(kc == 0), stop=(kc == 31)),
                         reads=wsk + ysk, writes=[pbk_], last=(kc == 31))
                k.op("dve", lambda e: e.tensor_tensor(out=m1[:], in0=psA[:], in1=gpt[i][:], op=ALU.mult), reads=[pak, "gpt%d" % i], writes=["m1"])
                k.op("dve", lambda e: e.tensor_tensor(out=m2[:], in0=psB[:], in1=gst[i][:], op=ALU.mult), reads=[pbk_, "gst%d" % i], writes=["m2"])
                k.op("dve", lambda e: e.tensor_tensor(out=mgT[:, fs * 4 + fc, :], in0=m1[:], in1=m2[:], op=ALU.add), reads=["m1", "m2"], writes=["mgT"])
        for fs in range(4):
            wo, wok = load_slab(w_out, fs * 512, 512, 16)
            for tt in range(4):
                i = it7[0] % 2
                it7[0] += 1
                tok0 = t0 + tt * 128
                k.dma("sp", xres[i][:], xh[128 + tok0:128 + tok0 + 128, fs * 512:(fs + 1) * 512], writes=["xres%d" % i])
                ps, pk = next_ps()
                for kc in range(16):
                    k.mm(lambda e, kc=kc: e.matmul(ps[:], lhsT=mgT[:, kc, tt * 128:(tt + 1) * 128], rhs=wo[:, kc, :], start=(kc == 0), stop=(kc == 15)),
                         reads=wok + ["mgT"], writes=[pk], last=(kc == 15))
                k.op("dve", lambda e: e.tensor_tensor(out=hres[i][:], in0=ps[:], in1=g1b[:, fs * 512:(fs + 1) * 512], op=ALU.mult), reads=[pk, "g1b"], writes=["hres%d" % i])
                k.op("dve", lambda e: e.tensor_tensor(out=hres[i][:], in0=hres[i][:], in1=xres[i][:], op=ALU.add), reads=["hres%d" % i, "xres%d" % i], writes=["hres%d" % i])
                k.dma("sp", H1[tok0:tok0 + 128, fs * 512:(fs + 1) * 512], hres[i][:], reads=["hres%d" % i], writes=["H1"])
                if dbg_out:
                    k.dma("sp", out[tok0:tok0 + 128, fs * 512:(fs + 1) * 512], hres[i][:], reads=["hres%d" % i], writes=["out"])
    s7.close()
    if stage <= 7:
        k.finish([])
        return nc, k

    s8 = Scope(nc, k)
    slot_i = s8.sb([128, 16, 4], I32, "slot_i")
    gate_k = s8.sb([128, 16, 4], F32, "gate_k")
    cntacc = s8.sb([128, NE], F32, "cntacc")
    ebase = s8.sb([128, NE], F32, "ebase")
    trash = s8.sb([128, 1], F32, "trash")
    brb = s8.sb([128, NE], F32, "brb")
    wr32 = s8.sb([128, 16, NE], F32, "wr32")
    whi = s8.sb([128, 16, NE], BF16, "whi")
    wlo = s8.sb([128, 16, NE], BF16, "wlo")
    k.dma("sp", ebase[:], ebase_in, writes=["ebase"])
    k.dma("sp", trash[:], trash_in, writes=["trash"])
    k.dma("sp", brb[:], b_router.partition_broadcast(128), writes=["brb"])
    k.dma("sp", wr32[:], w_router.rearrange("(kc p) e -> p kc e", p=128), writes=["wr32"])
    k.op("dve", lambda e: e.tensor_copy(whi[:], wr32[:]), reads=["wr32"], writes=["whi"])
    k.op("dve", lambda e: e.tensor_tensor(out=wr32[:], in0=wr32[:], in1=whi[:], op=ALU.subtract), reads=["wr32", "whi"], writes=["wr32"])
    k.op("dve", lambda e: e.tensor_copy(wlo[:], wr32[:]), reads=["wr32"], writes=["wlo"])
    k.op("pool", lambda e: e.memset(cntacc[:], 0.0), writes=["cntacc"])

    sA = Scope(nc, k)
    sh2 = sA.sb([128, D], F32, "sh2")
    sc2 = sA.sb([128, D], F32, "sc2")
    k.dma("sp", sh2[:], MODB[:, 3 * D:4 * D], writes=["sh2"])
    k.dma("sp", sc2[:], MODB[:, 4 * D:5 * D], writes=["sc2"])
    h1t = [sA.sb([128, D], F32, "h1t%d" % i) for i in range(2)]
    sqA = sA.sb([128, D], F32, "sqA")
    u2f = sA.sb([128, D], F32, "u2f")
    u2b = [sA.sb([128, D], BF16, "u2b%d" % i) for i in range(2)]
    ulo = sA.sb([128, D], BF16, "ulo")
    uTh = sA.sb([128, 16, 128], BF16, "uTh")
    uTl = sA.sb([128, 16, 128], BF16, "uTl")
    lg = sA.sb([128, NE], F32, "lg")
    m8 = sA.sb([128, 8], F32, "m8")
    mask = sA.sb([128, NE], F32, "mask")
    maskb = sA.sb([128, NE], BF16, "maskb")
    eg = sA.sb([128, NE], F32, "eg")
    rank = sA.sb([128, NE], F32, "rank")
    valid = sA.sb([128, NE], F32, "valid")
    inval = sA.sb([128, NE], F32, "inval")
    slotf = sA.sb([128, NE], F32, "slotf")
    gatev = sA.sb([128, NE], F32, "gatev")
    oh = sA.sb([128, NE], F32, "oh")
    t1 = sA.sb([128, NE], F32, "t1")
    sm1 = sA.sb([128, 1], F32, "sm1")
    negm = sA.sb([128, 1], F32, "negm")
    for ti in range(16):
        h_t = h1t[ti % 2]
        hk = "h1t%d" % (ti % 2)
        ub2 = u2b[ti % 2]
        ubk = "u2b%d" % (ti % 2)
        k.dma("sp", h_t[:], H1[ti * 128:(ti + 1) * 128, :], writes=[hk])
        rms_rstd(sqA[:], h_t[:], hk, D)
        k.op("dve", lambda e: e.scalar_tensor_tensor(out=u2f[:], in0=h_t[:], scalar=ss[:, 0:1], in1=sc2[:], op0=ALU.mult, op1=ALU.mult),
             reads=[hk, "ss", "sc2"], writes=["u2f"])
        k.op("dve", lambda e: e.tensor_tensor(out=u2f[:], in0=u2f[:], in1=sh2[:], op=ALU.add), reads=["u2f", "sh2"], writes=["u2f"])
        k.op("act", lambda e: e.copy(ub2[:], u2f[:]), reads=["u2f"], writes=[ubk])
        k.op("dve", lambda e: e.tensor_tensor(out=ulo[:], in0=u2f[:], in1=ub2[:], op=ALU.subtract), reads=["u2f", ubk], writes=["ulo"])
        for (srcb, srck, dstT, dstk) in ((ub2, ubk, uTh, "uTh"), (ulo, "ulo", uTl, "uTl")):
            for kq in range(4):
                pb, pbk = next_psb()
                for j in range(4):
                    k.op("pe", lambda e, j=j, srcb=srcb: e.transpose(pb[:, j * 128:(j + 1) * 128], srcb[:, (4 * kq + j) * 128:(4 * kq + j + 1) * 128], ident[:]),
                         reads=[srck, "ident"], writes=[pbk])
                k.op("act", lambda e, dstT=dstT: e.copy(dstT[:, 4 * kq:4 * kq + 4, :], pb[:].rearrange("p (j t) -> p j t", j=4)), reads=[pbk], writes=[dstk])
        ps, pk = next_ps()
        n_mm = 0
        for (aT, ak, wv, wk) in ((uTh, "uTh", whi, "whi"), (uTh, "uTh", wlo, "wlo"), (uTl, "uTl", whi, "whi")):
            for kc in range(16):
                n_mm += 1
                k.mm(lambda e, kc=kc, aT=aT, wv=wv, n_mm=n_mm: e.matmul(ps[:, 0:NE], lhsT=aT[:, kc, :], rhs=wv[:, kc, :], start=(n_mm == 1), stop=(n_mm == 48)),
                     reads=[ak, wk], writes=[pk], last=(n_mm == 48))
        k.op("dve", lambda e: e.tensor_tensor(out=lg[:], in0=ps[:, 0:NE], in1=brb[:], op=ALU.add), reads=[pk, "brb"], writes=["lg"])
        k.op("dve", lambda e: e.max(m8[:], lg[:]), reads=["lg"], writes=["m8"])
        k.op("dve", lambda e: e.tensor_scalar(out=mask[:], in0=lg[:], scalar1=m8[:, 3:4], scalar2=None, op0=ALU.is_ge), reads=["lg", "m8"], writes=["mask"])
        k.op("dve", lambda e: e.tensor_scalar(out=negm[:], in0=m8[:, 0:1], scalar1=-1.0, scalar2=None, op0=ALU.mult), reads=["m8"], writes=["negm"])
        k.op("act", lambda e: e.activation(out=eg[:], in_=lg[:], func=AF.Exp, bias=negm[:, 0:1], scale=1.0), reads=["lg", "negm"], writes=["eg"])
        k.op("dve", lambda e: e.tensor_tensor(out=eg[:], in0=eg[:], in1=mask[:], op=ALU.mult), reads=["eg", "mask"], writes=["eg"])
        k.op("dve", lambda e: e.reduce_sum(out=sm1[:], in_=eg[:], axis=AX.X), reads=["eg"], writes=["sm1"])
        k.op("dve", lambda e: e.reciprocal(sm1[:], sm1[:]), reads=["sm1"], writes=["sm1"])
        k.op("dve", lambda e: e.tensor_copy(maskb[:], mask[:]), reads=["mask"], writes=["maskb"])
        psr, prk = next_ps()
        k.mm(lambda e: e.matmul(psr[:, 0:NE], lhsT=ustr[:], rhs=maskb[:], start=True, stop=True), reads=["ustr", "maskb"], writes=[prk], last=True)
        k.op("dve", lambda e: e.tensor_tensor(out=rank[:], in0=psr[:, 0:NE], in1=cntacc[:], op=ALU.add), reads=[prk, "cntacc"], writes=["rank"])
        psc, pck = next_ps()
        k.mm(lambda e: e.matmul(psc[:, 0:NE], lhsT=onesb[:], rhs=maskb[:], start=True, stop=True), reads=["onesb", "maskb"], writes=[pck], last=True)
        k.op("dve", lambda e: e.tensor_tensor(out=cntacc[:], in0=cntacc[:], in1=psc[:, 0:NE], op=ALU.add), reads=[pck, "cntacc", "rank"], writes=["cntacc"])
        k.op("dve", lambda e: e.tensor_scalar(out=valid[:], in0=rank[:], scalar1=float(CAP), scalar2=None, op0=ALU.is_lt), reads=["rank"], writes=["valid"])
        k.op("dve", lambda e: e.tensor_tensor(out=valid[:], in0=valid[:], in1=mask[:], op=ALU.mult), reads=["valid", "mask"], writes=["valid"])
        k.op("dve", lambda e: e.tensor_scalar(out=inval[:], in0=valid[:], scalar1=-1.0, scalar2=1.0, op0=ALU.mult, op1=ALU.add), reads=["valid"], writes=["inval"])
        k.op("dve", lambda e: e.tensor_tensor(out=slotf[:], in0=rank[:], in1=ebase[:], op=ALU.add), reads=["rank", "ebase"], writes=["slotf"])
        k.op("dve", lambda e: e.tensor_tensor(out=slotf[:], in0=slotf[:], in1=valid[:], op=ALU.mult), reads=["slotf", "valid"], writes=["slotf"])
        k.op("dve", lambda e: e.scalar_tensor_tensor(out=slotf[:], in0=inval[:], scalar=trash[:, 0:1], in1=slotf[:], op0=ALU.mult, op1=ALU.add),
             reads=["inval", "trash", "slotf"], writes=["slotf"])
        k.op("dve", lambda e: e.tensor_tensor(out=gatev[:], in0=eg[:], in1=valid[:], op=ALU.mult), reads=["eg", "valid"], writes=["gatev"])
        k.op("dve", lambda e: e.tensor_scalar(out=gatev[:], in0=gatev[:], scalar1=sm1[:, 0:1], scalar2=None, op0=ALU.mult), reads=["gatev", "sm1"], writes=["gatev"])
        for kk in range(4):
            k.op("dve", lambda e, kk=kk: e.tensor_scalar(out=oh[:], in0=lg[:], scalar1=m8[:, kk:kk + 1], scalar2=None, op0=ALU.is_equal), reads=["lg", "m8"], writes=["oh"])
            k.op("dve", lambda e: e.tensor_tensor(out=t1[:], in0=oh[:], in1=slotf[:], op=ALU.mult), reads=["oh", "slotf"], writes=["t1"])
            k.op("dve", lambda e: e.reduce_sum(out=sm1[:], in_=t1[:], axis=AX.X), reads=["t1", "gatev"], writes=["sm1"])
            k.op("dve", lambda e, kk=kk: e.tensor_copy(slot_i[:, ti, kk:kk + 1], sm1[:]), reads=["sm1"], writes=["slot_i"])
            k.op("dve", lambda e: e.tensor_tensor(out=t1[:], in0=oh[:], in1=gatev[:], op=ALU.mult), reads=["oh", "gatev"], writes=["t1"])
            k.op("dve", lambda e, kk=kk: e.reduce_sum(out=gate_k[:, ti, kk:kk + 1], in_=t1[:], axis=AX.X), reads=["t1"], writes=["gate_k"])
            k.dma_custom("pool", lambda e, kk=kk: e.indirect_dma_start(
                out=XS[:, :], out_offset=bass.IndirectOffsetOnAxis(ap=slot_i[:, ti, kk:kk + 1], axis=0),
                in_=ub2[:, :], in_offset=None), reads=[ubk, "slot_i"], writes=["XS"])
    sA.close()

    sB = Scope(nc, k)
    Xg = sB.sb([128, NB, D], BF16, "Xg")
    XeT = sB.sb([128, 16, CAP], BF16, "XeT")
    actT = sB.sb([128, 16, CAP], BF16, "actT")
    bgu = sB.sb([128, NE, 32], F32, "bgu")
    k.dma("sp", bgu[:], b_guT, writes=["bgu"])
    gsb = sB.sb([128, 512], F32, "gsb")
    usb = sB.sb([128, 512], F32, "usb")
    sgb = sB.sb([128, 512], F32, "sgb")
    bdn_b = [sB.sb([128, 512], F32, "bdn%d" % i) for i in range(2)]
    yout = [sB.sb([128, 512], F32, "yout%d" % i) for i in range(2)]
    yi = [0]
    ppe_gu = D // (4 * piece_rows(8 * D, 4096))
    ppe_dn = D // (4 * piece_rows(8 * D, D))
    for ex in range(NE):
        XSv = XS[ex * CAP:(ex + 1) * CAP, :].rearrange("(b p) d -> p b d", p=128)
        for bq in range(NB):
            k.dma("sp", Xg[:, bq, :], XSv[:, bq, :], writes=["Xg%d" % bq])
        for blk in range(NB):
            for kq in range(4):
                pb, pbk = next_psb()
                for j in range(4):
                    k.op("pe", lambda e, j=j: e.transpose(pb[:, j * 128:(j + 1) * 128], Xg[:, blk, (4 * kq + j) * 128:(4 * kq + j + 1) * 128], ident[:]),
                         reads=["Xg%d" % blk, "ident"], writes=[pbk])
                k.op("act", lambda e: e.copy(XeT[:, 4 * kq:4 * kq + 4, blk * 128:(blk + 1) * 128], pb[:].rearrange("p (j t) -> p j t", j=4)),
                     reads=[pbk], writes=["XeT"])
        for s in range(4):
            wg, wgk = load_slab(w_gu_p[ex // 8], s * 512, 512, 16, row0=(ex % 8) * D, cc=t_gu[(ex + 1) * ppe_gu - 1])
            wu, wuk = load_slab(w_gu_p[ex // 8], 2048 + s * 512, 512, 16, row0=(ex % 8) * D, cc=t_gu[(ex + 1) * ppe_gu - 1])
            for fc in range(4):
                f = s * 4 + fc
                for (c0, cn) in ((0, 512), (512, CAP - 512)):
                    psG, pgk = next_ps()
                    for kc in range(16):
                        k.mm(lambda e, kc=kc: e.matmul(psG[:, 0:cn], lhsT=wg[:, kc, fc * 128:(fc + 1) * 128], rhs=XeT[:, kc, c0:c0 + cn], start=(kc == 0), stop=(kc == 15)),
                             reads=wgk + ["XeT"], writes=[pgk], last=(kc == 15))
                    psU, puk = next_ps()
                    for kc in range(16):
                        k.mm(lambda e, kc=kc: e.matmul(psU[:, 0:cn], lhsT=wu[:, kc, fc * 128:(fc + 1) * 128], rhs=XeT[:, kc, c0:c0 + cn], start=(kc == 0), stop=(kc == 15)),
                             reads=wuk + ["XeT"], writes=[puk], last=(kc == 15))
                    k.op("dve", lambda e: e.tensor_scalar(out=gsb[:, 0:cn], in0=psG[:, 0:cn], scalar1=bgu[:, ex, f:f + 1], scalar2=SWL, op0=ALU.add, op1=ALU.min),
                         reads=[pgk, "bgu"], writes=["gsb"])
                    k.op("act", lambda e: e.activation(out=sgb[:, 0:cn], in_=gsb[:, 0:cn], func=AF.Sigmoid, scale=SWA), reads=["gsb"], writes=["sgb"])
                    k.op("dve", lambda e: e.tensor_scalar(out=usb[:, 0:cn], in0=psU[:, 0:cn], scalar1=bgu[:, ex, 16 + f:17 + f], scalar2=SWL, op0=ALU.add, op1=ALU.min),
                         reads=[puk, "bgu"], writes=["usb"])
                    k.op("dve", lambda e: e.tensor_scalar(out=usb[:, 0:cn], in0=usb[:, 0:cn], scalar1=-SWL, scalar2=1.0, op0=ALU.max, op1=ALU.add), reads=["usb"], writes=["usb"])
                    k.op("dve", lambda e: e.tensor_tensor(out=gsb[:, 0:cn], in0=gsb[:, 0:cn], in1=sgb[:, 0:cn], op=ALU.mult), reads=["gsb", "sgb"], writes=["gsb"])
                    k.op("dve", lambda e: e.tensor_tensor(out=actT[:, f, c0:c0 + cn], in0=gsb[:, 0:cn], in1=usb[:, 0:cn], op=ALU.mult), reads=["gsb", "usb"], writes=["actT"])
        for ds in range(4):
            wd, wdk = load_slab(w_dn_p[ex // 8], ds * 512, 512, 16, row0=(ex % 8) * D, cc=t_dn[(ex + 1) * ppe_dn - 1])
            bd_ = bdn_b[ds % 2]
            bdk = "bdn%d" % (ds % 2)
            k.dma("sp", bd_[:], b_dn[ex:ex + 1, ds * 512:(ds + 1) * 512].partition_broadcast(128), writes=[bdk])
            for blk in range(NB):
                ps, pk = next_ps()
                for kc in range(16):
                    k.mm(lambda e, kc=kc: e.matmul(ps[:], lhsT=actT[:, kc, blk * 128:(blk + 1) * 128], rhs=wd[:, kc, :], start=(kc == 0), stop=(kc == 15)),
                         reads=wdk + ["actT"], writes=[pk], last=(kc == 15))
                yo_ = yout[yi[0] % 2]
                yk_ = "yout%d" % (yi[0] % 2)
                yi[0] += 1
                k.op("dve", lambda e: e.tensor_tensor(out=yo_[:], in0=ps[:], in1=bd_[:], op=ALU.add), reads=[pk, bdk], writes=[yk_])
                r0 = ex * CAP + blk * 128
                Yd = YSLa if ds < 2 else YSLb
                k.dma("sp", Yd[r0:r0 + 128, (ds % 2) * 512:(ds % 2 + 1) * 512], yo_[:], reads=[yk_], writes=["YSL"])
    sB.close()

    sC = Scope(nc, k)
    g2b = sC.sb([128, D], F32, "g2b")
    fnwb = sC.sb([128, D], F32, "fnwb")
    k.dma("sp", g2b[:], MODB[:, 5 * D:6 * D], writes=["g2b"])
    k.dma("sp", fnwb[:], fnw.partition_broadcast(128), writes=["fnwb"])
    h1c = [sC.sb([128, D], F32, "h1c%d" % i) for i in range(2)]
    Yg = [sC.sb([128, D], F32, "Yg%d" % i) for i in range(2)]
    accm = sC.sb([128, D], F32, "accm")
    sqC = sC.sb([128, D], F32, "sqC")
    outt = sC.sb([128, D], F32, "outt")
    gi_ = [0]
    for ti in range(16):
        h_t = h1c[ti % 2]
        hk = "h1c%d" % (ti % 2)
        k.dma("sp", h_t[:], H1[ti * 128:(ti + 1) * 128, :], writes=[hk])
        for kk in range(4):
            yg_ = Yg[gi_[0] % 2]
            ygk = "Yg%d" % (gi_[0] % 2)
            gi_[0] += 1
            k.dma_custom("pool", lambda e, kk=kk, yg_=yg_: e.indirect_dma_start(
                out=yg_[:, 0:D // 2], out_offset=None, in_=YSLa[:, :],
                in_offset=bass.IndirectOffsetOnAxis(ap=slot_i[:, ti, kk:kk + 1], axis=0)), reads=["slot_i"], writes=[ygk])
            k.dma_custom("pool", lambda e, kk=kk, yg_=yg_: e.indirect_dma_start(
                out=yg_[:, D // 2:D], out_offset=None, in_=YSLb[:, :],
                in_offset=bass.IndirectOffsetOnAxis(ap=slot_i[:, ti, kk:kk + 1], axis=0)), reads=["slot_i"], writes=[ygk + "b"])
            if kk == 0:
                k.op("dve", lambda e, yg_=yg_: e.tensor_scalar(out=accm[:], in0=yg_[:], scalar1=gate_k[:, ti, 0:1], scalar2=None, op0=ALU.mult),
                     reads=[ygk, ygk + "b", "gate_k"], writes=["accm"])
            else:
                k.op("dve", lambda e, kk=kk, yg_=yg_: e.scalar_tensor_tensor(out=accm[:], in0=yg_[:], scalar=gate_k[:, ti, kk:kk + 1], in1=accm[:],
                                                                             op0=ALU.mult, op1=ALU.add), reads=[ygk, ygk + "b", "gate_k", "accm"], writes=["accm"])
        k.op("dve", lambda e: e.tensor_tensor(out=accm[:], in0=accm[:], in1=g2b[:], op=ALU.mult), reads=["accm", "g2b"], writes=["accm"])
        k.op("dve", lambda e: e.tensor_tensor(out=accm[:], in0=accm[:], in1=h_t[:], op=ALU.add), reads=["accm", hk], writes=["accm"])
        rms_rstd(sqC[:], accm[:], "accm", D)
        k.op("dve", lambda e: e.scalar_tensor_tensor(out=outt[:], in0=accm[:], scalar=ss[:, 0:1], in1=fnwb[:], op0=ALU.mult, op1=ALU.mult),
             reads=["accm", "ss", "fnwb"], writes=["outt"])
        k.dma("sp", out[ti * 128:(ti + 1) * 128, :], outt[:], reads=["outt"], writes=["out"])
    sC.close()
    s8.close()
    k.finish([])
    return nc, k


def host_inputs(inputs, full=True):
    f = lambda a: np.ascontiguousarray(np.asarray(a, dtype=np.float32))
    x = f(inputs["x"])
    c = f(inputs["c"])
    def quarters(W2d, pr=None):
        R, C = W2d.shape
        rq = 1
        while rq * 2 * C * 2 <= (1 << 20) and (R // 4) % (rq * 2) == 0:
            rq *= 2
        pr = 4 * rq
        Wr = W2d.reshape(R // pr, 4, pr // 4, C)
        NP = (R // 4) // rq
        F = rq * C // 128
        return [np.ascontiguousarray(Wr[:, q].reshape(NP * 128, F)) for q in range(4)]

    big = {
        "w_ada_q": quarters(f(inputs["w_ada"][0]), 512), "w_in_q": quarters(f(inputs["w_in"][0]), 512),
        "pool_w_q": quarters(f(inputs["pool_w"][0]).reshape(2048, 512), 2048),
        "w_bp_q": quarters(f(inputs["w_branch_pool"][0]), 2048), "w_bs_q": quarters(f(inputs["w_branch_ssd"][0]), 4096),
        "w_out_q": quarters(f(inputs["w_out"][0]), 2048),
    }
    if full:
        wgu = np.asarray(inputs["w_gate_up"][0], dtype=np.float32)
        wdn = np.asarray(inputs["w_down"][0], dtype=np.float32)
        for gq in range(4):
            big["w_gu%d_q" % gq] = quarters(wgu[8 * gq:8 * gq + 8].reshape(8 * D, 4096))
            big["w_dn%d_q" % gq] = quarters(wdn[8 * gq:8 * gq + 8].reshape(8 * D, D))
    shared = {
        "b_ada": f(inputs["b_ada"][0]).reshape(1, -1),
        "pool_scaleT": f(np.asarray(inputs["pool_scale"][0]).reshape(16, 128).T),
        "cwT": f(np.asarray(inputs["conv_w"][0]).reshape(4, 48, 128).transpose(2, 1, 0)),
        "cbT": f(np.asarray(inputs["conv_b"][0]).reshape(48, 128).T),
        "dt_bias": f(inputs["dt_bias"][0]).reshape(1, 64), "a_log": f(inputs["a_log"][0]).reshape(1, 64),
        "d_skip": f(inputs["d_skip"][0]).reshape(1, 64), "ssd_norm_w": f(inputs["ssd_norm_w"][0]).reshape(1, 4096),
        "w_router": f(inputs["w_router"][0]), "b_router": f(inputs["b_router"][0]).reshape(1, NE),
        "b_guT": f(np.asarray(inputs["b_gate_up"][0]).reshape(NE, 32, 128).transpose(2, 0, 1)),
        "b_dn": f(inputs["b_down"][0]),
        "fnw": f(inputs["final_norm_w"]).reshape(1, D),
        "ebase": f(np.broadcast_to((np.arange(NE) * CAP)[None, :], (128, NE))),
        "trash": f((NSLOT + np.arange(128)).reshape(128, 1)),
    }
    maps = []
    for cid in range(NCORES):
        b, q = cid // 4, cid % 4
        xh = np.zeros((TT, D), np.float32)
        xh[HT:] = x[b, q * T:(q + 1) * T]
        if q > 0:
            xh[:HT] = x[b, q * T - HT:q * T]
        pos = np.arange(q * T + 1, (q + 1) * T + 1, dtype=np.float32)
        invcnt = np.stack([1.0 / np.minimum(pos, float(w)) for w in (2, 4, 8, 16)]).astype(np.float32)
        cm = np.zeros((128, 16), np.float32)
        cfv = np.zeros((128, 4), np.float32)
        for j in range(4):
            cfv[:, j] = 1.0 if j < q else 0.0
            for m in range(4):
                cm[:, 4 * j + m] = 1.0 if (j < m < q) else 0.0
        d = dict(shared)
        for kk, v in big.items():
            d[kk] = v[q]
        d.update({
            "xh": xh, "cT": f(c[b].reshape(16, 128).T), "hmask": np.full((128, 1), 1.0 if q > 0 else 0.0, np.float32),
            "invcnt": invcnt, "cm": cm, "cf": cfv,
        })
        maps.append(d)
    return maps


_CACHE = {}


def kernel(**inputs):
    maps = host_inputs(inputs)
    if "nc" not in _CACHE:
        _CACHE["nc"] = build()[0]
    res = run_bass_kernel_spmd(_CACHE["nc"], maps, core_ids=list(range(NCORES)))
    outs = [np.asarray(r["out"]) for r in res.results]
    o = np.stack(outs).reshape(2, 4 * T, D).astype(np.float32)
    return o
```
